# Optimizing a Trainium2 kernel written in Bass

```python
import math
import jax, jax.numpy as jnp
from jax import lax
import numpy as np

D_MODEL = 2048
BATCH = 2
SEQ = 8192
DEPTH = 1

SSM_GROUP = 16
SSM_WIDTH = 1024
SSM_GROUPS = SSM_WIDTH // SSM_GROUP
SSM_STATE = 64
DT_MIN = 1e-3
DT_MAX = 1e-1
SB_HEADS = 8
SB_HEAD_DIM = 128
SB_WIDTH = SB_HEADS * SB_HEAD_DIM
Q_BLOCK = 128
IN_COLS = SSM_WIDTH + 3 * SB_WIDTH + 2 * D_MODEL
SPLITS = (SSM_WIDTH, SSM_WIDTH + SB_WIDTH, SSM_WIDTH + 2 * SB_WIDTH,
          SSM_WIDTH + 3 * SB_WIDTH, SSM_WIDTH + 3 * SB_WIDTH + D_MODEL)
PEER_HEADS = 8
PEER_N_KEYS = 128
PEER_N_EXPERTS = PEER_N_KEYS * PEER_N_KEYS
PEER_TOPK = 16
PEER_QUERY_DIM = 256
PEER_HALF = PEER_QUERY_DIM // 2
PEER_CHUNK = 128
N_ADA = 6
EPS = 1e-6

kernel_name = "hybrid_s5_stickbreak_peer_adaln"


def _rms(x, g):
    xf = x.astype(jnp.float32)
    y = xf * lax.rsqrt(jnp.mean(xf * xf, axis=-1, keepdims=True) + EPS)
    return (y * g.astype(jnp.float32)).astype(x.dtype)


def _ada_norm(x, g, shift, scale):
    return _rms(x, g) * (1 + scale) + shift


def _s5(u, lam_re, lam_im, log_dt, b_re, b_im, c_re, c_im, d_skip):
    f32 = jnp.float32
    uf = u.astype(f32)
    lam = lax.complex(lam_re.astype(f32), lam_im.astype(f32))
    dt = jnp.exp(log_dt.astype(f32))[:, None]
    lam_bar = jnp.exp(lam * dt)
    b = lax.complex(b_re.astype(f32), b_im.astype(f32))
    b_bar = ((lam_bar - 1) / lam)[..., None] * b
    cmat = lax.complex(c_re.astype(f32), c_im.astype(f32))
    bu = jnp.einsum('bsgi,gpi->bsgp', uf.astype(jnp.complex64), b_bar)
    a = jnp.broadcast_to(lam_bar, (1, uf.shape[1]) + lam_bar.shape)

    def combine(left, right):
        a_l, b_l = left
        a_r, b_r = right
        return a_r * a_l, a_r * b_l + b_r

    _, states = lax.associative_scan(combine, (a, bu), axis=1)
    y = jnp.real(jnp.einsum('bsgp,gip->bsgi', states, cmat)) + d_skip.astype(f32) * uf
    return y.astype(u.dtype)


def _stick_breaking(q, k, v):
    f32 = jnp.float32
    seq = q.shape[2]
    scale = SB_HEAD_DIM ** -0.5
    outs = []
    for blk in range(seq // Q_BLOCK):
        t0 = blk * Q_BLOCK
        length = t0 + Q_BLOCK
        qb = q[:, :, t0:length].astype(f32)
        kb = k[:, :, :length].astype(f32)
        vb = v[:, :, :length].astype(f32)
        z = jnp.einsum('bhqd,bhkd->bhqk', qb, kb) * scale
        t_idx = t0 + jnp.arange(Q_BLOCK)[:, None]
        s_idx = jnp.arange(length)[None, :]
        causal = s_idx < t_idx
        log_fail = jnp.where(causal, jax.nn.log_sigmoid(-z), 0.0)
        suffix = lax.cumsum(log_fail, axis=3, reverse=True) - log_fail
        log_w = jnp.where(causal, jax.nn.log_sigmoid(z) + suffix, -jnp.inf)
        outs.append(jnp.einsum('bhqk,bhkd->bhqd', jnp.exp(log_w), vb))
    return jnp.concatenate(outs, axis=2).astype(v.dtype)


def _peer(h, wq, k1, k2, u_tab, v_tab):
    f32 = jnp.float32
    bsz, seq, d = h.shape
    t = bsz * seq
    hf = h.reshape(t, d)
    q = (hf @ wq).reshape(t, PEER_HEADS, 2, PEER_HALF).astype(f32)
    s1 = jnp.einsum('thd,nd->thn', q[:, :, 0], k1.astype(f32))
    s2 = jnp.einsum('thd,nd->thn', q[:, :, 1], k2.astype(f32))
    v1, i1 = lax.top_k(s1, PEER_TOPK)
    v2, i2 = lax.top_k(s2, PEER_TOPK)
    cand = (v1[..., :, None] + v2[..., None, :]).reshape(t, PEER_HEADS, PEER_TOPK * PEER_TOPK)
    cidx = (i1[..., :, None] * PEER_N_KEYS + i2[..., None, :]).reshape(t, PEER_HEADS, PEER_TOPK * PEER_TOPK)
    best, pos = lax.top_k(cand, PEER_TOPK)
    idx = jnp.take_along_axis(cidx, pos, axis=-1)
    gate = jax.nn.softmax(best, axis=-1).astype(h.dtype)
    n_chunks = t // PEER_CHUNK

    def chunk(args):
        xc, ic, gc = args
        ue = jnp.take(u_tab, ic, axis=0)
        act = gc * jax.nn.gelu(jnp.einsum('cd,chkd->chk', xc, ue))
        ve = jnp.take(v_tab, ic, axis=0)
        return jnp.einsum('chk,chkd->cd', act, ve)

    out = lax.map(chunk, (hf.reshape(n_chunks, PEER_CHUNK, d),
                          idx.reshape(n_chunks, PEER_CHUNK, PEER_HEADS, PEER_TOPK),
                          gate.reshape(n_chunks, PEER_CHUNK, PEER_HEADS, PEER_TOPK)))
    return out.reshape(bsz, seq, d)


def setup_inputs(seed: int = 0) -> dict:
    key = jax.random.key(seed)
    ks = jax.random.split(key, 26)
    nrm = jax.random.normal
    D = D_MODEL
    L = DEPTH
    x = nrm(ks[0], (BATCH, SEQ, D), jnp.float32)
    c = nrm(ks[1], (BATCH, D), jnp.float32)
    w_ada = nrm(ks[2], (L, D, N_ADA * D), jnp.float32) * (0.5 * D ** -0.5)
    b_ada = nrm(ks[3], (L, N_ADA * D), jnp.float32) * 0.01
    norm1_g = 1.0 + 0.02 * nrm(ks[4], (L, D), jnp.float32)
    w_in = nrm(ks[5], (L, D, IN_COLS), jnp.float32) * D ** -0.5
    lam_re = -0.5 * jnp.exp(0.01 * nrm(ks[6], (L, SSM_GROUPS, SSM_STATE), jnp.float32))
    lam_im = (jnp.pi * jnp.arange(SSM_STATE, dtype=jnp.float32))[None, None, :] \
        + 0.01 * nrm(ks[7], (L, SSM_GROUPS, SSM_STATE), jnp.float32)
    log_dt = jax.random.uniform(ks[8], (L, SSM_GROUPS), jnp.float32,
                                math.log(DT_MIN), math.log(DT_MAX))
    ssm_b_re = nrm(ks[9], (L, SSM_GROUPS, SSM_STATE, SSM_GROUP), jnp.float32) * (2 * SSM_GROUP) ** -0.5
    ssm_b_im = nrm(ks[10], (L, SSM_GROUPS, SSM_STATE, SSM_GROUP), jnp.float32) * (2 * SSM_GROUP) ** -0.5
    ssm_c_re = nrm(ks[11], (L, SSM_GROUPS, SSM_GROUP, SSM_STATE), jnp.float32) * 0.5
    ssm_c_im = nrm(ks[12], (L, SSM_GROUPS, SSM_GROUP, SSM_STATE), jnp.float32) * 0.5
    ssm_d = nrm(ks[13], (L, SSM_GROUPS, SSM_GROUP), jnp.float32) * 0.5
    w_glu = nrm(ks[14], (L, SSM_WIDTH, 2 * D), jnp.float32) * SSM_WIDTH ** -0.5
    q_norm_g = 1.0 + 0.02 * nrm(ks[15], (L, SB_HEAD_DIM), jnp.float32)
    k_norm_g = 1.0 + 0.02 * nrm(ks[16], (L, SB_HEAD_DIM), jnp.float32)
    w_att_up = nrm(ks[17], (L, SB_WIDTH, D), jnp.float32) * SB_WIDTH ** -0.5
    w_out = nrm(ks[18], (L, D, D), jnp.float32) * D ** -0.5
    norm2_g = 1.0 + 0.02 * nrm(ks[19], (L, D), jnp.float32)
    peer_wq = nrm(ks[20], (L, D, PEER_HEADS * PEER_QUERY_DIM), jnp.float32) * D ** -0.5
    peer_k1 = nrm(ks[21], (L, PEER_N_KEYS, PEER_HALF), jnp.float32) * PEER_HALF ** -0.5
    peer_k2 = nrm(ks[22], (L, PEER_N_KEYS, PEER_HALF), jnp.float32) * PEER_HALF ** -0.5
    peer_u = nrm(ks[23], (L, PEER_N_EXPERTS, D), jnp.float32) * D ** -0.5
    peer_v = nrm(ks[24], (L, PEER_N_EXPERTS, D), jnp.float32)
    return {"x": x, "c": c, "w_ada": w_ada, "b_ada": b_ada, "norm1_g": norm1_g, "w_in": w_in,
            "lam_re": lam_re, "lam_im": lam_im, "log_dt": log_dt,
            "ssm_b_re": ssm_b_re, "ssm_b_im": ssm_b_im, "ssm_c_re": ssm_c_re, "ssm_c_im": ssm_c_im,
            "ssm_d": ssm_d, "w_glu": w_glu, "q_norm_g": q_norm_g, "k_norm_g": k_norm_g,
            "w_att_up": w_att_up, "w_out": w_out, "norm2_g": norm2_g, "peer_wq": peer_wq,
            "peer_k1": peer_k1, "peer_k2": peer_k2, "peer_u": peer_u, "peer_v": peer_v}


def reference(x, c, w_ada, b_ada, norm1_g, w_in, lam_re, lam_im, log_dt,
              ssm_b_re, ssm_b_im, ssm_c_re, ssm_c_im, ssm_d, w_glu, q_norm_g, k_norm_g,
              w_att_up, w_out, norm2_g, peer_wq, peer_k1, peer_k2, peer_u, peer_v):
    bsz, seq, _ = x.shape
    c_act = jax.nn.silu(c)
    for l in range(DEPTH):
        mod = c_act @ w_ada[l] + b_ada[l]
        sh1, sc1, gt1, sh2, sc2, gt2 = [m[:, None, :] for m in jnp.split(mod, N_ADA, axis=-1)]

        h = _ada_norm(x, norm1_g[l], sh1, sc1)
        proj = h @ w_in[l]
        u_ssm, q, k, v, g_ssm, g_att = jnp.split(proj, SPLITS, axis=-1)

        y = _s5(u_ssm.reshape(bsz, seq, SSM_GROUPS, SSM_GROUP), lam_re[l], lam_im[l], log_dt[l],
                ssm_b_re[l], ssm_b_im[l], ssm_c_re[l], ssm_c_im[l], ssm_d[l])
        y = jax.nn.gelu(y.reshape(bsz, seq, SSM_WIDTH))
        y_val, y_gate = jnp.split(y @ w_glu[l], 2, axis=-1)
        ssm_out = y_val * jax.nn.sigmoid(y_gate)

        qh = _rms(q.reshape(bsz, seq, SB_HEADS, SB_HEAD_DIM), q_norm_g[l]).transpose(0, 2, 1, 3)
        kh = _rms(k.reshape(bsz, seq, SB_HEADS, SB_HEAD_DIM), k_norm_g[l]).transpose(0, 2, 1, 3)
        vh = v.reshape(bsz, seq, SB_HEADS, SB_HEAD_DIM).transpose(0, 2, 1, 3)
        att = _stick_breaking(qh, kh, vh).transpose(0, 2, 1, 3).reshape(bsz, seq, SB_WIDTH)
        att_out = att @ w_att_up[l]

        merged = jax.nn.sigmoid(g_ssm) * ssm_out + jax.nn.sigmoid(g_att) * att_out
        x = x + gt1 * (merged @ w_out[l])

        h2 = _ada_norm(x, norm2_g[l], sh2, sc2)
        x = x + gt2 * _peer(h2, peer_wq[l], peer_k1[l], peer_k2[l], peer_u[l], peer_v[l])
    return x
```

```python
import math
from contextlib import ExitStack
import numpy as np
import concourse.bass as bass
import concourse.mybir as mybir
from concourse.bass_utils import run_bass_kernel_spmd

F32 = mybir.dt.float32
BF16 = mybir.dt.bfloat16
I32 = mybir.dt.int32
AF = mybir.ActivationFunctionType
ALU = mybir.AluOpType
AX = mybir.AxisListType

D = 2048
SEQ = 8192
NT = 64
OWN0 = 48
EPS = 1e-6


import types


def _freeze(fn):
    if fn is None or fn.__closure__ is None:
        return fn
    cells = []
    for c in fn.__closure__:
        try:
            cells.append(types.CellType(c.cell_contents))
        except ValueError:
            cells.append(c)
    return types.FunctionType(fn.__code__, fn.__globals__, fn.__name__, fn.__defaults__, tuple(cells))


class Prog:
    ENG = ("pe", "act", "dve", "pool", "sp")

    def __init__(self, nc, stack, same_engine_sync=False):
        self.nc = nc
        self.stack = stack
        self.same = same_engine_sync
        self.streams = {e: [] for e in self.ENG}
        self.count = {e: 0 for e in self.ENG}
        self.sem = {e: stack.enter_context(nc.semaphore("prog_" + e)) for e in self.ENG}
        self.known = {e: {} for e in self.ENG}
        self.last_w = {}
        self.readers = {}
        self.dma_keys = {}

    def _sem_of(self, key):
        if isinstance(key, str):
            return self.sem[key]
        return self.dma_keys[key[1]]["sems"][key[2]]

    def _deps(self, reads, writes):
        deps = []
        for r in reads:
            if r in self.last_w:
                deps.append(self.last_w[r])
        for w in writes:
            if w in self.last_w:
                deps.append(self.last_w[w])
            deps.extend(self.readers.get(w, ()))
        return deps

    def _waits(self, eng, deps):
        need = {}
        for (k, v) in deps:
            if k == eng and (not self.same or eng == "pe"):
                continue
            if self.known[eng].get(k, 0) >= v:
                continue
            if need.get(k, 0) < v:
                need[k] = v
        for k, v in need.items():
            self.known[eng][k] = v
        return list(need.items())

    def _commit(self, ev, reads, writes):
        for r in reads:
            self.readers.setdefault(r, []).append(ev)
        for w in writes:
            self.last_w[w] = ev
            self.readers[w] = []

    def op(self, eng, fn, reads=(), writes=()):
        waits = self._waits(eng, self._deps(reads, writes))
        self.count[eng] += 1
        ev = (eng, self.count[eng])
        self.streams[eng].append((waits, _freeze(fn), (eng, 1)))
        self._commit(ev, reads, writes)
        return ev

    def dma(self, fn, reads=(), writes=(), key="d", nsem=4, queue="sp"):
        if key not in self.dma_keys:
            sems = [self.stack.enter_context(self.nc.semaphore("dma_%s_%d" % (key, i))) for i in range(nsem)]
            self.dma_keys[key] = {"sems": sems, "n": 0}
        st = self.dma_keys[key]
        i = st["n"]
        st["n"] += 1
        R = len(st["sems"])
        evk = ("dma", key, i % R)
        ev = (evk, 16 * (i // R + 1))
        waits = self._waits(queue, self._deps(reads, writes))
        if i >= R:
            prev = (evk, 16 * (i // R))
            if self.known[queue].get(evk, 0) < prev[1]:
                self.known[queue][evk] = prev[1]
                waits.append(prev)
        self.streams[queue].append((waits, _freeze(fn), (evk, 16)))
        self._commit(ev, reads, writes)
        return ev

    def _all_events(self):
        deps = []
        for e in self.ENG:
            if self.count[e]:
                deps.append((e, self.count[e]))
        for key, st in self.dma_keys.items():
            R = len(st["sems"])
            for j in range(min(R, st["n"])):
                n_on = (st["n"] - 1 - j) // R + 1
                deps.append((("dma", key, j), 16 * n_on))
        return deps

    def barrier(self):
        deps = self._all_events()
        for e in self.ENG:
            waits = self._waits(e, [d for d in deps if d[0] != e])
            if waits:
                self.streams[e].append((waits, None, None))

    def emit(self):
        nc = self.nc
        engobj = {"pe": "tensor", "act": "scalar", "dve": "vector", "pool": "gpsimd", "sp": "sync"}
        with nc.Block() as block:
            for e in self.ENG:
                stream = self.streams[e]
                if not stream:
                    continue

                def body(engine, stream=stream):
                    for waits, fn, inc in stream:
                        for k, v in waits:
                            engine.wait_ge(self._sem_of(k), v)
                        if fn is None:
                            continue
                        ins = fn(engine)
                        ins.then_inc(self._sem_of(inc[0]), inc[1])

                getattr(block, engobj[e])(body)


class _Stop(Exception):
    pass


def build_nc(stages=99, debug=False, small_tabs=False):
    nc = bass.Bass("TRN2", target_bir_lowering=False)

    def stage_end(n):
        if stages <= n:
            raise _Stop()

    dbg_state = {}

    def dbg_dump(P, name, ap, shape, dt, rkeys):
        if not debug:
            return
        t = nc.dram_tensor("dbg_" + name, list(shape), dt, kind="ExternalOutput").ap()
        P.dma(lambda q: q.dma_start(out=t, in_=ap), reads=rkeys, writes=["dbg_" + name], key="dbg")

    def din(name, shape, dt=F32):
        return nc.dram_tensor(name, list(shape), dt, kind="ExternalInput").ap()

    def dscr(name, shape, dt):
        return nc.dram_tensor(name, list(shape), dt, kind=("ExternalOutput" if debug else "Internal")).ap()

    xw = din("xw", [SEQ, D]); valid = din("valid", [128, NT]); cb = din("cb", [128, 16])
    w_ada = din("w_ada", [D, 6 * D]); b_ada = din("b_ada", [1, 6 * D])
    norm1_g = din("norm1_g", [1, D]); norm2_g = din("norm2_g", [1, D])
    qg = din("qg", [1, 128]); kg = din("kg", [1, 128])
    w_in = din("w_in", [D, 8192]); w_glu = din("w_glu", [1024, 4096]); w_up = din("w_up", [1024, D])
    w_out = din("w_out", [D, D]); wq = din("wq", [D, D]); k1T = din("k1T", [128, 128]); k2T = din("k2T", [128, 128])
    NTAB = 128 if small_tabs else 16384
    u_tab = din("u_tab", [NTAB, D]); v_tab = din("v_tab", [NTAB, D])
    lamT_re = din("lamT_re", [128, 32]); lamT_im = din("lamT_im", [128, 32]); ldtT = din("ldtT", [128, 32])
    BreT = din("BreT", [128, 32, 128]); BimT = din("BimT", [128, 32, 128])
    Cre = din("Cre", [128, 32, 128]); Cim = din("Cim", [128, 32, 128]); dsk = din("dsk", [128, 8])
    sidx = din("sidx", [128, 128])
    out = nc.dram_tensor("out", [2048, D], F32, kind="ExternalOutput").ap()

    mod_d = dscr("mod_d", [1, 6 * D], F32)
    hT_d = dscr("hT_d", [16, 128, SEQ], BF16)
    kT_d = dscr("kT_d", [8, 128, SEQ], BF16)
    v_d = dscr("v_d", [SEQ, 1024], BF16)
    qT_d = dscr("qT_d", [8, 128, 2048], BF16)
    ygT_d = dscr("ygT_d", [8, 128, 2048], BF16)
    attT_d = dscr("attT_d", [8, 128, 2048], BF16)
    h2T_d = dscr("h2T_d", [16, 128, 2048], BF16)

    with ExitStack() as top:
        P = Prog(nc, top, same_engine_sync=True)
        try:
            tsb = lambda n, s, d=F32: top.enter_context(nc.sbuf_tensor(n, list(s), d))
            ident = tsb("ident", [128, 128]); identb = tsb("identb", [128, 128], BF16)
            cmask = tsb("cmask", [128, 128])
            ones = tsb("ones", [128, 512])
            P.op("pool", lambda e: e.memset(ident[:], 0.0), writes=["ident"])
            P.op("pool", lambda e: e.affine_select(out=ident[:], in_=ident[:], pattern=[[-1, 128]], compare_op=ALU.not_equal,
                                                   fill=1.0, base=0, channel_multiplier=1), reads=["ident"], writes=["ident"])
            P.op("pool", lambda e: e.tensor_copy(out=identb[:], in_=ident[:]), reads=["ident"], writes=["identb"])
            P.op("pool", lambda e: e.memset(ones[:], 1.0), writes=["ones"])
            P.op("pool", lambda e: e.memset(cmask[:], 1.0), writes=["cmask"])
            P.op("pool", lambda e: e.affine_select(out=cmask[:], in_=cmask[:], pattern=[[-1, 128]], compare_op=ALU.is_gt,
                                                   fill=0.0, base=0, channel_multiplier=1), reads=["cmask"], writes=["cmask"])

            psum = [top.enter_context(nc.psum_tensor("ps%d" % i, [128, 512], F32)) for i in range(6)]
            psb = [top.enter_context(nc.psum_tensor("psb%d" % i, [128, 1024], BF16)) for i in range(2)]

            def load_w_bf16(dst, src, nk, width, stg, tag, col0=0):
                for kt in range(nk):
                    s = stg[kt % len(stg)]
                    sk = ("stg", id(stg), kt % len(stg))
                    P.dma(lambda q, s=s, kt=kt: q.dma_start(out=s[:, 0:width], in_=src[kt * 128:(kt + 1) * 128, :]),
                          writes=[sk], key="wld")
                    eng = ("act", "pool")[kt % 2]
                    if eng == "act":
                        P.op("act", lambda e, s=s, kt=kt: e.activation(out=dst[:, kt, col0:col0 + width], in_=s[:, 0:width], func=AF.Copy),
                             reads=[sk], writes=[(tag, kt)])
                    else:
                        P.op("pool", lambda e, s=s, kt=kt: e.tensor_copy(out=dst[:, kt, col0:col0 + width], in_=s[:, 0:width]),
                             reads=[sk], writes=[(tag, kt)])

            with ExitStack() as ph:
                sb = lambda n, s, d=F32: ph.enter_context(nc.sbuf_tensor(n, list(s), d))
                cbt = sb("cbt", [128, 16]); csl = sb("csl", [128, 16]); brow = sb("brow", [1, 6 * D]); mrow = sb("mrow", [1, 6 * D])
                wblk = [sb("wblk%d" % i, [128, 16, 512]) for i in range(2)]
                P.dma(lambda q: q.dma_start(out=cbt[:], in_=cb), writes=["cbt"], key="ld")
                P.dma(lambda q: q.dma_start(out=brow[:], in_=b_ada), writes=["brow"], key="ld")
                P.op("act", lambda e: e.activation(out=csl[:], in_=cbt[:], func=AF.Silu), reads=["cbt"], writes=["csl"])
                for nb in range(24):
                    wb = wblk[nb % 2]
                    P.dma(lambda q, wb=wb, nb=nb: q.dma_start(out=wb[:], in_=w_ada[:, nb * 512:(nb + 1) * 512].rearrange("(kt p) n -> p kt n", p=128)),
                          writes=[("wblk", nb % 2)], key="ld")
                    for kt in range(16):
                        P.op("pe", lambda e, wb=wb, kt=kt: e.matmul(psum[0][0:1, :], lhsT=csl[:, kt:kt + 1], rhs=wb[:, kt, :], start=(kt == 0), stop=(kt == 15)),
                             reads=["csl", ("wblk", nb % 2)], writes=["ps0"])
                    P.op("dve", lambda e, nb=nb: e.tensor_tensor(out=mrow[0:1, nb * 512:(nb + 1) * 512], in0=psum[0][0:1, :],
                                                                 in1=brow[0:1, nb * 512:(nb + 1) * 512], op=ALU.add),
                         reads=["ps0", "brow"], writes=["mrow"])
                P.dma(lambda q: q.dma_start(out=mod_d, in_=mrow[:]), reads=["mrow"], writes=["mod_d"], key="st")
            P.barrier()
            stage_end(1)

            def bc_row(dst, row_ap, n, wkey):
                P.dma(lambda q: q.dma_start(out=dst, in_=row_ap.broadcast_to([128, n])), reads=["mod_d"], writes=[wkey], key="ld")

            def rms_stats(xt, xkey, junk, ss, rstd, tagsfx):
                P.op("act", lambda e: e.activation(out=junk, in_=xt, func=AF.Square, accum_out=ss), reads=[xkey], writes=["junk" + tagsfx, "ss" + tagsfx])
                P.op("dve", lambda e: e.tensor_scalar(out=ss, in0=ss, scalar1=1.0 / D, scalar2=EPS, op0=ALU.mult, op1=ALU.add),
                     reads=["ss" + tagsfx], writes=["ss" + tagsfx])
                P.op("act", lambda e: e.activation(out=ss, in_=ss, func=AF.Sqrt), reads=["ss" + tagsfx], writes=["ss" + tagsfx])
                P.op("dve", lambda e: e.reciprocal(out=rstd, in_=ss), reads=["ss" + tagsfx], writes=["rstd" + tagsfx])

            with ExitStack() as ph:
                sb = lambda n, s, d=F32: ph.enter_context(nc.sbuf_tensor(n, list(s), d))
                gs1 = sb("gs1", [128, D]); sh1 = sb("sh1", [128, D]); g1 = sb("g1", [128, D]); vld = sb("vld", [128, NT])
                xt = [sb("xt%d" % i, [128, D]) for i in range(2)]
                junk = sb("junk", [128, D]); tA = sb("tA", [128, D]); tB = sb("tB", [128, D])
                hb = [sb("hb%d" % i, [128, D], BF16) for i in range(2)]
                hTt = [sb("hTt%d" % i, [128, 16, 512], BF16) for i in range(2)]
                ss = sb("ss", [128, 1]); rstd = sb("rstd", [128, 1])
                bc_row(sh1[:], mod_d[0:1, 0:D], D, "sh1")
                bc_row(gs1[:], mod_d[0:1, D:2 * D], D, "gs1")
                P.dma(lambda q: q.dma_start(out=g1[:], in_=norm1_g.broadcast_to([128, D])), writes=["g1"], key="ld")
                P.dma(lambda q: q.dma_start(out=vld[:], in_=valid), writes=["vld"], key="ld")
                P.op("dve", lambda e: e.scalar_tensor_tensor(out=gs1[:], in0=gs1[:], scalar=1.0, in1=g1[:], op0=ALU.add, op1=ALU.mult),
                     reads=["gs1", "g1"], writes=["gs1"])
                for c in range(NT):
                    x_ = xt[c % 2]; xk = ("xt", c % 2); g = c // 4; hT_ = hTt[g % 2]; hb_ = hb[c % 2]
                    P.dma(lambda q, x_=x_, c=c: q.dma_start(out=x_[:], in_=xw[c * 128:(c + 1) * 128, :]), writes=[xk], key="xld")
                    rms_stats(x_[:], xk, junk[:], ss[:], rstd[:], "A")
                    P.op("dve", lambda e, x_=x_: e.scalar_tensor_tensor(out=tA[:], in0=x_[:], scalar=rstd[:, 0:1], in1=gs1[:], op0=ALU.mult, op1=ALU.mult),
                         reads=[xk, "rstdA", "gs1"], writes=["tA"])
                    P.op("pool", lambda e: e.tensor_tensor(out=tB[:], in0=tA[:], in1=sh1[:], op=ALU.add), reads=["tA", "sh1"], writes=["tB"])
                    P.op("pool", lambda e, hb_=hb_, c=c: e.tensor_scalar(out=hb_[:], in0=tB[:], scalar1=vld[:, c:c + 1], scalar2=None, op0=ALU.mult),
                         reads=["tB", "vld"], writes=[("hb", c % 2)])
                    for half in range(2):
                        pb = psb[half]
                        for k8 in range(8):
                            kt = half * 8 + k8
                            P.op("pe", lambda e, pb=pb, k8=k8, kt=kt, hb_=hb_: e.transpose(pb[:, k8 * 128:(k8 + 1) * 128], in_=hb_[:, kt * 128:(kt + 1) * 128], identity=identb[:]),
                                 reads=[("hb", c % 2), "identb"], writes=["psb%d" % half])
                        eng = ("dve", "act")[half]
                        dst = hT_[:, half * 8:(half + 1) * 8, (c % 4) * 128:(c % 4 + 1) * 128]
                        src = pb[:].rearrange("p (k t) -> p k t", k=8)
                        if eng == "dve":
                            P.op("dve", lambda e, dst=dst, src=src: e.tensor_copy(out=dst, in_=src), reads=["psb%d" % half], writes=[("hTt", g % 2)])
                        else:
                            P.op("act", lambda e, dst=dst, src=src: e.activation(out=dst, in_=src, func=AF.Copy), reads=["psb%d" % half], writes=[("hTt", g % 2)])
                    if c % 4 == 3:
                        P.dma(lambda q, hT_=hT_, g=g: q.dma_start(out=hT_d[:, :, g * 512:(g + 1) * 512].rearrange("k p t -> p k t"), in_=hT_[:]),
                              reads=[("hTt", g % 2)], writes=["hT_d"], key="st")
            P.barrier()
            stage_end(2)

            with ExitStack() as ph:
                sb = lambda n, s, d=F32: ph.enter_context(nc.sbuf_tensor(n, list(s), d))
                Pr = sb("Pr", [128, 32, 128], BF16); Pi = sb("Pi", [128, 32, 128], BF16); Qr = sb("Qr", [128, 32, 128], BF16); Qi = sb("Qi", [128, 32, 128], BF16)
                lbr = sb("lbr", [128, 32]); lbi = sb("lbi", [128, 32]); cr = sb("cr", [128, 32]); ci = sb("ci", [128, 32])
                wi_re = sb("wi_re", [128, 32]); wi_im = sb("wi_im", [128, 32])
                Bre = sb("Bre", [128, 32, 128], BF16); Bim = sb("Bim", [128, 32, 128], BF16); dskt = sb("dskt", [128, 8])
                with ExitStack() as ph2:
                    sb2 = lambda n, s, d=F32: ph2.enter_context(nc.sbuf_tensor(n, list(s), d))
                    lr = sb2("lr", [128, 32]); li = sb2("li", [128, 32]); dt = sb2("dt", [128, 32]); a_ = sb2("a_", [128, 32]); om = sb2("om", [128, 32])
                    sidt = sb2("sidt", [128, 128])
                    T1 = sb2("T1", [128, 32, 128]); T2 = sb2("T2", [128, 32, 128]); T3 = sb2("T3", [128, 32, 128]); T4 = sb2("T4", [128, 32, 128])
                    TI = sb2("TI", [128, 32, 128], I32); T5 = sb2("T5", [128, 32, 128]); T6 = sb2("T6", [128, 32, 128])
                    s_a = sb2("s_a", [128, 32]); s_b = sb2("s_b", [128, 32]); s_c = sb2("s_c", [128, 32]); s_d = sb2("s_d", [128, 32])
                    s_i = sb2("s_i", [128, 32], I32); sn = sb2("sn", [128, 32]); cs = sb2("cs", [128, 32]); ea = sb2("ea", [128, 32])
                    for (t_, s_, k_) in [(lr[:], lamT_re, "lr"), (li[:], lamT_im, "li"), (dt[:], ldtT, "dt"), (sidt[:], sidx, "sidt"), (T1[:], BreT, "BreF"),
                                         (T2[:], BimT, "BimF"), (dskt[:], dsk, "dskt")]:
                        P.dma(lambda q, t_=t_, s_=s_: q.dma_start(out=t_, in_=s_), writes=[k_], key="ld")
                    P.op("dve", lambda e: e.tensor_copy(out=Bre[:], in_=T1[:]), reads=["BreF"], writes=["Bre"])
                    P.op("dve", lambda e: e.tensor_copy(out=Bim[:], in_=T2[:]), reads=["BimF"], writes=["Bim"])
                    P.barrier()
                    P.op("act", lambda e: e.activation(out=dt[:], in_=dt[:], func=AF.Exp), reads=["dt"], writes=["dt"])
                    P.op("dve", lambda e: e.tensor_tensor(out=a_[:], in0=lr[:], in1=dt[:], op=ALU.mult), reads=["lr", "dt"], writes=["a_"])
                    P.op("dve", lambda e: e.tensor_tensor(out=om[:], in0=li[:], in1=dt[:], op=ALU.mult), reads=["li", "dt"], writes=["om"])

                    def sincos(th, n_shape, u_, ui_, f_, s2_, c2_, sin_o, cos_o, tg):
                        P.op("dve", lambda e: e.tensor_scalar(out=u_, in0=th, scalar1=1.0 / (2 * math.pi), scalar2=None, op0=ALU.mult), reads=[tg + "th"], writes=[tg + "u"])
                        P.op("dve", lambda e: e.tensor_copy(out=ui_, in_=u_), reads=[tg + "u"], writes=[tg + "ui"])
                        P.op("dve", lambda e: e.tensor_copy(out=f_, in_=ui_), reads=[tg + "ui"], writes=[tg + "f"])
                        P.op("dve", lambda e: e.tensor_tensor(out=f_, in0=u_, in1=f_, op=ALU.subtract), reads=[tg + "u", tg + "f"], writes=[tg + "f"])
                        P.op("act", lambda e: e.activation(out=s2_, in_=f_, func=AF.Sin, scale=math.pi), reads=[tg + "f"], writes=[tg + "s2"])
                        P.op("act", lambda e: e.activation(out=f_, in_=f_, func=AF.Abs), reads=[tg + "f", tg + "s2"], writes=[tg + "f"])
                        P.op("dve", lambda e: e.tensor_scalar(out=f_, in0=f_, scalar1=-math.pi, scalar2=math.pi / 2, op0=ALU.mult, op1=ALU.add), reads=[tg + "f"], writes=[tg + "f"])
                        P.op("act", lambda e: e.activation(out=c2_, in_=f_, func=AF.Sin), reads=[tg + "f"], writes=[tg + "c2"])
                        P.op("dve", lambda e: e.scalar_tensor_tensor(out=sin_o, in0=s2_, scalar=2.0, in1=c2_, op0=ALU.mult, op1=ALU.mult),
                             reads=[tg + "s2", tg + "c2"], writes=[tg + "sin"])
                        P.op("dve", lambda e: e.tensor_tensor(out=c2_, in0=c2_, in1=c2_, op=ALU.mult), reads=[tg + "c2"], writes=[tg + "c2"])
                        P.op("dve", lambda e: e.tensor_tensor(out=s2_, in0=s2_, in1=s2_, op=ALU.mult), reads=[tg + "s2"], writes=[tg + "s2"])
                        P.op("dve", lambda e: e.tensor_tensor(out=cos_o, in0=c2_, in1=s2_, op=ALU.subtract), reads=[tg + "c2", tg + "s2"], writes=[tg + "cos"])

                    P.op("dve", lambda e: e.tensor_copy(out=s_a[:], in_=om[:]), reads=["om"], writes=["bth"])
                    sincos(s_a[:], None, s_b[:], s_i[:], s_c[:], s_d[:], cs[:], sn[:], cs[:], "b")
                    P.op("act", lambda e: e.activation(out=ea[:], in_=a_[:], func=AF.Exp), reads=["a_"], writes=["ea"])
                    P.op("dve", lambda e: e.tensor_tensor(out=lbr[:], in0=ea[:], in1=cs[:], op=ALU.mult), reads=["ea", "bcos"], writes=["lbr"])
                    P.op("dve", lambda e: e.tensor_tensor(out=lbi[:], in0=ea[:], in1=sn[:], op=ALU.mult), reads=["ea", "bsin"], writes=["lbi"])
                    P.op("dve", lambda e: e.tensor_scalar(out=s_a[:], in0=lbr[:], scalar1=-1.0, scalar2=None, op0=ALU.add), reads=["lbr"], writes=["s_a"])
                    P.op("dve", lambda e: e.tensor_tensor(out=s_b[:], in0=lr[:], in1=lr[:], op=ALU.mult), reads=["lr"], writes=["s_b"])
                    P.op("dve", lambda e: e.tensor_tensor(out=s_c[:], in0=li[:], in1=li[:], op=ALU.mult), reads=["li"], writes=["s_c"])
                    P.op("dve", lambda e: e.tensor_tensor(out=s_b[:], in0=s_b[:], in1=s_c[:], op=ALU.add), reads=["s_b", "s_c"], writes=["s_b"])
                    P.op("dve", lambda e: e.reciprocal(out=s_b[:], in_=s_b[:]), reads=["s_b"], writes=["s_b"])
                    P.op("dve", lambda e: e.tensor_tensor(out=s_c[:], in0=s_a[:], in1=lr[:], op=ALU.mult), reads=["s_a", "lr"], writes=["s_c"])
                    P.op("dve", lambda e: e.tensor_tensor(out=s_d[:], in0=lbi[:], in1=li[:], op=ALU.mult), reads=["lbi", "li"], writes=["s_d"])
                    P.op("dve", lambda e: e.tensor_tensor(out=s_c[:], in0=s_c[:], in1=s_d[:], op=ALU.add), reads=["s_c", "s_d"], writes=["s_c"])
                    P.op("dve", lambda e: e.tensor_tensor(out=cr[:], in0=s_c[:], in1=s_b[:], op=ALU.mult), reads=["s_c", "s_b"], writes=["cr"])
                    P.op("dve", lambda e: e.tensor_tensor(out=s_c[:], in0=lbi[:], in1=lr[:], op=ALU.mult), reads=["lbi", "lr"], writes=["s_c"])
                    P.op("dve", lambda e: e.tensor_tensor(out=s_d[:], in0=s_a[:], in1=li[:], op=ALU.mult), reads=["s_a", "li"], writes=["s_d"])
                    P.op("dve", lambda e: e.tensor_tensor(out=s_c[:], in0=s_c[:], in1=s_d[:], op=ALU.subtract), reads=["s_c", "s_d"], writes=["s_c"])
                    P.op("dve", lambda e: e.tensor_tensor(out=ci[:], in0=s_c[:], in1=s_b[:], op=ALU.mult), reads=["s_c", "s_b"], writes=["ci"])
                    om_b = om[:].unsqueeze(2).broadcast_to([128, 32, 128]); a_b = a_[:].unsqueeze(2).broadcast_to([128, 32, 128])
                    s_bb = sidt[:].unsqueeze(1).broadcast_to([128, 32, 128])
                    P.op("dve", lambda e: e.tensor_tensor(out=T1[:], in0=om_b, in1=s_bb, op=ALU.mult), reads=["om", "sidt"], writes=["tth"])
                    sincos(T1[:], None, T2[:], TI[:], T3[:], T4[:], T5[:], T6[:], T5[:], "t")
                    P.barrier()
                    P.op("dve", lambda e: e.tensor_tensor(out=T1[:], in0=a_b, in1=s_bb, op=ALU.mult), reads=["a_", "sidt"], writes=["T1as"])
                    P.op("act", lambda e: e.activation(out=T2[:], in_=T1[:], func=AF.Exp), reads=["T1as"], writes=["Ep"])
                    P.op("act", lambda e: e.activation(out=T3[:], in_=T1[:], func=AF.Exp, scale=-1.0), reads=["T1as"], writes=["Em"])
                    P.op("dve", lambda e: e.tensor_tensor(out=Qr[:], in0=T2[:], in1=T5[:], op=ALU.mult), reads=["Ep"], writes=["Qr"])
                    P.op("dve", lambda e: e.tensor_tensor(out=Qi[:], in0=T2[:], in1=T6[:], op=ALU.mult), reads=["Ep"], writes=["Qi"])
                    P.op("dve", lambda e: e.tensor_tensor(out=Pr[:], in0=T3[:], in1=T5[:], op=ALU.mult), reads=["Em"], writes=["Pr"])
                    P.op("dve", lambda e: e.scalar_tensor_tensor(out=Pi[:], in0=T3[:], scalar=-1.0, in1=T6[:], op0=ALU.mult, op1=ALU.mult),
                         reads=["Em"], writes=["Pi"])
                    P.barrier()
                    for nm_, t_ in [("lbr", lbr), ("lbi", lbi), ("cr", cr), ("ci", ci), ("om", om), ("a_", a_), ("sn", sn), ("cs", cs)]:
                        dbg_dump(P, nm_, t_[:], [128, 32], F32, [])
                    for nm_, t_ in [("Pr", Pr), ("Pi", Pi), ("Qr", Qr), ("Qi", Qi)]:
                        dbg_dump(P, nm_, t_[:], [128, 32, 128], BF16, [])
                    for nm_, t_ in [("T5", T5), ("T6", T6), ("T2", T2), ("T3", T3)]:
                        dbg_dump(P, nm_, t_[:], [128, 32, 128], F32, [])
                    P.barrier()
                Ccr = sb("Ccr", [128, 32, 128]); Cci = sb("Cci", [128, 32, 128])
                with ExitStack() as ph2:
                    sb2 = lambda n, s, d=F32: ph2.enter_context(nc.sbuf_tensor(n, list(s), d))
                    Cr_ = sb2("Cr_", [128, 32, 128]); Ci_ = sb2("Ci_", [128, 32, 128]); Tm = sb2("Tm", [128, 32, 128])
                    P.dma(lambda q: q.dma_start(out=Cr_[:], in_=Cre), writes=["Cr_"], key="ld")
                    P.dma(lambda q: q.dma_start(out=Ci_[:], in_=Cim), writes=["Ci_"], key="ld")
                    cr_b = cr[:].unsqueeze(2).broadcast_to([128, 32, 128]); ci_b = ci[:].unsqueeze(2).broadcast_to([128, 32, 128])
                    P.op("dve", lambda e: e.tensor_tensor(out=Ccr[:], in0=Cr_[:], in1=cr_b, op=ALU.mult), reads=["Cr_", "cr"], writes=["Ccr"])
                    P.op("dve", lambda e: e.tensor_tensor(out=Tm[:], in0=Ci_[:], in1=ci_b, op=ALU.mult), reads=["Ci_", "ci"], writes=["Tm"])
                    P.op("dve", lambda e: e.tensor_tensor(out=Ccr[:], in0=Ccr[:], in1=Tm[:], op=ALU.subtract), reads=["Ccr", "Tm"], writes=["Ccr"])
                    P.op("dve", lambda e: e.tensor_tensor(out=Cci[:], in0=Cr_[:], in1=ci_b, op=ALU.mult), reads=["Cr_", "ci"], writes=["Cci"])
                    P.op("dve", lambda e: e.tensor_tensor(out=Tm[:], in0=Ci_[:], in1=cr_b, op=ALU.mult), reads=["Ci_", "cr", "Ccr"], writes=["Tm"])
                    P.op("dve", lambda e: e.scalar_tensor_tensor(out=Cci[:], in0=Cci[:], scalar=-1.0, in1=Tm[:], op0=ALU.mult, op1=ALU.subtract),
                         reads=["Cci", "Tm"], writes=["Cci"])
                    P.barrier()
                wu = sb("wu", [128, 16, 1024], BF16)
                stg = [sb("stg%d" % i, [128, 1024]) for i in range(2)]
                load_w_bf16(wu, w_in[:, 0:1024], 16, 1024, stg, "wu")
                hTg = [sb("hTg%d" % i, [128, 16, 512], BF16) for i in range(2)]
                uT = [sb("uT%d" % i, [128, 8, 512], BF16) for i in range(2)]
                v_re = sb("v_re", [128, 4, 128]); v_im = sb("v_im", [128, 4, 128]); t1 = sb("t1", [128, 4, 128]); t2 = sb("t2", [128, 4, 128])
                w_re = sb("w_re", [128, 4, 128]); w_im = sb("w_im", [128, 4, 128])
                x_re = [sb("x_re%d" % i, [128, 4, 128]) for i in range(2)]; x_im = [sb("x_im%d" % i, [128, 4, 128]) for i in range(2)]
                yv = sb("yv", [128, 128]); ygT = sb("ygT", [128, 8, 128], BF16); c1 = sb("c1", [128, 4]); c2 = sb("c2", [128, 4])
                P.op("pool", lambda e: e.memset(wi_re[:], 0.0), writes=["wi_re"])
                P.op("pool", lambda e: e.memset(wi_im[:], 0.0), writes=["wi_im"])
                it = 0
                for g in range(16):
                    hT_ = hTg[g % 2]; uT_ = uT[g % 2]
                    P.dma(lambda q, hT_=hT_, g=g: q.dma_start(out=hT_[:], in_=hT_d[:, :, g * 512:(g + 1) * 512].rearrange("k p t -> p k t")),
                          reads=["hT_d"], writes=[("hTg", g % 2)], key="hld")
                    for fc in range(8):
                        pu = psum[fc % 2]
                        for kt in range(16):
                            P.op("pe", lambda e, pu=pu, kt=kt, fc=fc, hT_=hT_: e.matmul(pu[:], lhsT=wu[:, kt, fc * 128:(fc + 1) * 128], rhs=hT_[:, kt, :], start=(kt == 0), stop=(kt == 15)),
                                 reads=[("wu", kt), ("hTg", g % 2)], writes=["ps%d" % (fc % 2)])
                        P.op("act", lambda e, pu=pu, fc=fc, uT_=uT_: e.activation(out=uT_[:, fc, :], in_=pu[:], func=AF.Copy), reads=["ps%d" % (fc % 2)], writes=[("uT", g % 2, fc)])
                    for cc in range(4):
                        c = g * 4 + cc
                        tok = slice(cc * 128, (cc + 1) * 128)
                        for fc in range(8):
                            xr = x_re[it % 2]; xi = x_im[it % 2]; xk = ("x", it % 2); it += 1
                            pbr = psum[2]; pbi = psum[3]
                            for k4 in range(4):
                                st_ = fc * 4 + k4
                                P.op("pe", lambda e, k4=k4, st_=st_, fc=fc, uT_=uT_, tok=tok: e.matmul(pbr[:, k4 * 128:(k4 + 1) * 128], lhsT=Bre[:, st_, :], rhs=uT_[:, fc, tok], start=True, stop=True),
                                     reads=["Bre", ("uT", g % 2, fc)], writes=["ps2"])
                                P.op("pe", lambda e, k4=k4, st_=st_, fc=fc, uT_=uT_, tok=tok: e.matmul(pbi[:, k4 * 128:(k4 + 1) * 128], lhsT=Bim[:, st_, :], rhs=uT_[:, fc, tok], start=True, stop=True),
                                     reads=["Bim", ("uT", g % 2, fc)], writes=["ps3"])
                            st0 = fc * 4
                            pr_ = Pr[:, st0:st0 + 4, :]; pi_ = Pi[:, st0:st0 + 4, :]; qr_ = Qr[:, st0:st0 + 4, :]; qi_ = Qi[:, st0:st0 + 4, :]
                            br4 = pbr[:].rearrange("p (k t) -> p k t", k=4); bi4 = pbi[:].rearrange("p (k t) -> p k t", k=4)
                            P.op("dve", lambda e, br4=br4, pr_=pr_: e.tensor_tensor(out=t1[:], in0=br4, in1=pr_, op=ALU.mult), reads=["ps2", "Pr"], writes=["t1"])
                            P.op("dve", lambda e, bi4=bi4, pi_=pi_: e.tensor_tensor(out=t2[:], in0=bi4, in1=pi_, op=ALU.mult), reads=["ps3", "Pi"], writes=["t2"])
                            P.op("pool", lambda e: e.tensor_tensor(out=v_re[:], in0=t1[:], in1=t2[:], op=ALU.subtract), reads=["t1", "t2"], writes=["v_re"])
                            P.op("dve", lambda e, br4=br4, pi_=pi_: e.tensor_tensor(out=t1[:], in0=br4, in1=pi_, op=ALU.mult), reads=["ps2", "Pi"], writes=["t1"])
                            P.op("dve", lambda e, bi4=bi4, pr_=pr_: e.tensor_tensor(out=t2[:], in0=bi4, in1=pr_, op=ALU.mult), reads=["ps3", "Pr"], writes=["t2"])
                            P.op("pool", lambda e: e.tensor_tensor(out=v_im[:], in0=t1[:], in1=t2[:], op=ALU.add), reads=["t1", "t2"], writes=["v_im"])
                            for k4 in range(4):
                                st_ = st0 + k4
                                P.op("dve", lambda e, k4=k4, st_=st_: e.tensor_tensor_scan(out=w_re[:, k4, :], data0=ones[:, 0:128], data1=v_re[:, k4, :], initial=wi_re[:, st_:st_ + 1], op0=ALU.mult, op1=ALU.add),
                                     reads=["v_re", "ones", "wi_re"], writes=["w_re"])
                                P.op("dve", lambda e, k4=k4, st_=st_: e.tensor_tensor_scan(out=w_im[:, k4, :], data0=ones[:, 0:128], data1=v_im[:, k4, :], initial=wi_im[:, st_:st_ + 1], op0=ALU.mult, op1=ALU.add),
                                     reads=["v_im", "ones", "wi_im"], writes=["w_im"])
                            P.op("pool", lambda e, qr_=qr_: e.tensor_tensor(out=t1[:], in0=w_re[:], in1=qr_, op=ALU.mult), reads=["w_re", "Qr"], writes=["t1"])
                            P.op("pool", lambda e, qi_=qi_: e.tensor_tensor(out=t2[:], in0=w_im[:], in1=qi_, op=ALU.mult), reads=["w_im", "Qi"], writes=["t2"])
                            P.op("pool", lambda e, xr=xr: e.tensor_tensor(out=xr[:], in0=t1[:], in1=t2[:], op=ALU.subtract), reads=["t1", "t2"], writes=[xk])
                            P.op("dve", lambda e, qi_=qi_: e.tensor_tensor(out=t1[:], in0=w_re[:], in1=qi_, op=ALU.mult), reads=["w_re", "Qi"], writes=["t1"])
                            P.op("dve", lambda e, qr_=qr_: e.tensor_tensor(out=t2[:], in0=w_im[:], in1=qr_, op=ALU.mult), reads=["w_im", "Qr"], writes=["t2"])
                            P.op("pool", lambda e, xi=xi: e.tensor_tensor(out=xi[:], in0=t1[:], in1=t2[:], op=ALU.add), reads=["t1", "t2"], writes=[xk])
                            xr_l = xr[:, :, 127]; xi_l = xi[:, :, 127]
                            lr_s = lbr[:, st0:st0 + 4]; li_s = lbi[:, st0:st0 + 4]
                            P.op("dve", lambda e, xr_l=xr_l, lr_s=lr_s: e.tensor_tensor(out=c1[:], in0=xr_l, in1=lr_s, op=ALU.mult), reads=[xk, "lbr"], writes=["c1"])
                            P.op("dve", lambda e, xi_l=xi_l, li_s=li_s: e.tensor_tensor(out=c2[:], in0=xi_l, in1=li_s, op=ALU.mult), reads=[xk, "lbi"], writes=["c2"])
                            P.op("dve", lambda e, st0=st0: e.tensor_tensor(out=wi_re[:, st0:st0 + 4], in0=c1[:], in1=c2[:], op=ALU.subtract), reads=["c1", "c2"], writes=["wi_re"])
                            P.op("dve", lambda e, xr_l=xr_l, li_s=li_s: e.tensor_tensor(out=c1[:], in0=xr_l, in1=li_s, op=ALU.mult), reads=[xk, "lbi"], writes=["c1"])
                            P.op("dve", lambda e, xi_l=xi_l, lr_s=lr_s: e.tensor_tensor(out=c2[:], in0=xi_l, in1=lr_s, op=ALU.mult), reads=[xk, "lbr"], writes=["c2"])
                            P.op("dve", lambda e, st0=st0: e.tensor_tensor(out=wi_im[:, st0:st0 + 4], in0=c1[:], in1=c2[:], op=ALU.add), reads=["c1", "c2"], writes=["wi_im"])
                            if c >= OWN0:
                                py = psum[4]
                                for k4 in range(4):
                                    st_ = st0 + k4
                                    P.op("pe", lambda e, k4=k4, st_=st_, xr=xr: e.matmul(py[:, 0:128], lhsT=Ccr[:, st_, :], rhs=xr[:, k4, :], start=(k4 == 0), stop=False),
                                         reads=["Ccr", xk], writes=["ps4"])
                                    P.op("pe", lambda e, k4=k4, st_=st_, xi=xi: e.matmul(py[:, 0:128], lhsT=Cci[:, st_, :], rhs=xi[:, k4, :], start=False, stop=(k4 == 3)),
                                         reads=["Cci", xk], writes=["ps4"])
                                P.op("dve", lambda e, fc=fc, uT_=uT_, tok=tok: e.scalar_tensor_tensor(out=yv[:], in0=uT_[:, fc, tok], scalar=dskt[:, fc:fc + 1], in1=py[:, 0:128], op0=ALU.mult, op1=ALU.add),
                                     reads=[("uT", g % 2, fc), "dskt", "ps4"], writes=["yv"])
                                P.op("act", lambda e, fc=fc: e.activation(out=ygT[:, fc, :], in_=yv[:], func=AF.Gelu_apprx_tanh), reads=["yv"], writes=["ygT"])
                        if c >= OWN0:
                            o0 = (c - OWN0) * 128
                            P.dma(lambda q, o0=o0: q.dma_start(out=ygT_d[:, :, o0:o0 + 128].rearrange("k p t -> p k t"), in_=ygT[:]),
                                  reads=["ygT"], writes=["ygT_d"], key="st")
            P.barrier()
            stage_end(3)

            with ExitStack() as ph:
                sb = lambda n, s, d=F32: ph.enter_context(nc.sbuf_tensor(n, list(s), d))
                wk = sb("wk", [128, 16, 1024], BF16)
                stg = [sb("stgk%d" % i, [128, 1024]) for i in range(2)]
                hTg = [sb("hTk%d" % i, [128, 16, 512], BF16) for i in range(2)]
                gbc = sb("gbc", [128, 128]); junk = sb("junkk", [128, 128]); ssk = sb("ssk", [128, 8]); rsk = sb("rsk", [128, 8])
                kn = sb("kn", [128, 1024], BF16); kTt = [sb("kTt%d" % i, [128, 8, 512], BF16) for i in range(2)]
                vsb = [sb("vsb%d" % i, [128, 1024], BF16) for i in range(2)]
                for pas in ("k", "v", "q"):
                    col0 = {"k": 2048, "v": 3072, "q": 1024}[pas]
                    P.barrier()
                    load_w_bf16(wk, w_in[:, col0:col0 + 1024], 16, 1024, stg, "wk")
                    if pas in ("k", "q"):
                        gsrc = kg if pas == "k" else qg
                        P.dma(lambda q, gsrc=gsrc: q.dma_start(out=gbc[:], in_=gsrc.broadcast_to([128, 128])), writes=["gbc"], key="ld")
                        if pas == "q":
                            P.op("dve", lambda e: e.tensor_scalar(out=gbc[:], in0=gbc[:], scalar1=128.0 ** -0.5, scalar2=None, op0=ALU.mult), reads=["gbc"], writes=["gbc"])
                    g_lo = 12 if pas == "q" else 0
                    for g in range(g_lo, 16):
                        hT_ = hTg[g % 2]
                        P.dma(lambda q, hT_=hT_, g=g: q.dma_start(out=hT_[:], in_=hT_d[:, :, g * 512:(g + 1) * 512].rearrange("k p t -> p k t")),
                              reads=["hT_d"], writes=[("hTk", g % 2)], key="hld")
                        for cc in range(4):
                            c = g * 4 + cc
                            for nb in range(2):
                                pp = psum[nb]
                                for kt in range(16):
                                    P.op("pe", lambda e, pp=pp, kt=kt, nb=nb, hT_=hT_, cc=cc: e.matmul(pp[:], lhsT=hT_[:, kt, cc * 128:(cc + 1) * 128], rhs=wk[:, kt, nb * 512:(nb + 1) * 512], start=(kt == 0), stop=(kt == 15)),
                                         reads=[("hTk", g % 2), ("wk", kt)], writes=["ps%d" % nb])
                            if pas == "v":
                                v_ = vsb[c % 2]
                                P.op("act", lambda e, v_=v_: e.activation(out=v_[:, 0:512], in_=psum[0][:], func=AF.Copy), reads=["ps0"], writes=[("vsb", c % 2)])
                                P.op("dve", lambda e, v_=v_: e.tensor_copy(out=v_[:, 512:1024], in_=psum[1][:]), reads=["ps1"], writes=[("vsb", c % 2)])
                                P.dma(lambda q, v_=v_, c=c: q.dma_start(out=v_d[c * 128:(c + 1) * 128, :], in_=v_[:]), reads=[("vsb", c % 2)], writes=["v_d"], key="st")
                                continue
                            for h in range(8):
                                pp = psum[h // 4]; hs = slice((h % 4) * 128, (h % 4 + 1) * 128)
                                P.op("act", lambda e, pp=pp, hs=hs, h=h: e.activation(out=junk[:], in_=pp[:, hs], func=AF.Square, accum_out=ssk[:, h:h + 1]),
                                     reads=["ps%d" % (h // 4)], writes=["junkk", "ssk"])
                            P.op("dve", lambda e: e.tensor_scalar(out=ssk[:], in0=ssk[:], scalar1=1.0 / 128, scalar2=EPS, op0=ALU.mult, op1=ALU.add), reads=["ssk"], writes=["ssk"])
                            P.op("act", lambda e: e.activation(out=ssk[:], in_=ssk[:], func=AF.Sqrt), reads=["ssk"], writes=["ssk"])
                            P.op("dve", lambda e: e.reciprocal(out=rsk[:], in_=ssk[:]), reads=["ssk"], writes=["rsk"])
                            for h in range(8):
                                pp = psum[h // 4]; hs = slice((h % 4) * 128, (h % 4 + 1) * 128)
                                P.op("dve", lambda e, pp=pp, hs=hs, h=h: e.scalar_tensor_tensor(out=kn[:, h * 128:(h + 1) * 128], in0=pp[:, hs], scalar=rsk[:, h:h + 1], in1=gbc[:], op0=ALU.mult, op1=ALU.mult),
                                     reads=["ps%d" % (h // 4), "rsk", "gbc"], writes=["kn"])
                            for h in range(8):
                                P.op("pe", lambda e, h=h: e.transpose(psb[0][:, h * 128:(h + 1) * 128], in_=kn[:, h * 128:(h + 1) * 128], identity=identb[:]),
                                     reads=["kn", "identb"], writes=["psb0"])
                            kT_ = kTt[g % 2]
                            P.op("act", lambda e, kT_=kT_, cc=cc: e.activation(out=kT_[:, :, cc * 128:(cc + 1) * 128], in_=psb[0][:].rearrange("p (h t) -> p h t", h=8), func=AF.Copy),
                                 reads=["psb0"], writes=[("kTt", g % 2)])
                        if pas == "k":
                            P.dma(lambda q, g=g: q.dma_start(out=kT_d[:, :, g * 512:(g + 1) * 512].rearrange("h p t -> p h t"), in_=kTt[g % 2][:]),
                                  reads=[("kTt", g % 2)], writes=["kT_d"], key="st")
                        elif pas == "q":
                            o0 = (g - 12) * 512
                            P.dma(lambda q, g=g, o0=o0: q.dma_start(out=qT_d[:, :, o0:o0 + 512].rearrange("h p t -> p h t"), in_=kTt[g % 2][:]),
                                  reads=[("kTt", g % 2)], writes=["qT_d"], key="st")
            P.barrier()
            stage_end(4)

            with ExitStack() as ph:
                sb = lambda n, s, d=F32: ph.enter_context(nc.sbuf_tensor(n, list(s), d))
                kTh = [sb("kTh%d" % i, [128, SEQ], BF16) for i in range(2)]
                vh = [sb("vh%d" % i, [128, NT, 128], BF16) for i in range(2)]
                qTh = [sb("qTh%d" % i, [128, 2048], BF16) for i in range(2)]
                lw0 = sb("lw0", [128, SEQ]); ee = sb("ee", [128, 512]); sp = sb("sp", [128, 512]); pref = sb("pref", [128, 512]); zs = sb("zs", [128, 512])
                wb = [sb("wb%d" % i, [128, 512], BF16) for i in range(2)]; wTs = [sb("wTs%d" % i, [128, 4, 128], BF16) for i in range(2)]
                negT = sb("negT", [128, 1]); attT = [sb("attT%d" % i, [128, 2048], BF16) for i in range(2)]
                zero1 = sb("zero1", [128, 1]); nTt = sb("nTt", [128, 1])
                P.op("pool", lambda e: e.memset(zero1[:], 0.0), writes=["zero1"])
                pidx = 0
                for h in range(8):
                    kT_ = kTh[h % 2]; v_ = vh[h % 2]; q_ = qTh[h % 2]; at_ = attT[h % 2]
                    P.dma(lambda q, kT_=kT_, h=h: q.dma_start(out=kT_[:], in_=kT_d[h]), reads=["kT_d"], writes=[("kTh", h % 2)], key="ald")
                    P.dma(lambda q, v_=v_, h=h: q.dma_start(out=v_[:], in_=v_d[:, h * 128:(h + 1) * 128].rearrange("(c p) d -> p c d", p=128)),
                          reads=["v_d"], writes=[("vh", h % 2)], key="ald")
                    P.dma(lambda q, q_=q_, h=h: q.dma_start(out=q_[:], in_=qT_d[h]), reads=["qT_d"], writes=[("qTh", h % 2)], key="ald")
                    for j in range(16):
                        ntile = OWN0 + j + 1
                        pieces = []
                        t0 = 0
                        while t0 < ntile:
                            n = min(4, ntile - t0); pieces.append((t0, n)); t0 += n
                        for pi_, (t0, n) in enumerate(pieces):
                            W = n * 128; ks = slice(t0 * 128, t0 * 128 + W)
                            pz = psum[pidx % 2]; pzk = "ps%d" % (pidx % 2); pidx += 1
                            P.op("pe", lambda e, pz=pz, W=W, ks=ks, q_=q_, kT_=kT_, j=j: e.matmul(pz[:, 0:W], lhsT=q_[:, j * 128:(j + 1) * 128], rhs=kT_[:, ks], start=True, stop=True),
                                 reads=[("qTh", h % 2), ("kTh", h % 2)], writes=[pzk])
                            P.op("act", lambda e, pz=pz, W=W: e.activation(out=ee[:, 0:W], in_=pz[:, 0:W], func=AF.Exp), reads=[pzk], writes=["ee"])
                            P.op("act", lambda e, W=W: e.activation(out=sp[:, 0:W], in_=ee[:, 0:W], func=AF.Ln, bias=1.0), reads=["ee"], writes=["sp"])
                            last = (pi_ == len(pieces) - 1)
                            if last:
                                P.op("pool", lambda e, W=W: e.tensor_tensor(out=sp[:, W - 128:W], in0=sp[:, W - 128:W], in1=cmask[:], op=ALU.mult), reads=["sp", "cmask"], writes=["sp"])
                            init = zero1[:, 0:1] if pi_ == 0 else lw_prev_last
                            P.op("dve", lambda e, W=W, init=init: e.tensor_tensor_scan(out=pref[:, 0:W], data0=ones[:, 0:W], data1=sp[:, 0:W], initial=init, op0=ALU.mult, op1=ALU.add),
                                 reads=["sp", "ones", "zero1", "prefl"], writes=["pref"])
                            P.op("pool", lambda e, W=W: e.tensor_copy(out=negT[:], in_=pref[:, W - 1:W]), reads=["pref"], writes=["prefl"])
                            lw_prev_last = negT[:, 0:1]
                            P.op("dve", lambda e, pz=pz, W=W: e.tensor_tensor(out=zs[:, 0:W], in0=pz[:, 0:W], in1=sp[:, 0:W], op=ALU.subtract), reads=[pzk, "sp"], writes=["zs"])
                            P.op("pool", lambda e, W=W, ks=ks: e.tensor_tensor(out=lw0[:, ks], in0=zs[:, 0:W], in1=pref[:, 0:W], op=ALU.add), reads=["zs", "pref"], writes=["lw0"])
                        P.op("pool", lambda e: e.tensor_scalar(out=nTt[:], in0=negT[:], scalar1=-1.0, scalar2=None, op0=ALU.mult), reads=["prefl"], writes=["negTT"])
                        pa = psum[4]
                        for pi_, (t0, n) in enumerate(pieces):
                            W = n * 128; ks = slice(t0 * 128, t0 * 128 + W)
                            w_ = wb[pi_ % 2]; wT_ = wTs[pi_ % 2]
                            P.op("act", lambda e, W=W, ks=ks, w_=w_: e.activation(out=w_[:, 0:W], in_=lw0[:, ks], func=AF.Exp, bias=nTt[:, 0:1]), reads=["lw0", "negTT"], writes=[("wb", pi_ % 2)])
                            if pi_ == len(pieces) - 1:
                                P.op("pool", lambda e, W=W, w_=w_: e.tensor_tensor(out=w_[:, W - 128:W], in0=w_[:, W - 128:W], in1=cmask[:], op=ALU.mult), reads=[("wb", pi_ % 2), "cmask"], writes=[("wb", pi_ % 2)])
                            for i4 in range(n):
                                P.op("pe", lambda e, i4=i4, w_=w_: e.transpose(psb[1][:, i4 * 128:(i4 + 1) * 128], in_=w_[:, i4 * 128:(i4 + 1) * 128], identity=identb[:]),
                                     reads=[("wb", pi_ % 2), "identb"], writes=["psb1"])
                            P.op("dve", lambda e, n=n, wT_=wT_: e.tensor_copy(out=wT_[:, 0:n, :], in_=psb[1][:, 0:n * 128].rearrange("p (k t) -> p k t", k=n)), reads=["psb1"], writes=[("wTs", pi_ % 2)])
                            for i4 in range(n):
                                tl = t0 + i4
                                P.op("pe", lambda e, i4=i4, tl=tl, v_=v_, wT_=wT_, ntile=ntile: e.matmul(pa[:, 0:128], lhsT=v_[:, tl, :], rhs=wT_[:, i4, :], start=(tl == 0), stop=(tl == ntile - 1)),
                                     reads=[("vh", h % 2), ("wTs", pi_ % 2)], writes=["ps4"])
                        P.op("act", lambda e, at_=at_, j=j: e.activation(out=at_[:, j * 128:(j + 1) * 128], in_=pa[:, 0:128], func=AF.Copy), reads=["ps4"], writes=[("attT", h % 2)])
                    P.dma(lambda q, at_=at_, h=h: q.dma_start(out=attT_d[h], in_=at_[:]), reads=[("attT", h % 2)], writes=["attT_d"], key="st")
            P.barrier()
            stage_end(5)

            with ExitStack() as ph:
                sb = lambda n, s, d=F32: ph.enter_context(nc.sbuf_tensor(n, list(s), d))
                gt1 = sb("gt1", [128, D]); gs2 = sb("gs2", [128, D]); sh2 = sb("sh2", [128, D]); junk = sb("junkb", [128, D]); g2 = junk
                bc_row(gt1[:], mod_d[0:1, 2 * D:3 * D], D, "gt1")
                bc_row(sh2[:], mod_d[0:1, 3 * D:4 * D], D, "sh2")
                bc_row(gs2[:], mod_d[0:1, 4 * D:5 * D], D, "gs2")
                P.dma(lambda q: q.dma_start(out=g2[:], in_=norm2_g.broadcast_to([128, D])), writes=["junkB"], key="ld")
                P.op("dve", lambda e: e.scalar_tensor_tensor(out=gs2[:], in0=gs2[:], scalar=1.0, in1=g2[:], op0=ALU.add, op1=ALU.mult), reads=["gs2", "junkB"], writes=["gs2"])
                hTo = sb("hTo", [128, 16, 512], BF16); ygo = sb("ygo", [128, 8, 512], BF16); ato = sb("ato", [128, 8, 512], BF16)
                mT = sb("mT", [128, 16, 512], BF16)
                stg = [sb("stgb%d" % i, [128, 512]) for i in range(2)]
                wgs = sb("wgs", [128, 16, 128], BF16); wga = sb("wga", [128, 16, 128], BF16)
                wgv = sb("wgv", [128, 8, 128], BF16); wgg = sb("wgg", [128, 8, 128], BF16); wau = sb("wau", [128, 8, 128], BF16)
                wo = sb("wo", [128, 16, 512], BF16)
                sgs = sb("sgs", [128, 512]); sga = sb("sga", [128, 512]); syg = sb("syg", [128, 512]); tm1 = sb("tm1", [128, 512]); tm2 = sb("tm2", [128, 512])
                xall = sb("xall", [128, 4, D])
                tA = sb("tAb", [128, D]); hb2 = sb("hb2", [128, D], BF16)
                ss = sb("ssb", [128, 1]); rstd = sb("rstdb", [128, 1]); h2t = sb("h2t", [128, 16, 512], BF16)
                for tg in range(4):
                    o0 = tg * 512; wt0 = (OWN0 * 128) + o0
                    P.dma(lambda q, wt0=wt0: q.dma_start(out=hTo[:], in_=hT_d[:, :, wt0:wt0 + 512].rearrange("k p t -> p k t")), reads=["hT_d"], writes=["hTo"], key="ld")
                    P.dma(lambda q, o0=o0: q.dma_start(out=ygo[:], in_=ygT_d[:, :, o0:o0 + 512].rearrange("k p t -> p k t")), reads=["ygT_d"], writes=["ygo"], key="ld")
                    P.dma(lambda q, o0=o0: q.dma_start(out=ato[:], in_=attT_d[:, :, o0:o0 + 512].rearrange("k p t -> p k t")), reads=["attT_d"], writes=["ato"], key="ld")
                    for dc in range(16):
                        cs_ = slice(dc * 128, (dc + 1) * 128)
                        load_w_bf16(wgs, w_in[:, 4096 + dc * 128:4096 + (dc + 1) * 128], 16, 128, stg, "wgs")
                        load_w_bf16(wga, w_in[:, 6144 + dc * 128:6144 + (dc + 1) * 128], 16, 128, stg, "wga")
                        load_w_bf16(wgv, w_glu[:, dc * 128:(dc + 1) * 128], 8, 128, stg, "wgv")
                        load_w_bf16(wgg, w_glu[:, 2048 + dc * 128:2048 + (dc + 1) * 128], 8, 128, stg, "wgg")
                        load_w_bf16(wau, w_up[:, dc * 128:(dc + 1) * 128], 8, 128, stg, "wau")
                        for kt in range(16):
                            P.op("pe", lambda e, kt=kt: e.matmul(psum[0][:], lhsT=wgs[:, kt, :], rhs=hTo[:, kt, :], start=(kt == 0), stop=(kt == 15)), reads=[("wgs", kt), "hTo"], writes=["ps0"])
                        for kt in range(16):
                            P.op("pe", lambda e, kt=kt: e.matmul(psum[1][:], lhsT=wga[:, kt, :], rhs=hTo[:, kt, :], start=(kt == 0), stop=(kt == 15)), reads=[("wga", kt), "hTo"], writes=["ps1"])
                        for kt in range(8):
                            P.op("pe", lambda e, kt=kt: e.matmul(psum[2][:], lhsT=wgv[:, kt, :], rhs=ygo[:, kt, :], start=(kt == 0), stop=(kt == 7)), reads=[("wgv", kt), "ygo"], writes=["ps2"])
                        for kt in range(8):
                            P.op("pe", lambda e, kt=kt: e.matmul(psum[3][:], lhsT=wgg[:, kt, :], rhs=ygo[:, kt, :], start=(kt == 0), stop=(kt == 7)), reads=[("wgg", kt), "ygo"], writes=["ps3"])
                        for kt in range(8):
                            P.op("pe", lambda e, kt=kt: e.matmul(psum[4][:], lhsT=wau[:, kt, :], rhs=ato[:, kt, :], start=(kt == 0), stop=(kt == 7)), reads=[("wau", kt), "ato"], writes=["ps4"])
                        P.op("act", lambda e: e.activation(out=sgs[:], in_=psum[0][:], func=AF.Sigmoid), reads=["ps0"], writes=["sgs"])
                        P.op("act", lambda e: e.activation(out=sga[:], in_=psum[1][:], func=AF.Sigmoid), reads=["ps1"], writes=["sga"])
                        P.op("act", lambda e: e.activation(out=syg[:], in_=psum[3][:], func=AF.Sigmoid), reads=["ps3"], writes=["syg"])
                        P.op("dve", lambda e: e.tensor_tensor(out=tm1[:], in0=psum[2][:], in1=syg[:], op=ALU.mult), reads=["ps2", "syg"], writes=["tm1"])
                        P.op("pool", lambda e: e.tensor_tensor(out=tm1[:], in0=tm1[:], in1=sgs[:], op=ALU.mult), reads=["tm1", "sgs"], writes=["tm1"])
                        P.op("dve", lambda e: e.tensor_tensor(out=tm2[:], in0=psum[4][:], in1=sga[:], op=ALU.mult), reads=["ps4", "sga"], writes=["tm2"])
                        P.op("pool", lambda e, dc=dc: e.tensor_tensor(out=mT[:, dc, :], in0=tm1[:], in1=tm2[:], op=ALU.add), reads=["tm1", "tm2"], writes=[("mT", dc)])
                    for cc in range(4):
                        r0 = (OWN0 + tg * 4 + cc) * 128
                        P.dma(lambda q, cc=cc, r0=r0: q.dma_start(out=xall[:, cc, :], in_=xw[r0:r0 + 128, :]), writes=[("xall", cc)], key="xld")
                    for nb in range(4):
                        load_w_bf16(wo, w_out[:, nb * 512:(nb + 1) * 512], 16, 512, stg, "wo")
                        for cc in range(4):
                            pp = psum[(nb * 4 + cc) % 2]; ppk = "ps%d" % ((nb * 4 + cc) % 2)
                            for dc in range(16):
                                P.op("pe", lambda e, pp=pp, dc=dc, cc=cc: e.matmul(pp[:], lhsT=mT[:, dc, cc * 128:(cc + 1) * 128], rhs=wo[:, dc, :], start=(dc == 0), stop=(dc == 15)),
                                     reads=[("mT", dc), ("wo", dc)], writes=[ppk])
                            P.op("dve", lambda e, pp=pp, nb=nb: e.tensor_tensor(out=tm1[:], in0=pp[:], in1=gt1[:, nb * 512:(nb + 1) * 512], op=ALU.mult),
                                 reads=[ppk, "gt1"], writes=["tm1"])
                            P.op("pool", lambda e, nb=nb, cc=cc: e.tensor_tensor(out=xall[:, cc, nb * 512:(nb + 1) * 512], in0=xall[:, cc, nb * 512:(nb + 1) * 512], in1=tm1[:], op=ALU.add),
                                 reads=["tm1", ("xall", cc)], writes=[("xall", cc)])
                    for cc in range(4):
                        tl = tg * 4 + cc
                        x1_ = xall[:, cc, :]
                        P.dma(lambda q, x1_=x1_, tl=tl: q.dma_start(out=out[tl * 128:(tl + 1) * 128, :], in_=x1_), reads=[("xall", cc)], writes=["out"], key="st")
                        rms_stats(x1_, ("xall", cc), junk[:], ss[:], rstd[:], "B")
                        P.op("dve", lambda e, x1_=x1_: e.scalar_tensor_tensor(out=tA[:], in0=x1_, scalar=rstd[:, 0:1], in1=gs2[:], op0=ALU.mult, op1=ALU.mult),
                             reads=[("xall", cc), "rstdB", "gs2"], writes=["tAb"])
                        P.op("pool", lambda e: e.tensor_tensor(out=hb2[:], in0=tA[:], in1=sh2[:], op=ALU.add), reads=["tAb", "sh2"], writes=["hb2"])
                        for half in range(2):
                            pb = psb[half]
                            for k8 in range(8):
                                kt = half * 8 + k8
                                P.op("pe", lambda e, pb=pb, k8=k8, kt=kt: e.transpose(pb[:, k8 * 128:(k8 + 1) * 128], in_=hb2[:, kt * 128:(kt + 1) * 128], identity=identb[:]),
                                     reads=["hb2", "identb"], writes=["psb%d" % half])
                            dst = h2t[:, half * 8:(half + 1) * 8, cc * 128:(cc + 1) * 128]
                            src = pb[:].rearrange("p (k t) -> p k t", k=8)
                            if half == 0:
                                P.op("dve", lambda e, dst=dst, src=src: e.tensor_copy(out=dst, in_=src), reads=["psb0"], writes=["h2t"])
                            else:
                                P.op("act", lambda e, dst=dst, src=src: e.activation(out=dst, in_=src, func=AF.Copy), reads=["psb1"], writes=["h2t"])
                    P.dma(lambda q, o0=o0: q.dma_start(out=h2T_d[:, :, o0:o0 + 512].rearrange("k p t -> p k t"), in_=h2t[:]), reads=["h2t"], writes=["h2T_d"], key="st")
            P.barrier()
            stage_end(6)
            with ExitStack() as ph:
                sb = lambda n, s, d=F32: ph.enter_context(nc.sbuf_tensor(n, list(s), d))
                gt2 = sb("gt2", [128, D]); k1b = sb("k1b", [128, 128], BF16); k2b = sb("k2b", [128, 128], BF16)
                bc_row(gt2[:], mod_d[0:1, 5 * D:6 * D], D, "gt2")
                h2g = sb("h2g", [128, 16, 512], BF16); acc = sb("acc", [128, 4, D])
                s2 = sb("s2p", [128, 4, 8, 128]); thr = sb("thr", [128, 4, 8, 128])
                e1 = sb("e1p", [128, 4, 8, 128], BF16); E2 = sb("E2p", [128, 4, 8, 128], BF16)
                with ExitStack() as ph2:
                    sb2 = lambda n, s, d=F32: ph2.enter_context(nc.sbuf_tensor(n, list(s), d))
                    kst = sb2("kst", [128, 256])
                    P.dma(lambda q: q.dma_start(out=kst[:, 0:128], in_=k1T), writes=["kst"], key="ld")
                    P.dma(lambda q: q.dma_start(out=kst[:, 128:256], in_=k2T), writes=["kst"], key="ld")
                    P.op("dve", lambda e: e.tensor_copy(out=k1b[:], in_=kst[:, 0:128]), reads=["kst"], writes=["k1b"])
                    P.op("dve", lambda e: e.tensor_copy(out=k2b[:], in_=kst[:, 128:256]), reads=["kst"], writes=["k2b"])
                    P.barrier()
                for tg in range(4):
                    o0 = tg * 512
                    P.barrier()
                    P.dma(lambda q, o0=o0: q.dma_start(out=h2g[:], in_=h2T_d[:, :, o0:o0 + 512].rearrange("k p t -> p k t")), reads=["h2T_d"], writes=["h2g"], key="ld")
                    with ExitStack() as ph2:
                        sb2 = lambda n, s, d=F32, tg=tg: ph2.enter_context(nc.sbuf_tensor("%s_%d" % (n, tg), list(s), d))
                        qT = sb2("qTp", [128, 16, 512], BF16); wqc = sb2("wqc", [128, 16, 128], BF16)
                        stq = [sb2("stq%d" % i, [128, 128]) for i in range(2)]
                        s1 = sb2("s1p", [128, 8, 128]); v1 = sb2("v1p", [128, 8, 16]); v2 = sb2("v2p", [128, 8, 16])
                        scr = sb2("scr", [128, 256]); cand = sb2("cand", [128, 8, 256]); best = sb2("best", [128, 8, 16])
                        eb = sb2("eb", [128, 8, 16]); Zs = sb2("Zs", [128, 8]); lnZ = sb2("lnZ", [128, 8]); off = sb2("offp", [128, 8])
                        tmp = sb2("tmpp", [128, 8, 128])
                        for fcq in range(16):
                            load_w_bf16(wqc, wq[:, fcq * 128:(fcq + 1) * 128], 16, 128, stq, "wqc")
                            pq = psum[fcq % 2]
                            for kt in range(16):
                                P.op("pe", lambda e, pq=pq, kt=kt: e.matmul(pq[:], lhsT=wqc[:, kt, :], rhs=h2g[:, kt, :], start=(kt == 0), stop=(kt == 15)),
                                     reads=[("wqc", kt), "h2g"], writes=["ps%d" % (fcq % 2)])
                            P.op("act", lambda e, pq=pq, fcq=fcq: e.activation(out=qT[:, fcq, :], in_=pq[:], func=AF.Copy), reads=["ps%d" % (fcq % 2)], writes=[("qTp", fcq)])
                        for tl in range(4):
                            ts_ = slice(tl * 128, (tl + 1) * 128)
                            for h in range(8):
                                P.op("pe", lambda e, h=h, ts_=ts_: e.matmul(psum[2 + h // 4][:, (h % 4) * 128:(h % 4 + 1) * 128], lhsT=qT[:, 2 * h, ts_], rhs=k1b[:], start=True, stop=True),
                                     reads=[("qTp", 2 * h), "k1b"], writes=["ps%d" % (2 + h // 4)])
                                P.op("pe", lambda e, h=h, ts_=ts_: e.matmul(psum[4 + h // 4][:, (h % 4) * 128:(h % 4 + 1) * 128], lhsT=qT[:, 2 * h + 1, ts_], rhs=k2b[:], start=True, stop=True),
                                     reads=[("qTp", 2 * h + 1), "k2b"], writes=["ps%d" % (4 + h // 4)])
                            for hh in range(2):
                                P.op("act", lambda e, hh=hh: e.activation(out=s1[:, hh * 4:(hh + 1) * 4, :], in_=psum[2 + hh][:].rearrange("p (h i) -> p h i", h=4), func=AF.Copy),
                                     reads=["ps%d" % (2 + hh)], writes=["s1p"])
                                P.op("dve", lambda e, hh=hh, tl=tl: e.tensor_copy(out=s2[:, tl, hh * 4:(hh + 1) * 4, :], in_=psum[4 + hh][:].rearrange("p (h i) -> p h i", h=4)),
                                     reads=["ps%d" % (4 + hh)], writes=[("s2p", tl)])
                            for (src_, vv, rk) in ((s1, v1, "s1p"), (None, v2, ("s2p", tl))):
                                for h in range(8):
                                    sv = s1[:, h, :] if src_ is not None else s2[:, tl, h, :]
                                    P.op("dve", lambda e, sv=sv, vv=vv, h=h: e.max(out=vv[:, h, 0:8], in_=sv), reads=[rk], writes=["vtop"])
                                    P.op("dve", lambda e, sv=sv, vv=vv, h=h: e.match_replace(out=scr[:, 0:128], in_to_replace=vv[:, h, 0:8], in_values=sv, imm_value=-1e30),
                                         reads=[rk, "vtop"], writes=["scr"])
                                    P.op("dve", lambda e, vv=vv, h=h: e.max(out=vv[:, h, 8:16], in_=scr[:, 0:128]), reads=["scr"], writes=["vtop"])
                            P.op("dve", lambda e: e.tensor_tensor(out=cand[:].rearrange("p h (a b) -> p h a b", a=16), in0=v1[:].unsqueeze(3).broadcast_to([128, 8, 16, 16]),
                                                                  in1=v2[:].unsqueeze(2).broadcast_to([128, 8, 16, 16]), op=ALU.add), reads=["vtop"], writes=["cand"])
                            for h in range(8):
                                P.op("dve", lambda e, h=h: e.max(out=best[:, h, 0:8], in_=cand[:, h, :]), reads=["cand"], writes=["best"])
                                P.op("dve", lambda e, h=h: e.match_replace(out=scr[:], in_to_replace=best[:, h, 0:8], in_values=cand[:, h, :], imm_value=-1e30),
                                     reads=["cand", "best"], writes=["scr"])
                                P.op("dve", lambda e, h=h: e.max(out=best[:, h, 8:16], in_=scr[:]), reads=["scr"], writes=["best"])
                            P.op("dve", lambda e: e.tensor_tensor(out=eb[:], in0=best[:], in1=best[:, :, 0:1].broadcast_to([128, 8, 16]), op=ALU.subtract), reads=["best"], writes=["eb"])
                            P.op("act", lambda e: e.activation(out=eb[:], in_=eb[:], func=AF.Exp), reads=["eb"], writes=["eb"])
                            P.op("dve", lambda e: e.tensor_reduce(out=Zs[:], in_=eb[:], axis=AX.X, op=ALU.add), reads=["eb"], writes=["Zs"])
                            P.op("act", lambda e: e.activation(out=lnZ[:], in_=Zs[:], func=AF.Ln), reads=["Zs"], writes=["lnZ"])
                            P.op("dve", lambda e, tl=tl: e.tensor_tensor(out=thr[:, tl], in0=best[:, :, 15:16].broadcast_to([128, 8, 128]), in1=s1[:], op=ALU.subtract),
                                 reads=["best", "s1p"], writes=[("thr", tl)])
                            P.op("dve", lambda e: e.tensor_tensor(out=off[:], in0=v1[:, :, 0], in1=lnZ[:], op=ALU.add), reads=["vtop", "lnZ"], writes=["offp"])
                            P.op("dve", lambda e: e.tensor_tensor(out=tmp[:], in0=s1[:], in1=off[:].unsqueeze(2).broadcast_to([128, 8, 128]), op=ALU.subtract),
                                 reads=["s1p", "offp"], writes=["tmpp"])
                            P.op("act", lambda e, tl=tl: e.activation(out=e1[:, tl], in_=tmp[:], func=AF.Exp), reads=["tmpp"], writes=[("e1p", tl)])
                            P.op("dve", lambda e, tl=tl: e.tensor_tensor(out=tmp[:], in0=s2[:, tl], in1=v2[:, :, 0:1].broadcast_to([128, 8, 128]), op=ALU.subtract),
                                 reads=[("s2p", tl), "vtop", "tmpp"], writes=["tmpp"])
                            P.op("act", lambda e, tl=tl: e.activation(out=E2[:, tl], in_=tmp[:], func=AF.Exp), reads=["tmpp"], writes=[("E2p", tl)])
                        P.barrier()
                    with ExitStack() as ph2:
                        sb2 = lambda n, s, d=F32, tg=tg: ph2.enter_context(nc.sbuf_tensor("%s_%d" % (n, tg), list(s), d))
                        stU = [sb2("stU%d" % i, [128, D]) for i in range(2)]
                        Ub = sb2("Ub", [128, D], BF16)
                        UcT = [sb2("UcT%d" % i, [128, 16, 128], BF16) for i in range(4)]
                        Vc = [sb2("Vc%d" % i, [128, D], BF16) for i in range(4)]
                        gA = [sb2("gA%d" % i, [128, 512]) for i in range(2)]
                        GAT = [sb2("GAT%d" % i, [128, 512], BF16) for i in range(4)]
                        X = sb2("Xp", [128, 8, 128]); Gm = sb2("Gmp", [128, 8, 128]); Gs = sb2("Gsp", [128, 8, 128])
                        Wc = [sb2("Wc%d" % i, [128, 128], BF16) for i in range(2)]
                        xfin = stU
                        nld = 0
                        for cg in range(32):
                            for ci in range(4):
                                c = cg * 4 + ci
                                su = stU[nld % 2]; suk = ("stU", nld % 2); nld += 1
                                P.dma(lambda q, su=su, c=c: q.dma_start(out=su[:], in_=u_tab[c * 128:(c + 1) * 128, :]), writes=[suk], key="tab")
                                P.op("pool", lambda e, su=su: e.tensor_copy(out=Ub[:], in_=su[:]), reads=[suk], writes=["Ub"])
                                for half in range(2):
                                    for k8 in range(8):
                                        kt = half * 8 + k8
                                        P.op("pe", lambda e, k8=k8, kt=kt: e.transpose(psb[0][:, k8 * 128:(k8 + 1) * 128], in_=Ub[:, kt * 128:(kt + 1) * 128], identity=identb[:]),
                                             reads=["Ub", "identb"], writes=["psb0"])
                                    P.op("act", lambda e, half=half, ci=ci: e.activation(out=UcT[ci][:, half * 8:(half + 1) * 8, :], in_=psb[0][:].rearrange("p (k t) -> p k t", k=8), func=AF.Copy),
                                         reads=["psb0"], writes=[("UcT", ci)])
                                sv_ = stU[nld % 2]; svk = ("stU", nld % 2); nld += 1
                                P.dma(lambda q, sv_=sv_, c=c: q.dma_start(out=sv_[:], in_=v_tab[c * 128:(c + 1) * 128, :]), writes=[svk], key="tab")
                                P.op("act", lambda e, sv_=sv_, ci=ci: e.activation(out=Vc[ci][:], in_=sv_[:], func=AF.Copy), reads=[svk], writes=[("Vc", ci)])
                                pa = psum[ci % 2]; pak = "ps%d" % (ci % 2); ga_ = gA[ci % 2]; gak = ("gA", ci % 2)
                                for kt in range(16):
                                    P.op("pe", lambda e, pa=pa, kt=kt, ci=ci: e.matmul(pa[:], lhsT=UcT[ci][:, kt, :], rhs=h2g[:, kt, :], start=(kt == 0), stop=(kt == 15)),
                                         reads=[("UcT", ci), "h2g"], writes=[pak])
                                P.op("act", lambda e, pa=pa, ga_=ga_: e.activation(out=ga_[:], in_=pa[:], func=AF.Gelu_apprx_tanh), reads=[pak], writes=[gak])
                                for tl in range(4):
                                    wc_ = Wc[tl % 2]; wck = ("Wc", tl % 2)
                                    P.op("pool", lambda e, tl=tl, c=c: e.tensor_tensor(out=X[:], in0=s2[:, tl], in1=thr[:, tl, :, c:c + 1].broadcast_to([128, 8, 128]), op=ALU.subtract),
                                         reads=[("s2p", tl), ("thr", tl)], writes=["Xp"])
                                    P.op("dve", lambda e, tl=tl: e.scalar_tensor_tensor(out=Gm[:], in0=X[:], scalar=-1e-5, in1=E2[:, tl], op0=ALU.is_ge, op1=ALU.mult),
                                         reads=["Xp", ("E2p", tl)], writes=["Gmp"])
                                    P.op("pool", lambda e, tl=tl, c=c: e.tensor_tensor(out=Gs[:], in0=Gm[:], in1=e1[:, tl, :, c:c + 1].broadcast_to([128, 8, 128]), op=ALU.mult),
                                         reads=["Gmp", ("e1p", tl)], writes=["Gsp"])
                                    def _red(e, wc_=wc_):
                                        with nc.allow_low_precision("fp32 reduce, bf16 store"):
                                            return e.tensor_reduce(out=wc_[:], in_=Gs[:].rearrange("p h i -> p i h"), axis=AX.X, op=ALU.add)
                                    P.op("dve", _red, reads=["Gsp"], writes=[wck])
                                    P.op("pe", lambda e, tl=tl, wc_=wc_: e.transpose(psb[1][:, tl * 128:(tl + 1) * 128], in_=wc_[:], identity=identb[:]),
                                         reads=[wck, "identb"], writes=["psb1"])
                                P.op("dve", lambda e, ci=ci, ga_=ga_: e.tensor_tensor(out=GAT[ci][:], in0=psb[1][:, 0:512], in1=ga_[:], op=ALU.mult),
                                     reads=["psb1", gak], writes=[("GAT", ci)])
                            for tl in range(4):
                                for ci in range(4):
                                    for nb in range(4):
                                        P.op("pe", lambda e, tl=tl, ci=ci, nb=nb: e.matmul(psum[2 + nb][:], lhsT=GAT[ci][:, tl * 128:(tl + 1) * 128], rhs=Vc[ci][:, nb * 512:(nb + 1) * 512], start=(ci == 0), stop=(ci == 3)),
                                             reads=[("GAT", ci), ("Vc", ci)], writes=["ps%d" % (2 + nb)])
                                for nb in range(4):
                                    a_sl = acc[:, tl, nb * 512:(nb + 1) * 512]
                                    if cg == 0:
                                        P.op("dve", lambda e, a_sl=a_sl, nb=nb: e.tensor_copy(out=a_sl, in_=psum[2 + nb][:]), reads=["ps%d" % (2 + nb)], writes=[("acc", tl, nb)])
                                    else:
                                        P.op("dve", lambda e, a_sl=a_sl, nb=nb: e.tensor_tensor(out=a_sl, in0=psum[2 + nb][:], in1=a_sl, op=ALU.add), reads=["ps%d" % (2 + nb), ("acc", tl, nb)], writes=[("acc", tl, nb)])
                        for tl in range(4):
                            r0 = o0 + tl * 128
                            xf = xfin[tl % 2]; xfk = ("stU", tl % 2)
                            P.dma(lambda q, xf=xf, r0=r0: q.dma_start(out=xf[:], in_=out[r0:r0 + 128, :]), reads=["out"], writes=[xfk], key="tab")
                            P.op("pool", lambda e, tl=tl: e.tensor_tensor(out=acc[:, tl, :], in0=acc[:, tl, :], in1=gt2[:], op=ALU.mult),
                                 reads=[("acc", tl, 0), ("acc", tl, 1), ("acc", tl, 2), ("acc", tl, 3), "gt2"], writes=[("acc", tl, 0), ("acc", tl, 1), ("acc", tl, 2), ("acc", tl, 3)])
                            P.op("dve", lambda e, tl=tl, xf=xf: e.tensor_tensor(out=xf[:], in0=xf[:], in1=acc[:, tl, :], op=ALU.add),
                                 reads=[xfk, ("acc", tl, 0), ("acc", tl, 1), ("acc", tl, 2), ("acc", tl, 3)], writes=[xfk])
                            P.dma(lambda q, xf=xf, r0=r0: q.dma_start(out=out[r0:r0 + 128, :], in_=xf[:]), reads=[xfk], writes=["out"], key="st")
                        P.barrier()
            P.barrier()
            stage_end(7)
        except _Stop:
            pass
        P.barrier()
        P.emit()
    return nc


_NC_CACHE = {}


def _host_inputs(i, x, c, w_ada, b_ada, norm1_g, w_in, lam_re, lam_im, log_dt, ssm_b_re, ssm_b_im, ssm_c_re, ssm_c_im,
                 ssm_d, w_glu, q_norm_g, k_norm_g, w_att_up, w_out, norm2_g, peer_wq, peer_k1, peer_k2, peer_u, peer_v, shared):
    b, q = i // 4, i % 4
    t_end = (q + 1) * 2048
    t_start = t_end - SEQ
    xw = np.zeros((SEQ, D), np.float32)
    lo = max(t_start, 0)
    xw[lo - t_start:] = x[b, lo:t_end]
    tok = t_start + np.arange(SEQ)
    valid = (tok >= 0).astype(np.float32).reshape(NT, 128).T.copy()
    cbm = np.ascontiguousarray(c[b].reshape(16, 128).T)
    d = dict(shared)
    d.update(xw=xw, valid=valid, cb=cbm)
    return d


def _prep(x, c, w_ada, b_ada, norm1_g, w_in, lam_re, lam_im, log_dt, ssm_b_re, ssm_b_im, ssm_c_re, ssm_c_im,
           ssm_d, w_glu, q_norm_g, k_norm_g, w_att_up, w_out, norm2_g, peer_wq, peer_k1, peer_k2, peer_u, peer_v):
    f = lambda a: np.ascontiguousarray(np.asarray(a, dtype=np.float32))
    x = f(x); c = f(c)
    lamT_re = f(np.asarray(lam_re)[0].reshape(32, 2, 64).transpose(1, 2, 0).reshape(128, 32))
    lamT_im = f(np.asarray(lam_im)[0].reshape(32, 2, 64).transpose(1, 2, 0).reshape(128, 32))
    ldtT = f(np.broadcast_to(np.asarray(log_dt)[0].reshape(32, 2).T[:, None, :], (2, 64, 32)).reshape(128, 32))
    Bre = np.asarray(ssm_b_re)[0]; Bim = np.asarray(ssm_b_im)[0]
    BreT = np.zeros((128, 32, 128), np.float32); BimT = np.zeros((128, 32, 128), np.float32)
    for g in range(64):
        gl = g % 8
        st, g2 = g // 2, g % 2
        BreT[gl * 16:(gl + 1) * 16, st, g2 * 64:(g2 + 1) * 64] = Bre[g].T
        BimT[gl * 16:(gl + 1) * 16, st, g2 * 64:(g2 + 1) * 64] = Bim[g].T
    Cr = np.asarray(ssm_c_re)[0]; Ci = np.asarray(ssm_c_im)[0]
    Cre = np.zeros((128, 32, 128), np.float32); Cim = np.zeros((128, 32, 128), np.float32)
    for g in range(64):
        st, g2, gl = g // 2, g % 2, g % 8
        Cre[g2 * 64:(g2 + 1) * 64, st, gl * 16:(gl + 1) * 16] = Cr[g].T
        Cim[g2 * 64:(g2 + 1) * 64, st, gl * 16:(gl + 1) * 16] = Ci[g].T
    dsk = f(np.asarray(ssm_d)[0].reshape(8, 128).T)
    sidx = f(np.broadcast_to(np.arange(128, dtype=np.float32)[None, :], (128, 128)))
    shared = dict(w_ada=f(np.asarray(w_ada)[0]), b_ada=f(np.asarray(b_ada)[0][None, :]), norm1_g=f(np.asarray(norm1_g)[0][None, :]),
                  norm2_g=f(np.asarray(norm2_g)[0][None, :]), qg=f(np.asarray(q_norm_g)[0][None, :]), kg=f(np.asarray(k_norm_g)[0][None, :]),
                  w_in=f(np.asarray(w_in)[0]), w_glu=f(np.asarray(w_glu)[0]), w_up=f(np.asarray(w_att_up)[0]), w_out=f(np.asarray(w_out)[0]),
                  wq=f(np.asarray(peer_wq)[0]), k1T=f(np.asarray(peer_k1)[0].T), k2T=f(np.asarray(peer_k2)[0].T),
                  u_tab=f(np.asarray(peer_u)[0]), v_tab=f(np.asarray(peer_v)[0]),
                  lamT_re=lamT_re, lamT_im=lamT_im, ldtT=ldtT, BreT=BreT, BimT=BimT, Cre=Cre, Cim=Cim, dsk=dsk, sidx=sidx)
    args = (x, c, w_ada, b_ada, norm1_g, w_in, lam_re, lam_im, log_dt, ssm_b_re, ssm_b_im, ssm_c_re, ssm_c_im,
            ssm_d, w_glu, q_norm_g, k_norm_g, w_att_up, w_out, norm2_g, peer_wq, peer_k1, peer_k2, peer_u, peer_v)
    in_maps = [_host_inputs(i, *args, shared) for i in range(8)]
    return in_maps


def kernel(**inputs):
    in_maps = _prep(**inputs)
    if "nc" not in _NC_CACHE:
        _NC_CACHE["nc"] = build_nc()
    nc = _NC_CACHE["nc"]
    res = run_bass_kernel_spmd(nc, in_maps, core_ids=list(range(8)))
    outp = np.zeros((2, SEQ, D), np.float32)
    for i in range(8):
        b, q = i // 4, i % 4
        outp[b, q * 2048:(q + 1) * 2048] = res.results[i]["out"]
    return outp
```

```python
import math
from contextlib import ExitStack
import numpy as np
import concourse.bass as bass
import concourse.mybir as mybir
from concourse.bass_utils import run_bass_kernel_spmd

F32 = mybir.dt.float32
BF16 = mybir.dt.bfloat16
I32 = mybir.dt.int32
AF = mybir.ActivationFunctionType
ALU = mybir.AluOpType
AX = mybir.AxisListType

D = 2048
SEQ = 8192
NT = 64
OWN0 = 48
EPS = 1e-6


import types


def _freeze(fn):
    if fn is None or fn.__closure__ is None:
        return fn
    cells = []
    for c in fn.__closure__:
        try:
            cells.append(types.CellType(c.cell_contents))
        except ValueError:
            cells.append(c)
    return types.FunctionType(fn.__code__, fn.__globals__, fn.__name__, fn.__defaults__, tuple(cells))


class Prog:
    ENG = ("pe", "act", "dve", "pool", "sp")

    def __init__(self, nc, stack, same_engine_sync=False):
        self.nc = nc
        self.stack = stack
        self.same = same_engine_sync
        self.streams = {e: [] for e in self.ENG}
        self.count = {e: 0 for e in self.ENG}
        self.sem = {e: stack.enter_context(nc.semaphore("prog_" + e)) for e in self.ENG}
        self.known = {e: {} for e in self.ENG}
        self.last_w = {}
        self.readers = {}
        self.dma_keys = {}

    def _sem_of(self, key):
        if isinstance(key, str):
            return self.sem[key]
        return self.dma_keys[key[1]]["sems"][key[2]]

    def _deps(self, reads, writes):
        deps = []
        for r in reads:
            if r in self.last_w:
                deps.append(self.last_w[r])
        for w in writes:
            if w in self.last_w:
                deps.append(self.last_w[w])
            deps.extend(self.readers.get(w, ()))
        return deps

    def _waits(self, eng, deps):
        need = {}
        for (k, v) in deps:
            if k == eng and (not self.same or eng == "pe"):
                continue
            if self.known[eng].get(k, 0) >= v:
                continue
            if need.get(k, 0) < v:
                need[k] = v
        for k, v in need.items():
            self.known[eng][k] = v
        return list(need.items())

    def _commit(self, ev, reads, writes):
        for r in reads:
            self.readers.setdefault(r, []).append(ev)
        for w in writes:
            self.last_w[w] = ev
            self.readers[w] = []

    def op(self, eng, fn, reads=(), writes=()):
        waits = self._waits(eng, self._deps(reads, writes))
        self.count[eng] += 1
        ev = (eng, self.count[eng])
        self.streams[eng].append((waits, _freeze(fn), (eng, 1)))
        self._commit(ev, reads, writes)
        return ev

    def dma(self, fn, reads=(), writes=(), key="d", nsem=4, queue="sp"):
        if key not in self.dma_keys:
            sems = [self.stack.enter_context(self.nc.semaphore("dma_%s_%d" % (key, i))) for i in range(nsem)]
            self.dma_keys[key] = {"sems": sems, "n": 0}
        st = self.dma_keys[key]
        i = st["n"]
        st["n"] += 1
        R = len(st["sems"])
        evk = ("dma", key, i % R)
        ev = (evk, 16 * (i // R + 1))
        waits = self._waits(queue, self._deps(reads, writes))
        if i >= R:
            prev = (evk, 16 * (i // R))
            if self.known[queue].get(evk, 0) < prev[1]:
                self.known[queue][evk] = prev[1]
                waits.append(prev)
        self.streams[queue].append((waits, _freeze(fn), (evk, 16)))
        self._commit(ev, reads, writes)
        return ev

    def _all_events(self):
        deps = []
        for e in self.ENG:
            if self.count[e]:
                deps.append((e, self.count[e]))
        for key, st in self.dma_keys.items():
            R = len(st["sems"])
            for j in range(min(R, st["n"])):
                n_on = (st["n"] - 1 - j) // R + 1
                deps.append((("dma", key, j), 16 * n_on))
        return deps

    def barrier(self):
        deps = self._all_events()
        for e in self.ENG:
            waits = self._waits(e, [d for d in deps if d[0] != e])
            if waits:
                self.streams[e].append((waits, None, None))

    def emit(self):
        nc = self.nc
        engobj = {"pe": "tensor", "act": "scalar", "dve": "vector", "pool": "gpsimd", "sp": "sync"}
        with nc.Block() as block:
            for e in self.ENG:
                stream = self.streams[e]
                if not stream:
                    continue

                def body(engine, stream=stream):
                    for waits, fn, inc in stream:
                        for k, v in waits:
                            engine.wait_ge(self._sem_of(k), v)
                        if fn is None:
                            continue
                        ins = fn(engine)
                        ins.then_inc(self._sem_of(inc[0]), inc[1])

                getattr(block, engobj[e])(body)


class _Stop(Exception):
    pass


def build_nc(stages=99, debug=False, small_tabs=False):
    nc = bass.Bass("TRN2", target_bir_lowering=False)

    def stage_end(n):
        if stages <= n:
            raise _Stop()

    dbg_state = {}

    def dbg_dump(P, name, ap, shape, dt, rkeys):
        if not debug:
            return
        t = nc.dram_tensor("dbg_" + name, list(shape), dt, kind="ExternalOutput").ap()
        P.dma(lambda q: q.dma_start(out=t, in_=ap), reads=rkeys, writes=["dbg_" + name], key="dbg")

    def din(name, shape, dt=F32):
        return nc.dram_tensor(name, list(shape), dt, kind="ExternalInput").ap()

    def dscr(name, shape, dt):
        return nc.dram_tensor(name, list(shape), dt, kind=("ExternalOutput" if debug else "Internal")).ap()

    xw = din("xw", [SEQ, D]); valid = din("valid", [128, NT]); cb = din("cb", [128, 16])
    w_ada = din("w_ada", [D, 6 * D]); b_ada = din("b_ada", [1, 6 * D])
    norm1_g = din("norm1_g", [1, D]); norm2_g = din("norm2_g", [1, D])
    qg = din("qg", [1, 128]); kg = din("kg", [1, 128])
    w_in = din("w_in", [D, 8192]); w_glu = din("w_glu", [1024, 4096]); w_up = din("w_up", [1024, D])
    w_out = din("w_out", [D, D]); wq = din("wq", [D, D]); k1T = din("k1T", [128, 128]); k2T = din("k2T", [128, 128])
    NTAB = 128 if small_tabs else 16384
    u_tab = din("u_tab", [NTAB, D]); v_tab = din("v_tab", [NTAB, D])
    lamT_re = din("lamT_re", [128, 32]); lamT_im = din("lamT_im", [128, 32]); ldtT = din("ldtT", [128, 32])
    BreT = din("BreT", [128, 32, 128]); BimT = din("BimT", [128, 32, 128])
    Cre = din("Cre", [128, 32, 128]); Cim = din("Cim", [128, 32, 128]); dsk = din("dsk", [128, 8])
    sidx = din("sidx", [128, 128])
    out = nc.dram_tensor("out", [2048, D], F32, kind="ExternalOutput").ap()

    mod_d = dscr("mod_d", [1, 6 * D], F32)
    hT_d = dscr("hT_d", [16, 128, SEQ], BF16)
    kT_d = dscr("kT_d", [8, 128, SEQ], BF16)
    v_d = dscr("v_d", [SEQ, 1024], BF16)
    qT_d = dscr("qT_d", [8, 128, 2048], BF16)
    ygT_d = dscr("ygT_d", [8, 128, 2048], BF16)
    attT_d = dscr("attT_d", [8, 128, 2048], BF16)
    h2T_d = dscr("h2T_d", [16, 128, 2048], BF16)

    with ExitStack() as top:
        P = Prog(nc, top, same_engine_sync=True)
        try:
            tsb = lambda n, s, d=F32: top.enter_context(nc.sbuf_tensor(n, list(s), d))
            ident = tsb("ident", [128, 128]); identb = tsb("identb", [128, 128], BF16)
            cmask = tsb("cmask", [128, 128])
            ones = tsb("ones", [128, 512])
            P.op("pool", lambda e: e.memset(ident[:], 0.0), writes=["ident"])
            P.op("pool", lambda e: e.affine_select(out=ident[:], in_=ident[:], pattern=[[-1, 128]], compare_op=ALU.not_equal,
                                                   fill=1.0, base=0, channel_multiplier=1), reads=["ident"], writes=["ident"])
            P.op("pool", lambda e: e.tensor_copy(out=identb[:], in_=ident[:]), reads=["ident"], writes=["identb"])
            P.op("pool", lambda e: e.memset(ones[:], 1.0), writes=["ones"])
            P.op("pool", lambda e: e.memset(cmask[:], 1.0), writes=["cmask"])
            P.op("pool", lambda e: e.affine_select(out=cmask[:], in_=cmask[:], pattern=[[-1, 128]], compare_op=ALU.is_gt,
                                                   fill=0.0, base=0, channel_multiplier=1), reads=["cmask"], writes=["cmask"])

            psum = [top.enter_context(nc.psum_tensor("ps%d" % i, [128, 512], F32)) for i in range(6)]
            psb = [top.enter_context(nc.psum_tensor("psb%d" % i, [128, 1024], BF16)) for i in range(2)]

            def load_w_bf16(dst, src, nk, width, stg, tag, col0=0):
                for kt in range(nk):
                    s = stg[kt % len(stg)]
                    sk = ("stg", id(stg), kt % len(stg))
                    P.dma(lambda q, s=s, kt=kt: q.dma_start(out=s[:, 0:width], in_=src[kt * 128:(kt + 1) * 128, :]),
                          writes=[sk], key="wld")
                    eng = ("act", "pool")[kt % 2]
                    if eng == "act":
                        P.op("act", lambda e, s=s, kt=kt: e.activation(out=dst[:, kt, col0:col0 + width], in_=s[:, 0:width], func=AF.Copy),
                             reads=[sk], writes=[(tag, kt)])
                    else:
                        P.op("pool", lambda e, s=s, kt=kt: e.tensor_copy(out=dst[:, kt, col0:col0 + width], in_=s[:, 0:width]),
                             reads=[sk], writes=[(tag, kt)])

            with ExitStack() as ph:
                sb = lambda n, s, d=F32: ph.enter_context(nc.sbuf_tensor(n, list(s), d))
                cbt = sb("cbt", [128, 16]); csl = sb("csl", [128, 16]); brow = sb("brow", [1, 6 * D]); mrow = sb("mrow", [1, 6 * D])
                wblk = [sb("wblk%d" % i, [128, 16, 512]) for i in range(2)]
                P.dma(lambda q: q.dma_start(out=cbt[:], in_=cb), writes=["cbt"], key="ld")
                P.dma(lambda q: q.dma_start(out=brow[:], in_=b_ada), writes=["brow"], key="ld")
                P.op("act", lambda e: e.activation(out=csl[:], in_=cbt[:], func=AF.Silu), reads=["cbt"], writes=["csl"])
                for nb in range(24):
                    wb = wblk[nb % 2]
                    P.dma(lambda q, wb=wb, nb=nb: q.dma_start(out=wb[:], in_=w_ada[:, nb * 512:(nb + 1) * 512].rearrange("(kt p) n -> p kt n", p=128)),
                          writes=[("wblk", nb % 2)], key="ld")
                    for kt in range(16):
                        P.op("pe", lambda e, wb=wb, kt=kt: e.matmul(psum[0][0:1, :], lhsT=csl[:, kt:kt + 1], rhs=wb[:, kt, :], start=(kt == 0), stop=(kt == 15)),
                             reads=["csl", ("wblk", nb % 2)], writes=["ps0"])
                    P.op("dve", lambda e, nb=nb: e.tensor_tensor(out=mrow[0:1, nb * 512:(nb + 1) * 512], in0=psum[0][0:1, :],
                                                                 in1=brow[0:1, nb * 512:(nb + 1) * 512], op=ALU.add),
                         reads=["ps0", "brow"], writes=["mrow"])
                P.dma(lambda q: q.dma_start(out=mod_d, in_=mrow[:]), reads=["mrow"], writes=["mod_d"], key="st")
            P.barrier()
            stage_end(1)

            def bc_row(dst, row_ap, n, wkey):
                P.dma(lambda q: q.dma_start(out=dst, in_=row_ap.broadcast_to([128, n])), reads=["mod_d"], writes=[wkey], key="ld")

            def rms_stats(xt, xkey, junk, ss, rstd, tagsfx):
                P.op("act", lambda e: e.activation(out=junk, in_=xt, func=AF.Square, accum_out=ss), reads=[xkey], writes=["junk" + tagsfx, "ss" + tagsfx])
                P.op("dve", lambda e: e.tensor_scalar(out=ss, in0=ss, scalar1=1.0 / D, scalar2=EPS, op0=ALU.mult, op1=ALU.add),
                     reads=["ss" + tagsfx], writes=["ss" + tagsfx])
                P.op("act", lambda e: e.activation(out=ss, in_=ss, func=AF.Sqrt), reads=["ss" + tagsfx], writes=["ss" + tagsfx])
                P.op("dve", lambda e: e.reciprocal(out=rstd, in_=ss), reads=["ss" + tagsfx], writes=["rstd" + tagsfx])

            with ExitStack() as ph:
                sb = lambda n, s, d=F32: ph.enter_context(nc.sbuf_tensor(n, list(s), d))
                gs1 = sb("gs1", [128, D]); sh1 = sb("sh1", [128, D]); g1 = sb("g1", [128, D]); vld = sb("vld", [128, NT])
                xt = [sb("xt%d" % i, [128, D]) for i in range(2)]
                junk = sb("junk", [128, D]); tA = sb("tA", [128, D]); tB = sb("tB", [128, D])
                hb = [sb("hb%d" % i, [128, D], BF16) for i in range(2)]
                hTt = [sb("hTt%d" % i, [128, 16, 512], BF16) for i in range(2)]
                ss = sb("ss", [128, 1]); rstd = sb("rstd", [128, 1])
                bc_row(sh1[:], mod_d[0:1, 0:D], D, "sh1")
                bc_row(gs1[:], mod_d[0:1, D:2 * D], D, "gs1")
                P.dma(lambda q: q.dma_start(out=g1[:], in_=norm1_g.broadcast_to([128, D])), writes=["g1"], key="ld")
                P.dma(lambda q: q.dma_start(out=vld[:], in_=valid), writes=["vld"], key="ld")
                P.op("dve", lambda e: e.scalar_tensor_tensor(out=gs1[:], in0=gs1[:], scalar=1.0, in1=g1[:], op0=ALU.add, op1=ALU.mult),
                     reads=["gs1", "g1"], writes=["gs1"])
                for c in range(NT):
                    x_ = xt[c % 2]; xk = ("xt", c % 2); g = c // 4; hT_ = hTt[g % 2]; hb_ = hb[c % 2]
                    P.dma(lambda q, x_=x_, c=c: q.dma_start(out=x_[:], in_=xw[c * 128:(c + 1) * 128, :]), writes=[xk], key="xld")
                    rms_stats(x_[:], xk, junk[:], ss[:], rstd[:], "A")
                    P.op("dve", lambda e, x_=x_: e.scalar_tensor_tensor(out=tA[:], in0=x_[:], scalar=rstd[:, 0:1], in1=gs1[:], op0=ALU.mult, op1=ALU.mult),
                         reads=[xk, "rstdA", "gs1"], writes=["tA"])
                    P.op("pool", lambda e: e.tensor_tensor(out=tB[:], in0=tA[:], in1=sh1[:], op=ALU.add), reads=["tA", "sh1"], writes=["tB"])
                    P.op("pool", lambda e, hb_=hb_, c=c: e.tensor_scalar(out=hb_[:], in0=tB[:], scalar1=vld[:, c:c + 1], scalar2=None, op0=ALU.mult),
                         reads=["tB", "vld"], writes=[("hb", c % 2)])
                    for half in range(2):
                        pb = psb[half]
                        for k8 in range(8):
                            kt = half * 8 + k8
                            P.op("pe", lambda e, pb=pb, k8=k8, kt=kt, hb_=hb_: e.transpose(pb[:, k8 * 128:(k8 + 1) * 128], in_=hb_[:, kt * 128:(kt + 1) * 128], identity=identb[:]),
                                 reads=[("hb", c % 2), "identb"], writes=["psb%d" % half])
                        eng = ("dve", "act")[half]
                        dst = hT_[:, half * 8:(half + 1) * 8, (c % 4) * 128:(c % 4 + 1) * 128]
                        src = pb[:].rearrange("p (k t) -> p k t", k=8)
                        if eng == "dve":
                            P.op("dve", lambda e, dst=dst, src=src: e.tensor_copy(out=dst, in_=src), reads=["psb%d" % half], writes=[("hTt", g % 2)])
                        else:
                            P.op("act", lambda e, dst=dst, src=src: e.activation(out=dst, in_=src, func=AF.Copy), reads=["psb%d" % half], writes=[("hTt", g % 2)])
                    if c % 4 == 3:
                        P.dma(lambda q, hT_=hT_, g=g: q.dma_start(out=hT_d[:, :, g * 512:(g + 1) * 512].rearrange("k p t -> p k t"), in_=hT_[:]),
                              reads=[("hTt", g % 2)], writes=["hT_d"], key="st")
            P.barrier()
            stage_end(2)

            with ExitStack() as ph:
                sb = lambda n, s, d=F32: ph.enter_context(nc.sbuf_tensor(n, list(s), d))
                Pr = sb("Pr", [128, 32, 128], BF16); Pi = sb("Pi", [128, 32, 128], BF16); Qr = sb("Qr", [128, 32, 128], BF16); Qi = sb("Qi", [128, 32, 128], BF16)
                lbr = sb("lbr", [128, 32]); lbi = sb("lbi", [128, 32]); cr = sb("cr", [128, 32]); ci = sb("ci", [128, 32])
                wi_re = sb("wi_re", [128, 32]); wi_im = sb("wi_im", [128, 32]); L_re = sb("L_re", [128, 32]); L_im = sb("L_im", [128, 32])
                Bre = sb("Bre", [128, 32, 128], BF16); Bim = sb("Bim", [128, 32, 128], BF16); dskt = sb("dskt", [128, 8])
                with ExitStack() as ph2:
                    sb2 = lambda n, s, d=F32: ph2.enter_context(nc.sbuf_tensor(n, list(s), d))
                    lr = sb2("lr", [128, 32]); li = sb2("li", [128, 32]); dt = sb2("dt", [128, 32]); a_ = sb2("a_", [128, 32]); om = sb2("om", [128, 32])
                    sidt = sb2("sidt", [128, 128])
                    T1 = sb2("T1", [128, 32, 128]); T2 = sb2("T2", [128, 32, 128]); T3 = sb2("T3", [128, 32, 128]); T4 = sb2("T4", [128, 32, 128])
                    TI = sb2("TI", [128, 32, 128], I32); T5 = sb2("T5", [128, 32, 128]); T6 = sb2("T6", [128, 32, 128])
                    s_a = sb2("s_a", [128, 32]); s_b = sb2("s_b", [128, 32]); s_c = sb2("s_c", [128, 32]); s_d = sb2("s_d", [128, 32])
                    s_i = sb2("s_i", [128, 32], I32); sn = sb2("sn", [128, 32]); cs = sb2("cs", [128, 32]); ea = sb2("ea", [128, 32])
                    for (t_, s_, k_) in [(lr[:], lamT_re, "lr"), (li[:], lamT_im, "li"), (dt[:], ldtT, "dt"), (sidt[:], sidx, "sidt"), (T1[:], BreT, "BreF"),
                                         (T2[:], BimT, "BimF"), (dskt[:], dsk, "dskt")]:
                        P.dma(lambda q, t_=t_, s_=s_: q.dma_start(out=t_, in_=s_), writes=[k_], key="ld")
                    P.op("dve", lambda e: e.tensor_copy(out=Bre[:], in_=T1[:]), reads=["BreF"], writes=["Bre"])
                    P.op("dve", lambda e: e.tensor_copy(out=Bim[:], in_=T2[:]), reads=["BimF"], writes=["Bim"])
                    P.barrier()
                    P.op("act", lambda e: e.activation(out=dt[:], in_=dt[:], func=AF.Exp), reads=["dt"], writes=["dt"])
                    P.op("dve", lambda e: e.tensor_tensor(out=a_[:], in0=lr[:], in1=dt[:], op=ALU.mult), reads=["lr", "dt"], writes=["a_"])
                    P.op("dve", lambda e: e.tensor_tensor(out=om[:], in0=li[:], in1=dt[:], op=ALU.mult), reads=["li", "dt"], writes=["om"])

                    def sincos(th, n_shape, u_, ui_, f_, s2_, c2_, sin_o, cos_o, tg):
                        P.op("dve", lambda e: e.tensor_scalar(out=u_, in0=th, scalar1=1.0 / (2 * math.pi), scalar2=None, op0=ALU.mult), reads=[tg + "th"], writes=[tg + "u"])
                        P.op("dve", lambda e: e.tensor_copy(out=ui_, in_=u_), reads=[tg + "u"], writes=[tg + "ui"])
                        P.op("dve", lambda e: e.tensor_copy(out=f_, in_=ui_), reads=[tg + "ui"], writes=[tg + "f"])
                        P.op("dve", lambda e: e.tensor_tensor(out=f_, in0=u_, in1=f_, op=ALU.subtract), reads=[tg + "u", tg + "f"], writes=[tg + "f"])
                        P.op("act", lambda e: e.activation(out=s2_, in_=f_, func=AF.Sin, scale=math.pi), reads=[tg + "f"], writes=[tg + "s2"])
                        P.op("act", lambda e: e.activation(out=f_, in_=f_, func=AF.Abs), reads=[tg + "f", tg + "s2"], writes=[tg + "f"])
                        P.op("dve", lambda e: e.tensor_scalar(out=f_, in0=f_, scalar1=-math.pi, scalar2=math.pi / 2, op0=ALU.mult, op1=ALU.add), reads=[tg + "f"], writes=[tg + "f"])
                        P.op("act", lambda e: e.activation(out=c2_, in_=f_, func=AF.Sin), reads=[tg + "f"], writes=[tg + "c2"])
                        P.op("dve", lambda e: e.scalar_tensor_tensor(out=sin_o, in0=s2_, scalar=2.0, in1=c2_, op0=ALU.mult, op1=ALU.mult),
                             reads=[tg + "s2", tg + "c2"], writes=[tg + "sin"])
                        P.op("dve", lambda e: e.tensor_tensor(out=c2_, in0=c2_, in1=c2_, op=ALU.mult), reads=[tg + "c2"], writes=[tg + "c2"])
                        P.op("dve", lambda e: e.tensor_tensor(out=s2_, in0=s2_, in1=s2_, op=ALU.mult), reads=[tg + "s2"], writes=[tg + "s2"])
                        P.op("dve", lambda e: e.tensor_tensor(out=cos_o, in0=c2_, in1=s2_, op=ALU.subtract), reads=[tg + "c2", tg + "s2"], writes=[tg + "cos"])

                    P.op("dve", lambda e: e.tensor_copy(out=s_a[:], in_=om[:]), reads=["om"], writes=["bth"])
                    sincos(s_a[:], None, s_b[:], s_i[:], s_c[:], s_d[:], cs[:], sn[:], cs[:], "b")
                    P.op("act", lambda e: e.activation(out=ea[:], in_=a_[:], func=AF.Exp), reads=["a_"], writes=["ea"])
                    P.op("dve", lambda e: e.tensor_tensor(out=lbr[:], in0=ea[:], in1=cs[:], op=ALU.mult), reads=["ea", "bcos"], writes=["lbr"])
                    P.op("dve", lambda e: e.tensor_tensor(out=lbi[:], in0=ea[:], in1=sn[:], op=ALU.mult), reads=["ea", "bsin"], writes=["lbi"])
                    P.op("dve", lambda e: e.tensor_scalar(out=s_a[:], in0=lbr[:], scalar1=-1.0, scalar2=None, op0=ALU.add), reads=["lbr"], writes=["s_a"])
                    P.op("dve", lambda e: e.tensor_tensor(out=s_b[:], in0=lr[:], in1=lr[:], op=ALU.mult), reads=["lr"], writes=["s_b"])
                    P.op("dve", lambda e: e.tensor_tensor(out=s_c[:], in0=li[:], in1=li[:], op=ALU.mult), reads=["li"], writes=["s_c"])
                    P.op("dve", lambda e: e.tensor_tensor(out=s_b[:], in0=s_b[:], in1=s_c[:], op=ALU.add), reads=["s_b", "s_c"], writes=["s_b"])
                    P.op("dve", lambda e: e.reciprocal(out=s_b[:], in_=s_b[:]), reads=["s_b"], writes=["s_b"])
                    P.op("dve", lambda e: e.tensor_tensor(out=s_c[:], in0=s_a[:], in1=lr[:], op=ALU.mult), reads=["s_a", "lr"], writes=["s_c"])
                    P.op("dve", lambda e: e.tensor_tensor(out=s_d[:], in0=lbi[:], in1=li[:], op=ALU.mult), reads=["lbi", "li"], writes=["s_d"])
                    P.op("dve", lambda e: e.tensor_tensor(out=s_c[:], in0=s_c[:], in1=s_d[:], op=ALU.add), reads=["s_c", "s_d"], writes=["s_c"])
                    P.op("dve", lambda e: e.tensor_tensor(out=cr[:], in0=s_c[:], in1=s_b[:], op=ALU.mult), reads=["s_c", "s_b"], writes=["cr"])
                    P.op("dve", lambda e: e.tensor_tensor(out=s_c[:], in0=lbi[:], in1=lr[:], op=ALU.mult), reads=["lbi", "lr"], writes=["s_c"])
                    P.op("dve", lambda e: e.tensor_tensor(out=s_d[:], in0=s_a[:], in1=li[:], op=ALU.mult), reads=["s_a", "li"], writes=["s_d"])
                    P.op("dve", lambda e: e.tensor_tensor(out=s_c[:], in0=s_c[:], in1=s_d[:], op=ALU.subtract), reads=["s_c", "s_d"], writes=["s_c"])
                    P.op("dve", lambda e: e.tensor_tensor(out=ci[:], in0=s_c[:], in1=s_b[:], op=ALU.mult), reads=["s_c", "s_b"], writes=["ci"])
                    om_b = om[:].unsqueeze(2).broadcast_to([128, 32, 128]); a_b = a_[:].unsqueeze(2).broadcast_to([128, 32, 128])
                    s_bb = sidt[:].unsqueeze(1).broadcast_to([128, 32, 128])
                    P.op("dve", lambda e: e.tensor_tensor(out=T1[:], in0=om_b, in1=s_bb, op=ALU.mult), reads=["om", "sidt"], writes=["tth"])
                    sincos(T1[:], None, T2[:], TI[:], T3[:], T4[:], T5[:], T6[:], T5[:], "t")
                    P.barrier()
                    P.op("dve", lambda e: e.tensor_tensor(out=T1[:], in0=a_b, in1=s_bb, op=ALU.mult), reads=["a_", "sidt"], writes=["T1as"])
                    P.op("act", lambda e: e.activation(out=T2[:], in_=T1[:], func=AF.Exp), reads=["T1as"], writes=["Ep"])
                    P.op("act", lambda e: e.activation(out=T3[:], in_=T1[:], func=AF.Exp, scale=-1.0), reads=["T1as"], writes=["Em"])
                    P.op("dve", lambda e: e.tensor_tensor(out=Qr[:], in0=T2[:], in1=T5[:], op=ALU.mult), reads=["Ep"], writes=["Qr"])
                    P.op("dve", lambda e: e.tensor_tensor(out=Qi[:], in0=T2[:], in1=T6[:], op=ALU.mult), reads=["Ep"], writes=["Qi"])
                    P.op("dve", lambda e: e.tensor_tensor(out=Pr[:], in0=T3[:], in1=T5[:], op=ALU.mult), reads=["Em"], writes=["Pr"])
                    P.op("dve", lambda e: e.scalar_tensor_tensor(out=Pi[:], in0=T3[:], scalar=-1.0, in1=T6[:], op0=ALU.mult, op1=ALU.mult),
                         reads=["Em"], writes=["Pi"])
                    P.barrier()
                    P.op("dve", lambda e: e.tensor_tensor(out=s_a[:], in0=T2[:, :, 127], in1=T5[:, :, 127], op=ALU.mult), reads=[], writes=["q127"])
                    P.op("dve", lambda e: e.tensor_tensor(out=s_b[:], in0=T2[:, :, 127], in1=T6[:, :, 127], op=ALU.mult), reads=["q127"], writes=["q127"])
                    P.op("dve", lambda e: e.tensor_tensor(out=s_c[:], in0=s_a[:], in1=lbr[:], op=ALU.mult), reads=["q127"], writes=["q127"])
                    P.op("dve", lambda e: e.tensor_tensor(out=s_d[:], in0=s_b[:], in1=lbi[:], op=ALU.mult), reads=["q127"], writes=["q127"])
                    P.op("dve", lambda e: e.tensor_tensor(out=L_re[:], in0=s_c[:], in1=s_d[:], op=ALU.subtract), reads=["q127"], writes=["L_re"])
                    P.op("dve", lambda e: e.tensor_tensor(out=s_c[:], in0=s_a[:], in1=lbi[:], op=ALU.mult), reads=["q127", "L_re"], writes=["q127"])
                    P.op("dve", lambda e: e.tensor_tensor(out=s_d[:], in0=s_b[:], in1=lbr[:], op=ALU.mult), reads=["q127"], writes=["q127"])
                    P.op("dve", lambda e: e.tensor_tensor(out=L_im[:], in0=s_c[:], in1=s_d[:], op=ALU.add), reads=["q127"], writes=["L_im"])
                    P.barrier()
                    for nm_, t_ in [("lbr", lbr), ("lbi", lbi), ("cr", cr), ("ci", ci), ("om", om), ("a_", a_), ("sn", sn), ("cs", cs)]:
                        dbg_dump(P, nm_, t_[:], [128, 32], F32, [])
                    for nm_, t_ in [("Pr", Pr), ("Pi", Pi), ("Qr", Qr), ("Qi", Qi)]:
                        dbg_dump(P, nm_, t_[:], [128, 32, 128], BF16, [])
                    for nm_, t_ in [("T5", T5), ("T6", T6), ("T2", T2), ("T3", T3)]:
                        dbg_dump(P, nm_, t_[:], [128, 32, 128], F32, [])
                    P.barrier()
                Ccr = sb("Ccr", [128, 32, 128]); Cci = sb("Cci", [128, 32, 128])
                with ExitStack() as ph2:
                    sb2 = lambda n, s, d=F32: ph2.enter_context(nc.sbuf_tensor(n, list(s), d))
                    Cr_ = sb2("Cr_", [128, 32, 128]); Ci_ = sb2("Ci_", [128, 32, 128]); Tm = sb2("Tm", [128, 32, 128])
                    P.dma(lambda q: q.dma_start(out=Cr_[:], in_=Cre), writes=["Cr_"], key="ld")
                    P.dma(lambda q: q.dma_start(out=Ci_[:], in_=Cim), writes=["Ci_"], key="ld")
                    cr_b = cr[:].unsqueeze(2).broadcast_to([128, 32, 128]); ci_b = ci[:].unsqueeze(2).broadcast_to([128, 32, 128])
                    P.op("dve", lambda e: e.tensor_tensor(out=Ccr[:], in0=Cr_[:], in1=cr_b, op=ALU.mult), reads=["Cr_", "cr"], writes=["Ccr"])
                    P.op("dve", lambda e: e.tensor_tensor(out=Tm[:], in0=Ci_[:], in1=ci_b, op=ALU.mult), reads=["Ci_", "ci"], writes=["Tm"])
                    P.op("dve", lambda e: e.tensor_tensor(out=Ccr[:], in0=Ccr[:], in1=Tm[:], op=ALU.subtract), reads=["Ccr", "Tm"], writes=["Ccr"])
                    P.op("dve", lambda e: e.tensor_tensor(out=Cci[:], in0=Cr_[:], in1=ci_b, op=ALU.mult), reads=["Cr_", "ci"], writes=["Cci"])
                    P.op("dve", lambda e: e.tensor_tensor(out=Tm[:], in0=Ci_[:], in1=cr_b, op=ALU.mult), reads=["Ci_", "cr", "Ccr"], writes=["Tm"])
                    P.op("dve", lambda e: e.scalar_tensor_tensor(out=Cci[:], in0=Cci[:], scalar=-1.0, in1=Tm[:], op0=ALU.mult, op1=ALU.subtract),
                         reads=["Cci", "Tm"], writes=["Cci"])
                    P.barrier()
                wu = sb("wu", [128, 16, 1024], BF16)
                with ExitStack() as ph2:
                    stg = [ph2.enter_context(nc.sbuf_tensor("stg%d" % i, [128, 1024], F32)) for i in range(2)]
                    load_w_bf16(wu, w_in[:, 0:1024], 16, 1024, stg, "wu")
                    P.barrier()
                hTg = [sb("hTg%d" % i, [128, 16, 512], BF16) for i in range(2)]
                uT = [sb("uT%d" % i, [128, 8, 512], BF16) for i in range(2)]
                v_re = [sb("v_re%d" % i, [128, 4, 128]) for i in range(2)]; v_im = [sb("v_im%d" % i, [128, 4, 128]) for i in range(2)]
                tt = [sb("tt%d" % i, [128, 4, 128]) for i in range(8)]
                w_re = [sb("w_re%d" % i, [128, 4, 128]) for i in range(2)]; w_im = [sb("w_im%d" % i, [128, 4, 128]) for i in range(2)]
                rr = [sb("rr%d" % i, [128, 4]) for i in range(4)]
                x_re = [sb("x_re%d" % i, [128, 4, 128]) for i in range(2)]; x_im = [sb("x_im%d" % i, [128, 4, 128]) for i in range(2)]
                yv = sb("yv", [128, 128]); ygT = sb("ygT", [128, 8, 128], BF16); c1 = sb("c1", [128, 4]); c2 = sb("c2", [128, 4])
                P.op("pool", lambda e: e.memset(wi_re[:], 0.0), writes=["wi_re"])
                P.op("pool", lambda e: e.memset(wi_im[:], 0.0), writes=["wi_im"])
                it = 0
                for g in range(16):
                    hT_ = hTg[g % 2]; uT_ = uT[g % 2]
                    P.dma(lambda q, hT_=hT_, g=g: q.dma_start(out=hT_[:], in_=hT_d[:, :, g * 512:(g + 1) * 512].rearrange("k p t -> p k t")),
                          reads=["hT_d"], writes=[("hTg", g % 2)], key="hld")
                    for fc in range(8):
                        pu = psum[fc % 2]
                        for kt in range(16):
                            P.op("pe", lambda e, pu=pu, kt=kt, fc=fc, hT_=hT_: e.matmul(pu[:], lhsT=wu[:, kt, fc * 128:(fc + 1) * 128], rhs=hT_[:, kt, :], start=(kt == 0), stop=(kt == 15)),
                                 reads=[("wu", kt), ("hTg", g % 2)], writes=["ps%d" % (fc % 2)])
                        P.op("act", lambda e, pu=pu, fc=fc, uT_=uT_: e.activation(out=uT_[:, fc, :], in_=pu[:], func=AF.Copy), reads=["ps%d" % (fc % 2)], writes=[("uT", g % 2, fc)])
                    for cc in range(4):
                        c = g * 4 + cc
                        tok = slice(cc * 128, (cc + 1) * 128)
                        for fc in range(8):
                            xr = x_re[it % 2]; xi = x_im[it % 2]; xk = ("x", it % 2); it += 1
                            pbr = psum[2]; pbi = psum[3]
                            for k4 in range(4):
                                st_ = fc * 4 + k4
                                P.op("pe", lambda e, k4=k4, st_=st_, fc=fc, uT_=uT_, tok=tok: e.matmul(pbr[:, k4 * 128:(k4 + 1) * 128], lhsT=Bre[:, st_, :], rhs=uT_[:, fc, tok], start=True, stop=True),
                                     reads=["Bre", ("uT", g % 2, fc)], writes=["ps2"])
                                P.op("pe", lambda e, k4=k4, st_=st_, fc=fc, uT_=uT_, tok=tok: e.matmul(pbi[:, k4 * 128:(k4 + 1) * 128], lhsT=Bim[:, st_, :], rhs=uT_[:, fc, tok], start=True, stop=True),
                                     reads=["Bim", ("uT", g % 2, fc)], writes=["ps3"])
                            st0 = fc * 4
                            pr_ = Pr[:, st0:st0 + 4, :]; pi_ = Pi[:, st0:st0 + 4, :]; qr_ = Qr[:, st0:st0 + 4, :]; qi_ = Qi[:, st0:st0 + 4, :]
                            br4 = pbr[:].rearrange("p (k t) -> p k t", k=4); bi4 = pbi[:].rearrange("p (k t) -> p k t", k=4)
                            b2 = (it - 1) % 2
                            vr = v_re[b2]; vi = v_im[b2]; wr = w_re[b2]; wim = w_im[b2]
                            ta, tb, tc, td, te, tf, tg_, th_ = tt
                            P.op("dve", lambda e, br4=br4, pr_=pr_: e.tensor_tensor(out=ta[:], in0=br4, in1=pr_, op=ALU.mult), reads=["ps2", "Pr"], writes=["ta"])
                            P.op("dve", lambda e, bi4=bi4, pi_=pi_: e.tensor_tensor(out=tb[:], in0=bi4, in1=pi_, op=ALU.mult), reads=["ps3", "Pi"], writes=["tb"])
                            P.op("pool", lambda e, vr=vr: e.tensor_tensor(out=vr[:], in0=ta[:], in1=tb[:], op=ALU.subtract), reads=["ta", "tb"], writes=[("v_re", b2)])
                            P.op("dve", lambda e, br4=br4, pi_=pi_: e.tensor_tensor(out=tc[:], in0=br4, in1=pi_, op=ALU.mult), reads=["ps2", "Pi"], writes=["tc"])
                            P.op("dve", lambda e, bi4=bi4, pr_=pr_: e.tensor_tensor(out=td[:], in0=bi4, in1=pr_, op=ALU.mult), reads=["ps3", "Pr"], writes=["td"])
                            P.op("pool", lambda e, vi=vi: e.tensor_tensor(out=vi[:], in0=tc[:], in1=td[:], op=ALU.add), reads=["tc", "td"], writes=[("v_im", b2)])
                            if c < OWN0:
                                r0_, r1_, r2_, r3_ = rr
                                Lr_s = L_re[:, st0:st0 + 4]; Li_s = L_im[:, st0:st0 + 4]
                                P.op("dve", lambda e, vr=vr: e.tensor_reduce(out=r0_[:], in_=vr[:], axis=AX.X, op=ALU.add), reads=[("v_re", b2)], writes=["r0"])
                                P.op("dve", lambda e, vi=vi: e.tensor_reduce(out=r1_[:], in_=vi[:], axis=AX.X, op=ALU.add), reads=[("v_im", b2)], writes=["r1"])
                                P.op("dve", lambda e, st0=st0: e.tensor_tensor(out=r0_[:], in0=r0_[:], in1=wi_re[:, st0:st0 + 4], op=ALU.add), reads=["r0", "wi_re"], writes=["r0"])
                                P.op("dve", lambda e, st0=st0: e.tensor_tensor(out=r1_[:], in0=r1_[:], in1=wi_im[:, st0:st0 + 4], op=ALU.add), reads=["r1", "wi_im"], writes=["r1"])
                                P.op("dve", lambda e, Lr_s=Lr_s: e.tensor_tensor(out=r2_[:], in0=r0_[:], in1=Lr_s, op=ALU.mult), reads=["r0", "L_re"], writes=["r2"])
                                P.op("dve", lambda e, Li_s=Li_s: e.tensor_tensor(out=r3_[:], in0=r1_[:], in1=Li_s, op=ALU.mult), reads=["r1", "L_im"], writes=["r3"])
                                P.op("dve", lambda e, st0=st0: e.tensor_tensor(out=wi_re[:, st0:st0 + 4], in0=r2_[:], in1=r3_[:], op=ALU.subtract), reads=["r2", "r3"], writes=["wi_re"])
                                P.op("dve", lambda e, Li_s=Li_s: e.tensor_tensor(out=r2_[:], in0=r0_[:], in1=Li_s, op=ALU.mult), reads=["r0", "L_im"], writes=["r2"])
                                P.op("dve", lambda e, Lr_s=Lr_s: e.tensor_tensor(out=r3_[:], in0=r1_[:], in1=Lr_s, op=ALU.mult), reads=["r1", "L_re"], writes=["r3"])
                                P.op("dve", lambda e, st0=st0: e.tensor_tensor(out=wi_im[:, st0:st0 + 4], in0=r2_[:], in1=r3_[:], op=ALU.add), reads=["r2", "r3"], writes=["wi_im"])
                                continue
                            for k4 in range(4):
                                st_ = st0 + k4
                                P.op("dve", lambda e, k4=k4, st_=st_, vr=vr, wr=wr: e.tensor_tensor_scan(out=wr[:, k4, :], data0=ones[:, 0:128], data1=vr[:, k4, :], initial=wi_re[:, st_:st_ + 1], op0=ALU.mult, op1=ALU.add),
                                     reads=[("v_re", b2), "ones", "wi_re"], writes=[("w_re", b2)])
                                P.op("dve", lambda e, k4=k4, st_=st_, vi=vi, wim=wim: e.tensor_tensor_scan(out=wim[:, k4, :], data0=ones[:, 0:128], data1=vi[:, k4, :], initial=wi_im[:, st_:st_ + 1], op0=ALU.mult, op1=ALU.add),
                                     reads=[("v_im", b2), "ones", "wi_im"], writes=[("w_im", b2)])
                            P.op("pool", lambda e, qr_=qr_, wr=wr: e.tensor_tensor(out=te[:], in0=wr[:], in1=qr_, op=ALU.mult), reads=[("w_re", b2), "Qr"], writes=["te"])
                            P.op("pool", lambda e, qi_=qi_, wim=wim: e.tensor_tensor(out=tf[:], in0=wim[:], in1=qi_, op=ALU.mult), reads=[("w_im", b2), "Qi"], writes=["tf"])
                            P.op("pool", lambda e, xr=xr: e.tensor_tensor(out=xr[:], in0=te[:], in1=tf[:], op=ALU.subtract), reads=["te", "tf"], writes=[xk])
                            P.op("dve", lambda e, qi_=qi_, wr=wr: e.tensor_tensor(out=tg_[:], in0=wr[:], in1=qi_, op=ALU.mult), reads=[("w_re", b2), "Qi"], writes=["tg"])
                            P.op("dve", lambda e, qr_=qr_, wim=wim: e.tensor_tensor(out=th_[:], in0=wim[:], in1=qr_, op=ALU.mult), reads=[("w_im", b2), "Qr"], writes=["th"])
                            P.op("pool", lambda e, xi=xi: e.tensor_tensor(out=xi[:], in0=tg_[:], in1=th_[:], op=ALU.add), reads=["tg", "th"], writes=[xk])
                            xr_l = xr[:, :, 127]; xi_l = xi[:, :, 127]
                            lr_s = lbr[:, st0:st0 + 4]; li_s = lbi[:, st0:st0 + 4]
                            P.op("dve", lambda e, xr_l=xr_l, lr_s=lr_s: e.tensor_tensor(out=c1[:], in0=xr_l, in1=lr_s, op=ALU.mult), reads=[xk, "lbr"], writes=["c1"])
                            P.op("dve", lambda e, xi_l=xi_l, li_s=li_s: e.tensor_tensor(out=c2[:], in0=xi_l, in1=li_s, op=ALU.mult), reads=[xk, "lbi"], writes=["c2"])
                            P.op("dve", lambda e, st0=st0: e.tensor_tensor(out=wi_re[:, st0:st0 + 4], in0=c1[:], in1=c2[:], op=ALU.subtract), reads=["c1", "c2"], writes=["wi_re"])
                            P.op("dve", lambda e, xr_l=xr_l, li_s=li_s: e.tensor_tensor(out=c1[:], in0=xr_l, in1=li_s, op=ALU.mult), reads=[xk, "lbi"], writes=["c1"])
                            P.op("dve", lambda e, xi_l=xi_l, lr_s=lr_s: e.tensor_tensor(out=c2[:], in0=xi_l, in1=lr_s, op=ALU.mult), reads=[xk, "lbr"], writes=["c2"])
                            P.op("dve", lambda e, st0=st0: e.tensor_tensor(out=wi_im[:, st0:st0 + 4], in0=c1[:], in1=c2[:], op=ALU.add), reads=["c1", "c2"], writes=["wi_im"])
                            if c >= OWN0:
                                py = psum[4]
                                for k4 in range(4):
                                    st_ = st0 + k4
                                    P.op("pe", lambda e, k4=k4, st_=st_, xr=xr: e.matmul(py[:, 0:128], lhsT=Ccr[:, st_, :], rhs=xr[:, k4, :], start=(k4 == 0), stop=False),
                                         reads=["Ccr", xk], writes=["ps4"])
                                    P.op("pe", lambda e, k4=k4, st_=st_, xi=xi: e.matmul(py[:, 0:128], lhsT=Cci[:, st_, :], rhs=xi[:, k4, :], start=False, stop=(k4 == 3)),
                                         reads=["Cci", xk], writes=["ps4"])
                                P.op("dve", lambda e, fc=fc, uT_=uT_, tok=tok: e.scalar_tensor_tensor(out=yv[:], in0=uT_[:, fc, tok], scalar=dskt[:, fc:fc + 1], in1=py[:, 0:128], op0=ALU.mult, op1=ALU.add),
                                     reads=[("uT", g % 2, fc), "dskt", "ps4"], writes=["yv"])
                                P.op("act", lambda e, fc=fc: e.activation(out=ygT[:, fc, :], in_=yv[:], func=AF.Gelu_apprx_tanh), reads=["yv"], writes=["ygT"])
                        if c >= OWN0:
                            o0 = (c - OWN0) * 128
                            P.dma(lambda q, o0=o0: q.dma_start(out=ygT_d[:, :, o0:o0 + 128].rearrange("k p t -> p k t"), in_=ygT[:]),
                                  reads=["ygT"], writes=["ygT_d"], key="st")
            P.barrier()
            stage_end(3)

            with ExitStack() as ph:
                sb = lambda n, s, d=F32: ph.enter_context(nc.sbuf_tensor(n, list(s), d))
                wk = sb("wk", [128, 16, 1024], BF16)
                stg = [sb("stgk%d" % i, [128, 1024]) for i in range(2)]
                hTg = [sb("hTk%d" % i, [128, 16, 512], BF16) for i in range(2)]
                gbc = sb("gbc", [128, 128]); junk = sb("junkk", [128, 128]); ssk = sb("ssk", [128, 8]); rsk = sb("rsk", [128, 8])
                kn = sb("kn", [128, 1024], BF16); kTt = [sb("kTt%d" % i, [128, 8, 512], BF16) for i in range(2)]
                vsb = [sb("vsb%d" % i, [128, 1024], BF16) for i in range(2)]
                for pas in ("k", "v", "q"):
                    col0 = {"k": 2048, "v": 3072, "q": 1024}[pas]
                    P.barrier()
                    load_w_bf16(wk, w_in[:, col0:col0 + 1024], 16, 1024, stg, "wk")
                    if pas in ("k", "q"):
                        gsrc = kg if pas == "k" else qg
                        P.dma(lambda q, gsrc=gsrc: q.dma_start(out=gbc[:], in_=gsrc.broadcast_to([128, 128])), writes=["gbc"], key="ld")
                        if pas == "q":
                            P.op("dve", lambda e: e.tensor_scalar(out=gbc[:], in0=gbc[:], scalar1=128.0 ** -0.5, scalar2=None, op0=ALU.mult), reads=["gbc"], writes=["gbc"])
                    g_lo = 12 if pas == "q" else 0
                    for g in range(g_lo, 16):
                        hT_ = hTg[g % 2]
                        P.dma(lambda q, hT_=hT_, g=g: q.dma_start(out=hT_[:], in_=hT_d[:, :, g * 512:(g + 1) * 512].rearrange("k p t -> p k t")),
                              reads=["hT_d"], writes=[("hTk", g % 2)], key="hld")
                        for cc in range(4):
                            c = g * 4 + cc
                            for nb in range(2):
                                pp = psum[nb]
                                for kt in range(16):
                                    P.op("pe", lambda e, pp=pp, kt=kt, nb=nb, hT_=hT_, cc=cc: e.matmul(pp[:], lhsT=hT_[:, kt, cc * 128:(cc + 1) * 128], rhs=wk[:, kt, nb * 512:(nb + 1) * 512], start=(kt == 0), stop=(kt == 15)),
                                         reads=[("hTk", g % 2), ("wk", kt)], writes=["ps%d" % nb])
                            if pas == "v":
                                v_ = vsb[c % 2]
                                P.op("act", lambda e, v_=v_: e.activation(out=v_[:, 0:512], in_=psum[0][:], func=AF.Copy), reads=["ps0"], writes=[("vsb", c % 2)])
                                P.op("dve", lambda e, v_=v_: e.tensor_copy(out=v_[:, 512:1024], in_=psum[1][:]), reads=["ps1"], writes=[("vsb", c % 2)])
                                P.dma(lambda q, v_=v_, c=c: q.dma_start(out=v_d[c * 128:(c + 1) * 128, :], in_=v_[:]), reads=[("vsb", c % 2)], writes=["v_d"], key="st")
                                continue
                            for h in range(8):
                                pp = psum[h // 4]; hs = slice((h % 4) * 128, (h % 4 + 1) * 128)
                                P.op("act", lambda e, pp=pp, hs=hs, h=h: e.activation(out=junk[:], in_=pp[:, hs], func=AF.Square, accum_out=ssk[:, h:h + 1]),
                                     reads=["ps%d" % (h // 4)], writes=["junkk", "ssk"])
                            P.op("dve", lambda e: e.tensor_scalar(out=ssk[:], in0=ssk[:], scalar1=1.0 / 128, scalar2=EPS, op0=ALU.mult, op1=ALU.add), reads=["ssk"], writes=["ssk"])
                            P.op("act", lambda e: e.activation(out=ssk[:], in_=ssk[:], func=AF.Sqrt), reads=["ssk"], writes=["ssk"])
                            P.op("dve", lambda e: e.reciprocal(out=rsk[:], in_=ssk[:]), reads=["ssk"], writes=["rsk"])
                            for h in range(8):
                                pp = psum[h // 4]; hs = slice((h % 4) * 128, (h % 4 + 1) * 128)
                                P.op("dve", lambda e, pp=pp, hs=hs, h=h: e.scalar_tensor_tensor(out=kn[:, h * 128:(h + 1) * 128], in0=pp[:, hs], scalar=rsk[:, h:h + 1], in1=gbc[:], op0=ALU.mult, op1=ALU.mult),
                                     reads=["ps%d" % (h // 4), "rsk", "gbc"], writes=["kn"])
                            for h in range(8):
                                P.op("pe", lambda e, h=h: e.transpose(psb[0][:, h * 128:(h + 1) * 128], in_=kn[:, h * 128:(h + 1) * 128], identity=identb[:]),
                                     reads=["kn", "identb"], writes=["psb0"])
                            kT_ = kTt[g % 2]
                            P.op("act", lambda e, kT_=kT_, cc=cc: e.activation(out=kT_[:, :, cc * 128:(cc + 1) * 128], in_=psb[0][:].rearrange("p (h t) -> p h t", h=8), func=AF.Copy),
                                 reads=["psb0"], writes=[("kTt", g % 2)])
                        if pas == "k":
                            P.dma(lambda q, g=g: q.dma_start(out=kT_d[:, :, g * 512:(g + 1) * 512].rearrange("h p t -> p h t"), in_=kTt[g % 2][:]),
                                  reads=[("kTt", g % 2)], writes=["kT_d"], key="st")
                        elif pas == "q":
                            o0 = (g - 12) * 512
                            P.dma(lambda q, g=g, o0=o0: q.dma_start(out=qT_d[:, :, o0:o0 + 512].rearrange("h p t -> p h t"), in_=kTt[g % 2][:]),
                                  reads=[("kTt", g % 2)], writes=["qT_d"], key="st")
            P.barrier()
            stage_end(4)

            with ExitStack() as ph:
                sb = lambda n, s, d=F32: ph.enter_context(nc.sbuf_tensor(n, list(s), d))
                kTh = [sb("kTh%d" % i, [128, SEQ], BF16) for i in range(2)]
                vh = [sb("vh%d" % i, [128, NT, 128], BF16) for i in range(2)]
                qTh = [sb("qTh%d" % i, [128, 2048], BF16) for i in range(2)]
                lw0 = sb("lw0", [128, SEQ]); ee = [sb("ee%d" % i, [128, 512]) for i in range(2)]; sp = [sb("sp%d" % i, [128, 512]) for i in range(2)]
                pref = [sb("pref%d" % i, [128, 512]) for i in range(2)]; zs = [sb("zs%d" % i, [128, 512]) for i in range(2)]
                wb = [sb("wb%d" % i, [128, 512], BF16) for i in range(2)]; wTs = [sb("wTs%d" % i, [128, 4, 128], BF16) for i in range(2)]
                negT = sb("negT", [128, 1]); attT = [sb("attT%d" % i, [128, 2048], BF16) for i in range(2)]
                zero1 = sb("zero1", [128, 1]); nTt = sb("nTt", [128, 1])
                P.op("pool", lambda e: e.memset(zero1[:], 0.0), writes=["zero1"])
                pidx = 0
                for h in range(8):
                    kT_ = kTh[h % 2]; v_ = vh[h % 2]; q_ = qTh[h % 2]; at_ = attT[h % 2]
                    P.dma(lambda q, kT_=kT_, h=h: q.dma_start(out=kT_[:], in_=kT_d[h]), reads=["kT_d"], writes=[("kTh", h % 2)], key="ald")
                    P.dma(lambda q, v_=v_, h=h: q.dma_start(out=v_[:], in_=v_d[:, h * 128:(h + 1) * 128].rearrange("(c p) d -> p c d", p=128)),
                          reads=["v_d"], writes=[("vh", h % 2)], key="ald")
                    P.dma(lambda q, q_=q_, h=h: q.dma_start(out=q_[:], in_=qT_d[h]), reads=["qT_d"], writes=[("qTh", h % 2)], key="ald")
                    for j in range(16):
                        ntile = OWN0 + j + 1
                        pieces = []
                        t0 = 0
                        while t0 < ntile:
                            n = min(4, ntile - t0); pieces.append((t0, n)); t0 += n
                        for pi_, (t0, n) in enumerate(pieces):
                            W = n * 128; ks = slice(t0 * 128, t0 * 128 + W)
                            pz = psum[pidx % 2]; pzk = "ps%d" % (pidx % 2); b_ = pidx % 2; pidx += 1
                            ee_ = ee[b_]; sp_ = sp[b_]; pref_ = pref[b_]; zs_ = zs[b_]
                            P.op("pe", lambda e, pz=pz, W=W, ks=ks, q_=q_, kT_=kT_, j=j: e.matmul(pz[:, 0:W], lhsT=q_[:, j * 128:(j + 1) * 128], rhs=kT_[:, ks], start=True, stop=True),
                                 reads=[("qTh", h % 2), ("kTh", h % 2)], writes=[pzk])
                            P.op("act", lambda e, pz=pz, W=W, ee_=ee_: e.activation(out=ee_[:, 0:W], in_=pz[:, 0:W], func=AF.Exp), reads=[pzk], writes=[("ee", b_)])
                            P.op("act", lambda e, W=W, ee_=ee_, sp_=sp_: e.activation(out=sp_[:, 0:W], in_=ee_[:, 0:W], func=AF.Ln, bias=1.0), reads=[("ee", b_)], writes=[("sp", b_)])
                            last = (pi_ == len(pieces) - 1)
                            if last:
                                P.op("pool", lambda e, W=W, sp_=sp_: e.tensor_tensor(out=sp_[:, W - 128:W], in0=sp_[:, W - 128:W], in1=cmask[:], op=ALU.mult), reads=[("sp", b_), "cmask"], writes=[("sp", b_)])
                            init = zero1[:, 0:1] if pi_ == 0 else pref[1 - b_][:, 511:512]
                            P.op("dve", lambda e, W=W, init=init, sp_=sp_, pref_=pref_: e.tensor_tensor_scan(out=pref_[:, 0:W], data0=ones[:, 0:W], data1=sp_[:, 0:W], initial=init, op0=ALU.mult, op1=ALU.add),
                                 reads=[("sp", b_), "ones", "zero1", ("pref", 1 - b_)], writes=[("pref", b_)])
                            P.op("dve", lambda e, pz=pz, W=W, sp_=sp_, zs_=zs_: e.tensor_tensor(out=zs_[:, 0:W], in0=pz[:, 0:W], in1=sp_[:, 0:W], op=ALU.subtract), reads=[pzk, ("sp", b_)], writes=[("zs", b_)])
                            P.op("pool", lambda e, W=W, ks=ks, zs_=zs_, pref_=pref_: e.tensor_tensor(out=lw0[:, ks], in0=zs_[:, 0:W], in1=pref_[:, 0:W], op=ALU.add), reads=[("zs", b_), ("pref", b_)], writes=["lw0"])
                            if last:
                                P.op("pool", lambda e, W=W, pref_=pref_: e.tensor_scalar(out=nTt[:], in0=pref_[:, W - 1:W], scalar1=-1.0, scalar2=None, op0=ALU.mult), reads=[("pref", b_)], writes=["negTT"])
                        pa = psum[4]
                        for pi_, (t0, n) in enumerate(pieces):
                            W = n * 128; ks = slice(t0 * 128, t0 * 128 + W)
                            w_ = wb[pi_ % 2]; wT_ = wTs[pi_ % 2]
                            P.op("act", lambda e, W=W, ks=ks, w_=w_: e.activation(out=w_[:, 0:W], in_=lw0[:, ks], func=AF.Exp, bias=nTt[:, 0:1]), reads=["lw0", "negTT"], writes=[("wb", pi_ % 2)])
                            if pi_ == len(pieces) - 1:
                                P.op("pool", lambda e, W=W, w_=w_: e.tensor_tensor(out=w_[:, W - 128:W], in0=w_[:, W - 128:W], in1=cmask[:], op=ALU.mult), reads=[("wb", pi_ % 2), "cmask"], writes=[("wb", pi_ % 2)])
                            for i4 in range(n):
                                P.op("pe", lambda e, i4=i4, w_=w_: e.transpose(psb[1][:, i4 * 128:(i4 + 1) * 128], in_=w_[:, i4 * 128:(i4 + 1) * 128], identity=identb[:]),
                                     reads=[("wb", pi_ % 2), "identb"], writes=["psb1"])
                            P.op("dve", lambda e, n=n, wT_=wT_: e.tensor_copy(out=wT_[:, 0:n, :], in_=psb[1][:, 0:n * 128].rearrange("p (k t) -> p k t", k=n)), reads=["psb1"], writes=[("wTs", pi_ % 2)])
                            for i4 in range(n):
                                tl = t0 + i4
                                P.op("pe", lambda e, i4=i4, tl=tl, v_=v_, wT_=wT_, ntile=ntile: e.matmul(pa[:, 0:128], lhsT=v_[:, tl, :], rhs=wT_[:, i4, :], start=(tl == 0), stop=(tl == ntile - 1)),
                                     reads=[("vh", h % 2), ("wTs", pi_ % 2)], writes=["ps4"])
                        P.op("act", lambda e, at_=at_, j=j: e.activation(out=at_[:, j * 128:(j + 1) * 128], in_=pa[:, 0:128], func=AF.Copy), reads=["ps4"], writes=[("attT", h % 2)])
                    P.dma(lambda q, at_=at_, h=h: q.dma_start(out=attT_d[h], in_=at_[:]), reads=[("attT", h % 2)], writes=["attT_d"], key="st")
            P.barrier()
            stage_end(5)

            with ExitStack() as ph:
                sb = lambda n, s, d=F32: ph.enter_context(nc.sbuf_tensor(n, list(s), d))
                gt1 = sb("gt1", [128, D]); gs2 = sb("gs2", [128, D]); sh2 = sb("sh2", [128, D]); junk = sb("junkb", [128, D]); g2 = junk
                bc_row(gt1[:], mod_d[0:1, 2 * D:3 * D], D, "gt1")
                bc_row(sh2[:], mod_d[0:1, 3 * D:4 * D], D, "sh2")
                bc_row(gs2[:], mod_d[0:1, 4 * D:5 * D], D, "gs2")
                P.dma(lambda q: q.dma_start(out=g2[:], in_=norm2_g.broadcast_to([128, D])), writes=["junkB"], key="ld")
                P.op("dve", lambda e: e.scalar_tensor_tensor(out=gs2[:], in0=gs2[:], scalar=1.0, in1=g2[:], op0=ALU.add, op1=ALU.mult), reads=["gs2", "junkB"], writes=["gs2"])
                hTo = sb("hTo", [128, 16, 512], BF16); ygo = sb("ygo", [128, 8, 512], BF16); ato = sb("ato", [128, 8, 512], BF16)
                mT = sb("mT", [128, 16, 512], BF16)
                stg = [sb("stgb%d" % i, [128, 512]) for i in range(2)]
                wgs = sb("wgs", [128, 16, 128], BF16); wga = sb("wga", [128, 16, 128], BF16)
                wgv = sb("wgv", [128, 8, 128], BF16); wgg = sb("wgg", [128, 8, 128], BF16); wau = sb("wau", [128, 8, 128], BF16)
                wo = sb("wo", [128, 16, 512], BF16)
                sgs = sb("sgs", [128, 512]); sga = sb("sga", [128, 512]); syg = sb("syg", [128, 512]); tm1 = sb("tm1", [128, 512]); tm2 = sb("tm2", [128, 512])
                xall = sb("xall", [128, 4, D])
                tA = sb("tAb", [128, D]); hb2 = sb("hb2", [128, D], BF16)
                ss = sb("ssb", [128, 1]); rstd = sb("rstdb", [128, 1]); h2t = sb("h2t", [128, 16, 512], BF16)
                for tg in range(4):
                    o0 = tg * 512; wt0 = (OWN0 * 128) + o0
                    P.dma(lambda q, wt0=wt0: q.dma_start(out=hTo[:], in_=hT_d[:, :, wt0:wt0 + 512].rearrange("k p t -> p k t")), reads=["hT_d"], writes=["hTo"], key="ld")
                    P.dma(lambda q, o0=o0: q.dma_start(out=ygo[:], in_=ygT_d[:, :, o0:o0 + 512].rearrange("k p t -> p k t")), reads=["ygT_d"], writes=["ygo"], key="ld")
                    P.dma(lambda q, o0=o0: q.dma_start(out=ato[:], in_=attT_d[:, :, o0:o0 + 512].rearrange("k p t -> p k t")), reads=["attT_d"], writes=["ato"], key="ld")
                    for dc in range(16):
                        cs_ = slice(dc * 128, (dc + 1) * 128)
                        load_w_bf16(wgs, w_in[:, 4096 + dc * 128:4096 + (dc + 1) * 128], 16, 128, stg, "wgs")
                        load_w_bf16(wga, w_in[:, 6144 + dc * 128:6144 + (dc + 1) * 128], 16, 128, stg, "wga")
                        load_w_bf16(wgv, w_glu[:, dc * 128:(dc + 1) * 128], 8, 128, stg, "wgv")
                        load_w_bf16(wgg, w_glu[:, 2048 + dc * 128:2048 + (dc + 1) * 128], 8, 128, stg, "wgg")
                        load_w_bf16(wau, w_up[:, dc * 128:(dc + 1) * 128], 8, 128, stg, "wau")
                        for kt in range(16):
                            P.op("pe", lambda e, kt=kt: e.matmul(psum[0][:], lhsT=wgs[:, kt, :], rhs=hTo[:, kt, :], start=(kt == 0), stop=(kt == 15)), reads=[("wgs", kt), "hTo"], writes=["ps0"])
                        for kt in range(16):
                            P.op("pe", lambda e, kt=kt: e.matmul(psum[1][:], lhsT=wga[:, kt, :], rhs=hTo[:, kt, :], start=(kt == 0), stop=(kt == 15)), reads=[("wga", kt), "hTo"], writes=["ps1"])
                        for kt in range(8):
                            P.op("pe", lambda e, kt=kt: e.matmul(psum[2][:], lhsT=wgv[:, kt, :], rhs=ygo[:, kt, :], start=(kt == 0), stop=(kt == 7)), reads=[("wgv", kt), "ygo"], writes=["ps2"])
                        for kt in range(8):
                            P.op("pe", lambda e, kt=kt: e.matmul(psum[3][:], lhsT=wgg[:, kt, :], rhs=ygo[:, kt, :], start=(kt == 0), stop=(kt == 7)), reads=[("wgg", kt), "ygo"], writes=["ps3"])
                        for kt in range(8):
                            P.op("pe", lambda e, kt=kt: e.matmul(psum[4][:], lhsT=wau[:, kt, :], rhs=ato[:, kt, :], start=(kt == 0), stop=(kt == 7)), reads=[("wau", kt), "ato"], writes=["ps4"])
                        P.op("act", lambda e: e.activation(out=sgs[:], in_=psum[0][:], func=AF.Sigmoid), reads=["ps0"], writes=["sgs"])
                        P.op("act", lambda e: e.activation(out=sga[:], in_=psum[1][:], func=AF.Sigmoid), reads=["ps1"], writes=["sga"])
                        P.op("act", lambda e: e.activation(out=syg[:], in_=psum[3][:], func=AF.Sigmoid), reads=["ps3"], writes=["syg"])
                        P.op("dve", lambda e: e.tensor_tensor(out=tm1[:], in0=psum[2][:], in1=syg[:], op=ALU.mult), reads=["ps2", "syg"], writes=["tm1"])
                        P.op("pool", lambda e: e.tensor_tensor(out=tm1[:], in0=tm1[:], in1=sgs[:], op=ALU.mult), reads=["tm1", "sgs"], writes=["tm1"])
                        P.op("dve", lambda e: e.tensor_tensor(out=tm2[:], in0=psum[4][:], in1=sga[:], op=ALU.mult), reads=["ps4", "sga"], writes=["tm2"])
                        P.op("pool", lambda e, dc=dc: e.tensor_tensor(out=mT[:, dc, :], in0=tm1[:], in1=tm2[:], op=ALU.add), reads=["tm1", "tm2"], writes=[("mT", dc)])
                    for cc in range(4):
                        r0 = (OWN0 + tg * 4 + cc) * 128
                        P.dma(lambda q, cc=cc, r0=r0: q.dma_start(out=xall[:, cc, :], in_=xw[r0:r0 + 128, :]), writes=[("xall", cc)], key="xld")
                    for nb in range(4):
                        load_w_bf16(wo, w_out[:, nb * 512:(nb + 1) * 512], 16, 512, stg, "wo")
                        for cc in range(4):
                            pp = psum[(nb * 4 + cc) % 2]; ppk = "ps%d" % ((nb * 4 + cc) % 2)
                            for dc in range(16):
                                P.op("pe", lambda e, pp=pp, dc=dc, cc=cc: e.matmul(pp[:], lhsT=mT[:, dc, cc * 128:(cc + 1) * 128], rhs=wo[:, dc, :], start=(dc == 0), stop=(dc == 15)),
                                     reads=[("mT", dc), ("wo", dc)], writes=[ppk])
                            P.op("dve", lambda e, pp=pp, nb=nb: e.tensor_tensor(out=tm1[:], in0=pp[:], in1=gt1[:, nb * 512:(nb + 1) * 512], op=ALU.mult),
                                 reads=[ppk, "gt1"], writes=["tm1"])
                            P.op("pool", lambda e, nb=nb, cc=cc: e.tensor_tensor(out=xall[:, cc, nb * 512:(nb + 1) * 512], in0=xall[:, cc, nb * 512:(nb + 1) * 512], in1=tm1[:], op=ALU.add),
                                 reads=["tm1", ("xall", cc)], writes=[("xall", cc)])
                    for cc in range(4):
                        tl = tg * 4 + cc
                        x1_ = xall[:, cc, :]
                        P.dma(lambda q, x1_=x1_, tl=tl: q.dma_start(out=out[tl * 128:(tl + 1) * 128, :], in_=x1_), reads=[("xall", cc)], writes=["out"], key="st")
                        rms_stats(x1_, ("xall", cc), junk[:], ss[:], rstd[:], "B")
                        P.op("dve", lambda e, x1_=x1_: e.scalar_tensor_tensor(out=tA[:], in0=x1_, scalar=rstd[:, 0:1], in1=gs2[:], op0=ALU.mult, op1=ALU.mult),
                             reads=[("xall", cc), "rstdB", "gs2"], writes=["tAb"])
                        P.op("pool", lambda e: e.tensor_tensor(out=hb2[:], in0=tA[:], in1=sh2[:], op=ALU.add), reads=["tAb", "sh2"], writes=["hb2"])
                        for half in range(2):
                            pb = psb[half]
                            for k8 in range(8):
                                kt = half * 8 + k8
                                P.op("pe", lambda e, pb=pb, k8=k8, kt=kt: e.transpose(pb[:, k8 * 128:(k8 + 1) * 128], in_=hb2[:, kt * 128:(kt + 1) * 128], identity=identb[:]),
                                     reads=["hb2", "identb"], writes=["psb%d" % half])
                            dst = h2t[:, half * 8:(half + 1) * 8, cc * 128:(cc + 1) * 128]
                            src = pb[:].rearrange("p (k t) -> p k t", k=8)
                            if half == 0:
                                P.op("dve", lambda e, dst=dst, src=src: e.tensor_copy(out=dst, in_=src), reads=["psb0"], writes=["h2t"])
                            else:
                                P.op("act", lambda e, dst=dst, src=src: e.activation(out=dst, in_=src, func=AF.Copy), reads=["psb1"], writes=["h2t"])
                    P.dma(lambda q, o0=o0: q.dma_start(out=h2T_d[:, :, o0:o0 + 512].rearrange("k p t -> p k t"), in_=h2t[:]), reads=["h2t"], writes=["h2T_d"], key="st")
            P.barrier()
            stage_end(6)
            with ExitStack() as ph:
                sb = lambda n, s, d=F32: ph.enter_context(nc.sbuf_tensor(n, list(s), d))
                gt2 = sb("gt2", [128, D]); k1b = sb("k1b", [128, 128], BF16); k2b = sb("k2b", [128, 128], BF16)
                bc_row(gt2[:], mod_d[0:1, 5 * D:6 * D], D, "gt2")
                h2g = sb("h2g", [128, 16, 512], BF16); acc = sb("acc", [128, 4, D])
                s2 = sb("s2p", [128, 4, 8, 128]); thr = sb("thr", [128, 4, 8, 128])
                e1 = sb("e1p", [128, 4, 8, 128]); E2 = sb("E2p", [128, 4, 8, 128], BF16)
                with ExitStack() as ph2:
                    sb2 = lambda n, s, d=F32: ph2.enter_context(nc.sbuf_tensor(n, list(s), d))
                    kst = sb2("kst", [128, 256])
                    P.dma(lambda q: q.dma_start(out=kst[:, 0:128], in_=k1T), writes=["kst"], key="ld")
                    P.dma(lambda q: q.dma_start(out=kst[:, 128:256], in_=k2T), writes=["kst"], key="ld")
                    P.op("dve", lambda e: e.tensor_copy(out=k1b[:], in_=kst[:, 0:128]), reads=["kst"], writes=["k1b"])
                    P.op("dve", lambda e: e.tensor_copy(out=k2b[:], in_=kst[:, 128:256]), reads=["kst"], writes=["k2b"])
                    P.barrier()
                for tg in range(4):
                    o0 = tg * 512
                    P.barrier()
                    P.dma(lambda q, o0=o0: q.dma_start(out=h2g[:], in_=h2T_d[:, :, o0:o0 + 512].rearrange("k p t -> p k t")), reads=["h2T_d"], writes=["h2g"], key="ld")
                    with ExitStack() as ph2:
                        sb2 = lambda n, s, d=F32, tg=tg: ph2.enter_context(nc.sbuf_tensor("%s_%d" % (n, tg), list(s), d))
                        qT = sb2("qTp", [128, 16, 512], BF16); wqc = sb2("wqc", [128, 16, 128], BF16)
                        stq = [sb2("stq%d" % i, [128, 128]) for i in range(2)]
                        s1 = sb2("s1p", [128, 8, 128]); v1 = sb2("v1p", [128, 8, 16]); v2 = sb2("v2p", [128, 8, 16])
                        scr = sb2("scr", [128, 256]); cand = sb2("cand", [128, 8, 256]); best = sb2("best", [128, 8, 16])
                        eb = sb2("eb", [128, 8, 16]); Zs = sb2("Zs", [128, 8]); lnZ = sb2("lnZ", [128, 8]); off = sb2("offp", [128, 8])
                        tmp = sb2("tmpp", [128, 8, 128])
                        for fcq in range(16):
                            load_w_bf16(wqc, wq[:, fcq * 128:(fcq + 1) * 128], 16, 128, stq, "wqc")
                            pq = psum[fcq % 2]
                            for kt in range(16):
                                P.op("pe", lambda e, pq=pq, kt=kt: e.matmul(pq[:], lhsT=wqc[:, kt, :], rhs=h2g[:, kt, :], start=(kt == 0), stop=(kt == 15)),
                                     reads=[("wqc", kt), "h2g"], writes=["ps%d" % (fcq % 2)])
                            P.op("act", lambda e, pq=pq, fcq=fcq: e.activation(out=qT[:, fcq, :], in_=pq[:], func=AF.Copy), reads=["ps%d" % (fcq % 2)], writes=[("qTp", fcq)])
                        for tl in range(4):
                            ts_ = slice(tl * 128, (tl + 1) * 128)
                            for h in range(8):
                                P.op("pe", lambda e, h=h, ts_=ts_: e.matmul(psum[2 + h // 4][:, (h % 4) * 128:(h % 4 + 1) * 128], lhsT=qT[:, 2 * h, ts_], rhs=k1b[:], start=True, stop=True),
                                     reads=[("qTp", 2 * h), "k1b"], writes=["ps%d" % (2 + h // 4)])
                                P.op("pe", lambda e, h=h, ts_=ts_: e.matmul(psum[4 + h // 4][:, (h % 4) * 128:(h % 4 + 1) * 128], lhsT=qT[:, 2 * h + 1, ts_], rhs=k2b[:], start=True, stop=True),
                                     reads=[("qTp", 2 * h + 1), "k2b"], writes=["ps%d" % (4 + h // 4)])
                            for hh in range(2):
                                P.op("act", lambda e, hh=hh: e.activation(out=s1[:, hh * 4:(hh + 1) * 4, :], in_=psum[2 + hh][:].rearrange("p (h i) -> p h i", h=4), func=AF.Copy),
                                     reads=["ps%d" % (2 + hh)], writes=["s1p"])
                                P.op("dve", lambda e, hh=hh, tl=tl: e.tensor_copy(out=s2[:, tl, hh * 4:(hh + 1) * 4, :], in_=psum[4 + hh][:].rearrange("p (h i) -> p h i", h=4)),
                                     reads=["ps%d" % (4 + hh)], writes=[("s2p", tl)])
                            for (src_, vv, rk) in ((s1, v1, "s1p"), (None, v2, ("s2p", tl))):
                                for h in range(8):
                                    sv = s1[:, h, :] if src_ is not None else s2[:, tl, h, :]
                                    P.op("dve", lambda e, sv=sv, vv=vv, h=h: e.max(out=vv[:, h, 0:8], in_=sv), reads=[rk], writes=["vtop"])
                                    P.op("dve", lambda e, sv=sv, vv=vv, h=h: e.match_replace(out=scr[:, 0:128], in_to_replace=vv[:, h, 0:8], in_values=sv, imm_value=-1e30),
                                         reads=[rk, "vtop"], writes=["scr"])
                                    P.op("dve", lambda e, vv=vv, h=h: e.max(out=vv[:, h, 8:16], in_=scr[:, 0:128]), reads=["scr"], writes=["vtop"])
                            P.op("dve", lambda e: e.tensor_tensor(out=cand[:].rearrange("p h (a b) -> p h a b", a=16), in0=v1[:].unsqueeze(3).broadcast_to([128, 8, 16, 16]),
                                                                  in1=v2[:].unsqueeze(2).broadcast_to([128, 8, 16, 16]), op=ALU.add), reads=["vtop"], writes=["cand"])
                            for h in range(8):
                                P.op("dve", lambda e, h=h: e.max(out=best[:, h, 0:8], in_=cand[:, h, :]), reads=["cand"], writes=["best"])
                                P.op("dve", lambda e, h=h: e.match_replace(out=scr[:], in_to_replace=best[:, h, 0:8], in_values=cand[:, h, :], imm_value=-1e30),
                                     reads=["cand", "best"], writes=["scr"])
                                P.op("dve", lambda e, h=h: e.max(out=best[:, h, 8:16], in_=scr[:]), reads=["scr"], writes=["best"])
                            P.op("dve", lambda e: e.tensor_tensor(out=eb[:], in0=best[:], in1=best[:, :, 0:1].broadcast_to([128, 8, 16]), op=ALU.subtract), reads=["best"], writes=["eb"])
                            P.op("act", lambda e: e.activation(out=eb[:], in_=eb[:], func=AF.Exp), reads=["eb"], writes=["eb"])
                            P.op("dve", lambda e: e.tensor_reduce(out=Zs[:], in_=eb[:], axis=AX.X, op=ALU.add), reads=["eb"], writes=["Zs"])
                            P.op("act", lambda e: e.activation(out=lnZ[:], in_=Zs[:], func=AF.Ln), reads=["Zs"], writes=["lnZ"])
                            P.op("dve", lambda e, tl=tl: e.tensor_tensor(out=thr[:, tl], in0=best[:, :, 15:16].broadcast_to([128, 8, 128]), in1=s1[:], op=ALU.subtract),
                                 reads=["best", "s1p"], writes=[("thr", tl)])
                            P.op("dve", lambda e, tl=tl: e.tensor_scalar(out=thr[:, tl], in0=thr[:, tl], scalar1=-1e-5, scalar2=None, op0=ALU.add),
                                 reads=[("thr", tl)], writes=[("thr", tl)])
                            P.op("dve", lambda e: e.tensor_tensor(out=off[:], in0=v1[:, :, 0], in1=lnZ[:], op=ALU.add), reads=["vtop", "lnZ"], writes=["offp"])
                            P.op("dve", lambda e: e.tensor_tensor(out=tmp[:], in0=s1[:], in1=off[:].unsqueeze(2).broadcast_to([128, 8, 128]), op=ALU.subtract),
                                 reads=["s1p", "offp"], writes=["tmpp"])
                            P.op("act", lambda e, tl=tl: e.activation(out=e1[:, tl], in_=tmp[:], func=AF.Exp), reads=["tmpp"], writes=[("e1p", tl)])
                            P.op("dve", lambda e, tl=tl: e.tensor_tensor(out=tmp[:], in0=s2[:, tl], in1=v2[:, :, 0:1].broadcast_to([128, 8, 128]), op=ALU.subtract),
                                 reads=[("s2p", tl), "vtop", "tmpp"], writes=["tmpp"])
                            P.op("act", lambda e, tl=tl: e.activation(out=E2[:, tl], in_=tmp[:], func=AF.Exp), reads=["tmpp"], writes=[("E2p", tl)])
                        P.barrier()
                    with ExitStack() as ph2:
                        sb2 = lambda n, s, d=F32, tg=tg: ph2.enter_context(nc.sbuf_tensor("%s_%d" % (n, tg), list(s), d))
                        stU = [sb2("stU%d" % i, [128, D]) for i in range(2)]
                        Ub = sb2("Ub", [128, D], BF16)
                        UcT = [sb2("UcT%d" % i, [128, 16, 128], BF16) for i in range(2)]
                        Vc = [sb2("Vc%d" % i, [128, D], BF16) for i in range(8)]
                        gA = [sb2("gA%d" % i, [128, 512]) for i in range(2)]
                        GAT = [sb2("GAT%d" % i, [128, 512], BF16) for i in range(4)]
                        Gm = [sb2("Gmp%d" % i, [128, 8, 128], BF16) for i in range(3)]
                        Gs = [sb2("Gsp%d" % i, [128, 8, 128], BF16) for i in range(3)]
                        xfin = stU
                        WTp = psb[1][:].bitcast(F32)
                        state = {"nld": 0, "ng": 0}

                        def prepA(c):
                            su = stU[state["nld"] % 2]; suk = ("stU", state["nld"] % 2); state["nld"] += 1
                            uT_ = UcT[c % 2]; utk = ("UcT", c % 2)
                            P.dma(lambda q: q.dma_start(out=su[:], in_=u_tab[c * 128:(c + 1) * 128, :]), writes=[suk], key="tab")
                            P.op("pool", lambda e: e.tensor_copy(out=Ub[:], in_=su[:]), reads=[suk], writes=["Ub"])
                            for half in range(2):
                                for k8 in range(8):
                                    kt = half * 8 + k8
                                    P.op("pe", lambda e, k8=k8, kt=kt: e.transpose(psb[0][:, k8 * 128:(k8 + 1) * 128], in_=Ub[:, kt * 128:(kt + 1) * 128], identity=identb[:]),
                                         reads=["Ub", "identb"], writes=["psb0"])
                                P.op("act", lambda e, half=half: e.activation(out=uT_[:, half * 8:(half + 1) * 8, :], in_=psb[0][:].rearrange("p (k t) -> p k t", k=8), func=AF.Copy),
                                     reads=["psb0"], writes=[utk])
                            sv_ = stU[state["nld"] % 2]; svk = ("stU", state["nld"] % 2); state["nld"] += 1
                            vc_ = Vc[c % 8]; vck = ("Vc", c % 8)
                            P.dma(lambda q: q.dma_start(out=sv_[:], in_=v_tab[c * 128:(c + 1) * 128, :]), writes=[svk], key="tab")
                            P.op("act", lambda e: e.activation(out=vc_[:], in_=sv_[:], func=AF.Copy), reads=[svk], writes=[vck])
                            pa = psum[c % 2]; pak = "ps%d" % (c % 2); ga_ = gA[c % 2]; gak = ("gA", c % 2)
                            for kt in range(16):
                                P.op("pe", lambda e, kt=kt: e.matmul(pa[:], lhsT=uT_[:, kt, :], rhs=h2g[:, kt, :], start=(kt == 0), stop=(kt == 15)),
                                     reads=[utk, "h2g"], writes=[pak])
                            P.op("act", lambda e: e.activation(out=ga_[:], in_=pa[:], func=AF.Gelu_apprx_tanh), reads=[pak], writes=[gak])

                        def gate(c):
                            ga_ = gA[c % 2]; gak = ("gA", c % 2); gat_ = GAT[c % 4]; gatk = ("GAT", c % 4)
                            for tl in range(4):
                                b_ = state["ng"] % 3; state["ng"] += 1
                                gm_ = Gm[b_]; gs_ = Gs[b_]
                                for h in range(8):
                                    P.op("dve", lambda e, tl=tl, h=h, gm_=gm_: e.tensor_scalar(out=gm_[:, h, :], in0=s2[:, tl, h, :], scalar1=thr[:, tl, h, c:c + 1], scalar2=e1[:, tl, h, c:c + 1], op0=ALU.is_ge, op1=ALU.mult),
                                         reads=[("s2p", tl), ("thr", tl), ("e1p", tl)], writes=[("Gmp", b_, h)])
                                eng = "dve" if tl % 2 == 0 else "pool"
                                P.op(eng, lambda e, tl=tl, gm_=gm_, gs_=gs_: e.tensor_tensor(out=gs_[:], in0=gm_[:], in1=E2[:, tl], op=ALU.mult),
                                     reads=[("Gmp", b_, h) for h in range(8)] + [("E2p", tl)], writes=[("Gsp", b_)])
                                for h in range(8):
                                    P.op("pe", lambda e, tl=tl, h=h, gs_=gs_: e.matmul(WTp[:, tl * 128:(tl + 1) * 128], lhsT=gs_[:, h, :], rhs=identb[:], start=(h == 0), stop=(h == 7)),
                                         reads=[("Gsp", b_), "identb"], writes=["psb1"])
                            P.op("dve", lambda e: e.tensor_tensor(out=gat_[:], in0=WTp, in1=ga_[:], op=ALU.mult), reads=["psb1", gak], writes=[gatk])

                        def vmm(cg):
                            for tl in range(4):
                                for ci in range(4):
                                    c = cg * 4 + ci
                                    for nb in range(4):
                                        P.op("pe", lambda e, tl=tl, ci=ci, nb=nb, c=c: e.matmul(psum[2 + nb][:], lhsT=GAT[c % 4][:, tl * 128:(tl + 1) * 128], rhs=Vc[c % 8][:, nb * 512:(nb + 1) * 512], start=(ci == 0), stop=(ci == 3)),
                                             reads=[("GAT", c % 4), ("Vc", c % 8)], writes=["ps%d" % (2 + nb)])
                                for nb in range(4):
                                    a_sl = acc[:, tl, nb * 512:(nb + 1) * 512]
                                    if cg == 0:
                                        P.op("dve", lambda e, a_sl=a_sl, nb=nb: e.tensor_copy(out=a_sl, in_=psum[2 + nb][:]), reads=["ps%d" % (2 + nb)], writes=[("acc", tl, nb)])
                                    else:
                                        P.op("dve", lambda e, a_sl=a_sl, nb=nb: e.tensor_tensor(out=a_sl, in0=psum[2 + nb][:], in1=a_sl, op=ALU.add), reads=["ps%d" % (2 + nb), ("acc", tl, nb)], writes=[("acc", tl, nb)])

                        prepA(0)
                        for c in range(128):
                            if c + 1 < 128:
                                prepA(c + 1)
                            gate(c)
                            if c % 4 == 3:
                                vmm(c // 4)
                        for tl in range(4):
                            r0 = o0 + tl * 128
                            xf = xfin[tl % 2]; xfk = ("stU", tl % 2)
                            P.dma(lambda q, xf=xf, r0=r0: q.dma_start(out=xf[:], in_=out[r0:r0 + 128, :]), reads=["out"], writes=[xfk], key="tab")
                            P.op("pool", lambda e, tl=tl: e.tensor_tensor(out=acc[:, tl, :], in0=acc[:, tl, :], in1=gt2[:], op=ALU.mult),
                                 reads=[("acc", tl, 0), ("acc", tl, 1), ("acc", tl, 2), ("acc", tl, 3), "gt2"], writes=[("acc", tl, 0), ("acc", tl, 1), ("acc", tl, 2), ("acc", tl, 3)])
                            P.op("dve", lambda e, tl=tl, xf=xf: e.tensor_tensor(out=xf[:], in0=xf[:], in1=acc[:, tl, :], op=ALU.add),
                                 reads=[xfk, ("acc", tl, 0), ("acc", tl, 1), ("acc", tl, 2), ("acc", tl, 3)], writes=[xfk])
                            P.dma(lambda q, xf=xf, r0=r0: q.dma_start(out=out[r0:r0 + 128, :], in_=xf[:]), reads=[xfk], writes=["out"], key="st")
                        P.barrier()
            P.barrier()
            stage_end(7)
        except _Stop:
            pass
        P.barrier()
        P.emit()
    return nc


_NC_CACHE = {}


def _host_inputs(i, x, c, w_ada, b_ada, norm1_g, w_in, lam_re, lam_im, log_dt, ssm_b_re, ssm_b_im, ssm_c_re, ssm_c_im,
                 ssm_d, w_glu, q_norm_g, k_norm_g, w_att_up, w_out, norm2_g, peer_wq, peer_k1, peer_k2, peer_u, peer_v, shared):
    b, q = i // 4, i % 4
    t_end = (q + 1) * 2048
    t_start = t_end - SEQ
    xw = np.zeros((SEQ, D), np.float32)
    lo = max(t_start, 0)
    xw[lo - t_start:] = x[b, lo:t_end]
    tok = t_start + np.arange(SEQ)
    valid = (tok >= 0).astype(np.float32).reshape(NT, 128).T.copy()
    cbm = np.ascontiguousarray(c[b].reshape(16, 128).T)
    d = dict(shared)
    d.update(xw=xw, valid=valid, cb=cbm)
    return d


def _prep(x, c, w_ada, b_ada, norm1_g, w_in, lam_re, lam_im, log_dt, ssm_b_re, ssm_b_im, ssm_c_re, ssm_c_im,
           ssm_d, w_glu, q_norm_g, k_norm_g, w_att_up, w_out, norm2_g, peer_wq, peer_k1, peer_k2, peer_u, peer_v):
    f = lambda a: np.ascontiguousarray(np.asarray(a, dtype=np.float32))
    x = f(x); c = f(c)
    lamT_re = f(np.asarray(lam_re)[0].reshape(32, 2, 64).transpose(1, 2, 0).reshape(128, 32))
    lamT_im = f(np.asarray(lam_im)[0].reshape(32, 2, 64).transpose(1, 2, 0).reshape(128, 32))
    ldtT = f(np.broadcast_to(np.asarray(log_dt)[0].reshape(32, 2).T[:, None, :], (2, 64, 32)).reshape(128, 32))
    Bre = np.asarray(ssm_b_re)[0]; Bim = np.asarray(ssm_b_im)[0]
    BreT = np.zeros((128, 32, 128), np.float32); BimT = np.zeros((128, 32, 128), np.float32)
    for g in range(64):
        gl = g % 8
        st, g2 = g // 2, g % 2
        BreT[gl * 16:(gl + 1) * 16, st, g2 * 64:(g2 + 1) * 64] = Bre[g].T
        BimT[gl * 16:(gl + 1) * 16, st, g2 * 64:(g2 + 1) * 64] = Bim[g].T
    Cr = np.asarray(ssm_c_re)[0]; Ci = np.asarray(ssm_c_im)[0]
    Cre = np.zeros((128, 32, 128), np.float32); Cim = np.zeros((128, 32, 128), np.float32)
    for g in range(64):
        st, g2, gl = g // 2, g % 2, g % 8
        Cre[g2 * 64:(g2 + 1) * 64, st, gl * 16:(gl + 1) * 16] = Cr[g].T
        Cim[g2 * 64:(g2 + 1) * 64, st, gl * 16:(gl + 1) * 16] = Ci[g].T
    dsk = f(np.asarray(ssm_d)[0].reshape(8, 128).T)
    sidx = f(np.broadcast_to(np.arange(128, dtype=np.float32)[None, :], (128, 128)))
    shared = dict(w_ada=f(np.asarray(w_ada)[0]), b_ada=f(np.asarray(b_ada)[0][None, :]), norm1_g=f(np.asarray(norm1_g)[0][None, :]),
                  norm2_g=f(np.asarray(norm2_g)[0][None, :]), qg=f(np.asarray(q_norm_g)[0][None, :]), kg=f(np.asarray(k_norm_g)[0][None, :]),
                  w_in=f(np.asarray(w_in)[0]), w_glu=f(np.asarray(w_glu)[0]), w_up=f(np.asarray(w_att_up)[0]), w_out=f(np.asarray(w_out)[0]),
                  wq=f(np.asarray(peer_wq)[0]), k1T=f(np.asarray(peer_k1)[0].T), k2T=f(np.asarray(peer_k2)[0].T),
                  u_tab=f(np.asarray(peer_u)[0]), v_tab=f(np.asarray(peer_v)[0]),
                  lamT_re=lamT_re, lamT_im=lamT_im, ldtT=ldtT, BreT=BreT, BimT=BimT, Cre=Cre, Cim=Cim, dsk=dsk, sidx=sidx)
    args = (x, c, w_ada, b_ada, norm1_g, w_in, lam_re, lam_im, log_dt, ssm_b_re, ssm_b_im, ssm_c_re, ssm_c_im,
            ssm_d, w_glu, q_norm_g, k_norm_g, w_att_up, w_out, norm2_g, peer_wq, peer_k1, peer_k2, peer_u, peer_v)
    in_maps = [_host_inputs(i, *args, shared) for i in range(8)]
    return in_maps


def kernel(**inputs):
    in_maps = _prep(**inputs)
    if "nc" not in _NC_CACHE:
        _NC_CACHE["nc"] = build_nc()
    nc = _NC_CACHE["nc"]
    res = run_bass_kernel_spmd(nc, in_maps, core_ids=list(range(8)))
    outp = np.zeros((2, SEQ, D), np.float32)
    for i in range(8):
        b, q = i // 4, i % 4
        outp[b, q * 2048:(q + 1) * 2048] = res.results[i]["out"]
    return outp
```

```python
import math
from contextlib import ExitStack
import numpy as np
import concourse.bass as bass
import concourse.mybir as mybir
from concourse.bass_utils import run_bass_kernel_spmd

F32 = mybir.dt.float32
BF16 = mybir.dt.bfloat16
I32 = mybir.dt.int32
AF = mybir.ActivationFunctionType
ALU = mybir.AluOpType
AX = mybir.AxisListType

D = 2048
SEQ = 8192
NT = 64
OWN0 = 48
EPS = 1e-6


import types


def _freeze(fn):
    if fn is None or fn.__closure__ is None:
        return fn
    cells = []
    for c in fn.__closure__:
        try:
            cells.append(types.CellType(c.cell_contents))
        except ValueError:
            cells.append(c)
    return types.FunctionType(fn.__code__, fn.__globals__, fn.__name__, fn.__defaults__, tuple(cells))


class Prog:
    ENG = ("pe", "act", "dve", "pool", "sp")

    def __init__(self, nc, stack, same_engine_sync=False):
        self.nc = nc
        self.stack = stack
        self.same = same_engine_sync
        self.streams = {e: [] for e in self.ENG}
        self.count = {e: 0 for e in self.ENG}
        self.sem = {e: stack.enter_context(nc.semaphore("prog_" + e)) for e in self.ENG}
        self.known = {e: {} for e in self.ENG}
        self.last_w = {}
        self.readers = {}
        self.dma_keys = {}

    def _sem_of(self, key):
        if isinstance(key, str):
            return self.sem[key]
        return self.dma_keys[key[1]]["sems"][key[2]]

    def _deps(self, reads, writes):
        deps = []
        for r in reads:
            if r in self.last_w:
                deps.append(self.last_w[r])
        for w in writes:
            if w in self.last_w:
                deps.append(self.last_w[w])
            deps.extend(self.readers.get(w, ()))
        return deps

    def _waits(self, eng, deps):
        need = {}
        for (k, v) in deps:
            if k == eng and (not self.same or eng == "pe"):
                continue
            if self.known[eng].get(k, 0) >= v:
                continue
            if need.get(k, 0) < v:
                need[k] = v
        for k, v in need.items():
            self.known[eng][k] = v
        return list(need.items())

    def _commit(self, ev, reads, writes):
        for r in reads:
            self.readers.setdefault(r, []).append(ev)
        for w in writes:
            self.last_w[w] = ev
            self.readers[w] = []

    def op(self, eng, fn, reads=(), writes=()):
        waits = self._waits(eng, self._deps(reads, writes))
        self.count[eng] += 1
        ev = (eng, self.count[eng])
        self.streams[eng].append((waits, _freeze(fn), (eng, 1)))
        self._commit(ev, reads, writes)
        return ev

    def dma(self, fn, reads=(), writes=(), key="d", nsem=4, queue="sp"):
        if key not in self.dma_keys:
            sems = [self.stack.enter_context(self.nc.semaphore("dma_%s_%d" % (key, i))) for i in range(nsem)]
            self.dma_keys[key] = {"sems": sems, "n": 0}
        st = self.dma_keys[key]
        i = st["n"]
        st["n"] += 1
        R = len(st["sems"])
        evk = ("dma", key, i % R)
        ev = (evk, 16 * (i // R + 1))
        waits = self._waits(queue, self._deps(reads, writes))
        if i >= R:
            prev = (evk, 16 * (i // R))
            if self.known[queue].get(evk, 0) < prev[1]:
                self.known[queue][evk] = prev[1]
                waits.append(prev)
        self.streams[queue].append((waits, _freeze(fn), (evk, 16)))
        self._commit(ev, reads, writes)
        return ev

    def _all_events(self):
        deps = []
        for e in self.ENG:
            if self.count[e]:
                deps.append((e, self.count[e]))
        for key, st in self.dma_keys.items():
            R = len(st["sems"])
            for j in range(min(R, st["n"])):
                n_on = (st["n"] - 1 - j) // R + 1
                deps.append((("dma", key, j), 16 * n_on))
        return deps

    def barrier(self):
        deps = self._all_events()
        for e in self.ENG:
            waits = self._waits(e, [d for d in deps if d[0] != e])
            if waits:
                self.streams[e].append((waits, None, None))

    def emit(self):
        nc = self.nc
        engobj = {"pe": "tensor", "act": "scalar", "dve": "vector", "pool": "gpsimd", "sp": "sync"}
        with nc.Block() as block:
            for e in self.ENG:
                stream = self.streams[e]
                if not stream:
                    continue

                def body(engine, stream=stream):
                    for waits, fn, inc in stream:
                        for k, v in waits:
                            engine.wait_ge(self._sem_of(k), v)
                        if fn is None:
                            continue
                        ins = fn(engine)
                        ins.then_inc(self._sem_of(inc[0]), inc[1])

                getattr(block, engobj[e])(body)


class _Stop(Exception):
    pass


def build_nc(stages=99, debug=False, small_tabs=False):
    nc = bass.Bass("TRN2", target_bir_lowering=False)

    def stage_end(n):
        if stages <= n:
            raise _Stop()

    dbg_state = {}

    def dbg_dump(P, name, ap, shape, dt, rkeys):
        if not debug:
            return
        t = nc.dram_tensor("dbg_" + name, list(shape), dt, kind="ExternalOutput").ap()
        P.dma(lambda q: q.dma_start(out=t, in_=ap), reads=rkeys, writes=["dbg_" + name], key="dbg")

    def din(name, shape, dt=F32):
        return nc.dram_tensor(name, list(shape), dt, kind="ExternalInput").ap()

    def dscr(name, shape, dt):
        return nc.dram_tensor(name, list(shape), dt, kind=("ExternalOutput" if debug else "Internal")).ap()

    xw = din("xw", [SEQ, D]); valid = din("valid", [128, NT]); cb = din("cb", [128, 16])
    w_ada = din("w_ada", [D, 6 * D]); b_ada = din("b_ada", [1, 6 * D])
    norm1_g = din("norm1_g", [1, D]); norm2_g = din("norm2_g", [1, D])
    qg = din("qg", [1, 128]); kg = din("kg", [1, 128])
    w_in = din("w_in", [D, 8192]); w_glu = din("w_glu", [1024, 4096]); w_up = din("w_up", [1024, D])
    w_out = din("w_out", [D, D]); wq = din("wq", [D, D]); k1T = din("k1T", [128, 128]); k2T = din("k2T", [128, 128])
    NTAB = 128 if small_tabs else 16384
    u_tab = din("u_tab", [NTAB, D]); v_tab = din("v_tab", [NTAB, D])
    lamT_re = din("lamT_re", [128, 32]); lamT_im = din("lamT_im", [128, 32]); ldtT = din("ldtT", [128, 32])
    BreT = din("BreT", [128, 32, 128]); BimT = din("BimT", [128, 32, 128])
    Cre = din("Cre", [128, 32, 128]); Cim = din("Cim", [128, 32, 128]); dsk = din("dsk", [128, 8])
    sidx = din("sidx", [128, 128])
    out = nc.dram_tensor("out", [2048, D], F32, kind="ExternalOutput").ap()

    mod_d = dscr("mod_d", [1, 6 * D], F32)
    hT_d = dscr("hT_d", [16, 128, SEQ], BF16)
    kT_d = dscr("kT_d", [8, 128, SEQ], BF16)
    v_d = dscr("v_d", [SEQ, 1024], BF16)
    qT_d = dscr("qT_d", [8, 128, 2048], BF16)
    ygT_d = dscr("ygT_d", [8, 128, 2048], BF16)
    attT_d = dscr("attT_d", [8, 128, 2048], BF16)
    h2T_d = dscr("h2T_d", [16, 128, 2048], BF16)

    with ExitStack() as top:
        P = Prog(nc, top, same_engine_sync=True)
        try:
            tsb = lambda n, s, d=F32: top.enter_context(nc.sbuf_tensor(n, list(s), d))
            ident = tsb("ident", [128, 128]); identb = tsb("identb", [128, 128], BF16)
            cmask = tsb("cmask", [128, 128])
            ones = tsb("ones", [128, 512])
            P.op("pool", lambda e: e.memset(ident[:], 0.0), writes=["ident"])
            P.op("pool", lambda e: e.affine_select(out=ident[:], in_=ident[:], pattern=[[-1, 128]], compare_op=ALU.not_equal,
                                                   fill=1.0, base=0, channel_multiplier=1), reads=["ident"], writes=["ident"])
            P.op("pool", lambda e: e.tensor_copy(out=identb[:], in_=ident[:]), reads=["ident"], writes=["identb"])
            P.op("pool", lambda e: e.memset(ones[:], 1.0), writes=["ones"])
            P.op("pool", lambda e: e.memset(cmask[:], 1.0), writes=["cmask"])
            P.op("pool", lambda e: e.affine_select(out=cmask[:], in_=cmask[:], pattern=[[-1, 128]], compare_op=ALU.is_gt,
                                                   fill=0.0, base=0, channel_multiplier=1), reads=["cmask"], writes=["cmask"])

            psum = [top.enter_context(nc.psum_tensor("ps%d" % i, [128, 512], F32)) for i in range(6)]
            psb = [top.enter_context(nc.psum_tensor("psb%d" % i, [128, 1024], BF16)) for i in range(2)]

            def load_w_bf16(dst, src, nk, width, stg, tag, col0=0):
                for kt in range(nk):
                    s = stg[kt % len(stg)]
                    sk = ("stg", id(stg), kt % len(stg))
                    P.dma(lambda q, s=s, kt=kt: q.dma_start(out=s[:, 0:width], in_=src[kt * 128:(kt + 1) * 128, :]),
                          writes=[sk], key="wld", nsem=8)
                    eng = ("act", "pool")[kt % 2]
                    if eng == "act":
                        P.op("act", lambda e, s=s, kt=kt: e.activation(out=dst[:, kt, col0:col0 + width], in_=s[:, 0:width], func=AF.Copy),
                             reads=[sk], writes=[(tag, kt)])
                    else:
                        P.op("pool", lambda e, s=s, kt=kt: e.tensor_copy(out=dst[:, kt, col0:col0 + width], in_=s[:, 0:width]),
                             reads=[sk], writes=[(tag, kt)])

            with ExitStack() as ph:
                sb = lambda n, s, d=F32: ph.enter_context(nc.sbuf_tensor(n, list(s), d))
                cbt = sb("cbt", [128, 16]); csl = sb("csl", [128, 16]); brow = sb("brow", [1, 6 * D]); mrow = sb("mrow", [1, 6 * D])
                wblk = [sb("wblk%d" % i, [128, 16, 512]) for i in range(2)]
                P.dma(lambda q: q.dma_start(out=cbt[:], in_=cb), writes=["cbt"], key="ld")
                P.dma(lambda q: q.dma_start(out=brow[:], in_=b_ada), writes=["brow"], key="ld")
                P.op("act", lambda e: e.activation(out=csl[:], in_=cbt[:], func=AF.Silu), reads=["cbt"], writes=["csl"])
                for nb in range(24):
                    wb = wblk[nb % 2]
                    P.dma(lambda q, wb=wb, nb=nb: q.dma_start(out=wb[:], in_=w_ada[:, nb * 512:(nb + 1) * 512].rearrange("(kt p) n -> p kt n", p=128)),
                          writes=[("wblk", nb % 2)], key="ld")
                    for kt in range(16):
                        P.op("pe", lambda e, wb=wb, kt=kt: e.matmul(psum[0][0:1, :], lhsT=csl[:, kt:kt + 1], rhs=wb[:, kt, :], start=(kt == 0), stop=(kt == 15)),
                             reads=["csl", ("wblk", nb % 2)], writes=["ps0"])
                    P.op("dve", lambda e, nb=nb: e.tensor_tensor(out=mrow[0:1, nb * 512:(nb + 1) * 512], in0=psum[0][0:1, :],
                                                                 in1=brow[0:1, nb * 512:(nb + 1) * 512], op=ALU.add),
                         reads=["ps0", "brow"], writes=["mrow"])
                P.dma(lambda q: q.dma_start(out=mod_d, in_=mrow[:]), reads=["mrow"], writes=["mod_d"], key="st")
            P.barrier()
            stage_end(1)

            def bc_row(dst, row_ap, n, wkey):
                P.dma(lambda q: q.dma_start(out=dst, in_=row_ap.broadcast_to([128, n])), reads=["mod_d"], writes=[wkey], key="ld")

            def rms_stats(xt, xkey, junk, ss, rstd, tagsfx):
                P.op("act", lambda e: e.activation(out=junk, in_=xt, func=AF.Square, accum_out=ss), reads=[xkey], writes=["junk" + tagsfx, "ss" + tagsfx])
                P.op("dve", lambda e: e.tensor_scalar(out=ss, in0=ss, scalar1=1.0 / D, scalar2=EPS, op0=ALU.mult, op1=ALU.add),
                     reads=["ss" + tagsfx], writes=["ss" + tagsfx])
                P.op("act", lambda e: e.activation(out=ss, in_=ss, func=AF.Sqrt), reads=["ss" + tagsfx], writes=["ss" + tagsfx])
                P.op("dve", lambda e: e.reciprocal(out=rstd, in_=ss), reads=["ss" + tagsfx], writes=["rstd" + tagsfx])

            with ExitStack() as ph:
                sb = lambda n, s, d=F32: ph.enter_context(nc.sbuf_tensor(n, list(s), d))
                gs1 = sb("gs1", [128, D]); sh1 = sb("sh1", [128, D]); g1 = sb("g1", [128, D]); vld = sb("vld", [128, NT])
                xt = [sb("xt%d" % i, [128, D]) for i in range(2)]
                junk = sb("junk", [128, D]); tA = sb("tA", [128, D]); tB = sb("tB", [128, D])
                hb = [sb("hb%d" % i, [128, D], BF16) for i in range(2)]
                hTt = [sb("hTt%d" % i, [128, 16, 512], BF16) for i in range(2)]
                ss = sb("ss", [128, 1]); rstd = sb("rstd", [128, 1])
                bc_row(sh1[:], mod_d[0:1, 0:D], D, "sh1")
                bc_row(gs1[:], mod_d[0:1, D:2 * D], D, "gs1")
                P.dma(lambda q: q.dma_start(out=g1[:], in_=norm1_g.broadcast_to([128, D])), writes=["g1"], key="ld")
                P.dma(lambda q: q.dma_start(out=vld[:], in_=valid), writes=["vld"], key="ld")
                P.op("dve", lambda e: e.scalar_tensor_tensor(out=gs1[:], in0=gs1[:], scalar=1.0, in1=g1[:], op0=ALU.add, op1=ALU.mult),
                     reads=["gs1", "g1"], writes=["gs1"])
                for c in range(NT):
                    x_ = xt[c % 2]; xk = ("xt", c % 2); g = c // 4; hT_ = hTt[g % 2]; hb_ = hb[c % 2]
                    P.dma(lambda q, x_=x_, c=c: q.dma_start(out=x_[:], in_=xw[c * 128:(c + 1) * 128, :]), writes=[xk], key="xld")
                    rms_stats(x_[:], xk, junk[:], ss[:], rstd[:], "A")
                    P.op("dve", lambda e, x_=x_: e.scalar_tensor_tensor(out=tA[:], in0=x_[:], scalar=rstd[:, 0:1], in1=gs1[:], op0=ALU.mult, op1=ALU.mult),
                         reads=[xk, "rstdA", "gs1"], writes=["tA"])
                    P.op("pool", lambda e: e.tensor_tensor(out=tB[:], in0=tA[:], in1=sh1[:], op=ALU.add), reads=["tA", "sh1"], writes=["tB"])
                    P.op("pool", lambda e, hb_=hb_, c=c: e.tensor_scalar(out=hb_[:], in0=tB[:], scalar1=vld[:, c:c + 1], scalar2=None, op0=ALU.mult),
                         reads=["tB", "vld"], writes=[("hb", c % 2)])
                    for half in range(2):
                        pb = psb[half]
                        for k8 in range(8):
                            kt = half * 8 + k8
                            P.op("pe", lambda e, pb=pb, k8=k8, kt=kt, hb_=hb_: e.transpose(pb[:, k8 * 128:(k8 + 1) * 128], in_=hb_[:, kt * 128:(kt + 1) * 128], identity=identb[:]),
                                 reads=[("hb", c % 2), "identb"], writes=["psb%d" % half])
                        eng = ("dve", "act")[half]
                        dst = hT_[:, half * 8:(half + 1) * 8, (c % 4) * 128:(c % 4 + 1) * 128]
                        src = pb[:].rearrange("p (k t) -> p k t", k=8)
                        if eng == "dve":
                            P.op("dve", lambda e, dst=dst, src=src: e.tensor_copy(out=dst, in_=src), reads=["psb%d" % half], writes=[("hTt", g % 2)])
                        else:
                            P.op("act", lambda e, dst=dst, src=src: e.activation(out=dst, in_=src, func=AF.Copy), reads=["psb%d" % half], writes=[("hTt", g % 2)])
                    if c % 4 == 3:
                        P.dma(lambda q, hT_=hT_, g=g: q.dma_start(out=hT_d[:, :, g * 512:(g + 1) * 512].rearrange("k p t -> p k t"), in_=hT_[:]),
                              reads=[("hTt", g % 2)], writes=["hT_d"], key="st")
            P.barrier()
            stage_end(2)

            with ExitStack() as ph:
                sb = lambda n, s, d=F32: ph.enter_context(nc.sbuf_tensor(n, list(s), d))
                Pr = sb("Pr", [128, 32, 128], BF16); Pi = sb("Pi", [128, 32, 128], BF16); Qr = sb("Qr", [128, 32, 128], BF16); Qi = sb("Qi", [128, 32, 128], BF16)
                lbr = sb("lbr", [128, 32]); lbi = sb("lbi", [128, 32]); cr = sb("cr", [128, 32]); ci = sb("ci", [128, 32])
                wi_re = sb("wi_re", [128, 32]); wi_im = sb("wi_im", [128, 32]); L_re = sb("L_re", [128, 32]); L_im = sb("L_im", [128, 32])
                Bre = sb("Bre", [128, 32, 128], BF16); Bim = sb("Bim", [128, 32, 128], BF16); dskt = sb("dskt", [128, 8])
                with ExitStack() as ph2:
                    sb2 = lambda n, s, d=F32: ph2.enter_context(nc.sbuf_tensor(n, list(s), d))
                    lr = sb2("lr", [128, 32]); li = sb2("li", [128, 32]); dt = sb2("dt", [128, 32]); a_ = sb2("a_", [128, 32]); om = sb2("om", [128, 32])
                    sidt = sb2("sidt", [128, 128])
                    T1 = sb2("T1", [128, 32, 128]); T2 = sb2("T2", [128, 32, 128]); T3 = sb2("T3", [128, 32, 128]); T4 = sb2("T4", [128, 32, 128])
                    TI = sb2("TI", [128, 32, 128], I32); T5 = sb2("T5", [128, 32, 128]); T6 = sb2("T6", [128, 32, 128])
                    s_a = sb2("s_a", [128, 32]); s_b = sb2("s_b", [128, 32]); s_c = sb2("s_c", [128, 32]); s_d = sb2("s_d", [128, 32])
                    s_i = sb2("s_i", [128, 32], I32); sn = sb2("sn", [128, 32]); cs = sb2("cs", [128, 32]); ea = sb2("ea", [128, 32])
                    for (t_, s_, k_) in [(lr[:], lamT_re, "lr"), (li[:], lamT_im, "li"), (dt[:], ldtT, "dt"), (sidt[:], sidx, "sidt"), (T1[:], BreT, "BreF"),
                                         (T2[:], BimT, "BimF"), (dskt[:], dsk, "dskt")]:
                        P.dma(lambda q, t_=t_, s_=s_: q.dma_start(out=t_, in_=s_), writes=[k_], key="ld")
                    P.op("dve", lambda e: e.tensor_copy(out=Bre[:], in_=T1[:]), reads=["BreF"], writes=["Bre"])
                    P.op("dve", lambda e: e.tensor_copy(out=Bim[:], in_=T2[:]), reads=["BimF"], writes=["Bim"])
                    P.barrier()
                    P.op("act", lambda e: e.activation(out=dt[:], in_=dt[:], func=AF.Exp), reads=["dt"], writes=["dt"])
                    P.op("dve", lambda e: e.tensor_tensor(out=a_[:], in0=lr[:], in1=dt[:], op=ALU.mult), reads=["lr", "dt"], writes=["a_"])
                    P.op("dve", lambda e: e.tensor_tensor(out=om[:], in0=li[:], in1=dt[:], op=ALU.mult), reads=["li", "dt"], writes=["om"])

                    def sincos(th, n_shape, u_, ui_, f_, s2_, c2_, sin_o, cos_o, tg):
                        P.op("dve", lambda e: e.tensor_scalar(out=u_, in0=th, scalar1=1.0 / (2 * math.pi), scalar2=None, op0=ALU.mult), reads=[tg + "th"], writes=[tg + "u"])
                        P.op("dve", lambda e: e.tensor_copy(out=ui_, in_=u_), reads=[tg + "u"], writes=[tg + "ui"])
                        P.op("dve", lambda e: e.tensor_copy(out=f_, in_=ui_), reads=[tg + "ui"], writes=[tg + "f"])
                        P.op("dve", lambda e: e.tensor_tensor(out=f_, in0=u_, in1=f_, op=ALU.subtract), reads=[tg + "u", tg + "f"], writes=[tg + "f"])
                        P.op("act", lambda e: e.activation(out=s2_, in_=f_, func=AF.Sin, scale=math.pi), reads=[tg + "f"], writes=[tg + "s2"])
                        P.op("act", lambda e: e.activation(out=f_, in_=f_, func=AF.Abs), reads=[tg + "f", tg + "s2"], writes=[tg + "f"])
                        P.op("dve", lambda e: e.tensor_scalar(out=f_, in0=f_, scalar1=-math.pi, scalar2=math.pi / 2, op0=ALU.mult, op1=ALU.add), reads=[tg + "f"], writes=[tg + "f"])
                        P.op("act", lambda e: e.activation(out=c2_, in_=f_, func=AF.Sin), reads=[tg + "f"], writes=[tg + "c2"])
                        P.op("dve", lambda e: e.scalar_tensor_tensor(out=sin_o, in0=s2_, scalar=2.0, in1=c2_, op0=ALU.mult, op1=ALU.mult),
                             reads=[tg + "s2", tg + "c2"], writes=[tg + "sin"])
                        P.op("dve", lambda e: e.tensor_tensor(out=c2_, in0=c2_, in1=c2_, op=ALU.mult), reads=[tg + "c2"], writes=[tg + "c2"])
                        P.op("dve", lambda e: e.tensor_tensor(out=s2_, in0=s2_, in1=s2_, op=ALU.mult), reads=[tg + "s2"], writes=[tg + "s2"])
                        P.op("dve", lambda e: e.tensor_tensor(out=cos_o, in0=c2_, in1=s2_, op=ALU.subtract), reads=[tg + "c2", tg + "s2"], writes=[tg + "cos"])

                    P.op("dve", lambda e: e.tensor_copy(out=s_a[:], in_=om[:]), reads=["om"], writes=["bth"])
                    sincos(s_a[:], None, s_b[:], s_i[:], s_c[:], s_d[:], cs[:], sn[:], cs[:], "b")
                    P.op("act", lambda e: e.activation(out=ea[:], in_=a_[:], func=AF.Exp), reads=["a_"], writes=["ea"])
                    P.op("dve", lambda e: e.tensor_tensor(out=lbr[:], in0=ea[:], in1=cs[:], op=ALU.mult), reads=["ea", "bcos"], writes=["lbr"])
                    P.op("dve", lambda e: e.tensor_tensor(out=lbi[:], in0=ea[:], in1=sn[:], op=ALU.mult), reads=["ea", "bsin"], writes=["lbi"])
                    P.op("dve", lambda e: e.tensor_scalar(out=s_a[:], in0=lbr[:], scalar1=-1.0, scalar2=None, op0=ALU.add), reads=["lbr"], writes=["s_a"])
                    P.op("dve", lambda e: e.tensor_tensor(out=s_b[:], in0=lr[:], in1=lr[:], op=ALU.mult), reads=["lr"], writes=["s_b"])
                    P.op("dve", lambda e: e.tensor_tensor(out=s_c[:], in0=li[:], in1=li[:], op=ALU.mult), reads=["li"], writes=["s_c"])
                    P.op("dve", lambda e: e.tensor_tensor(out=s_b[:], in0=s_b[:], in1=s_c[:], op=ALU.add), reads=["s_b", "s_c"], writes=["s_b"])
                    P.op("dve", lambda e: e.reciprocal(out=s_b[:], in_=s_b[:]), reads=["s_b"], writes=["s_b"])
                    P.op("dve", lambda e: e.tensor_tensor(out=s_c[:], in0=s_a[:], in1=lr[:], op=ALU.mult), reads=["s_a", "lr"], writes=["s_c"])
                    P.op("dve", lambda e: e.tensor_tensor(out=s_d[:], in0=lbi[:], in1=li[:], op=ALU.mult), reads=["lbi", "li"], writes=["s_d"])
                    P.op("dve", lambda e: e.tensor_tensor(out=s_c[:], in0=s_c[:], in1=s_d[:], op=ALU.add), reads=["s_c", "s_d"], writes=["s_c"])
                    P.op("dve", lambda e: e.tensor_tensor(out=cr[:], in0=s_c[:], in1=s_b[:], op=ALU.mult), reads=["s_c", "s_b"], writes=["cr"])
                    P.op("dve", lambda e: e.tensor_tensor(out=s_c[:], in0=lbi[:], in1=lr[:], op=ALU.mult), reads=["lbi", "lr"], writes=["s_c"])
                    P.op("dve", lambda e: e.tensor_tensor(out=s_d[:], in0=s_a[:], in1=li[:], op=ALU.mult), reads=["s_a", "li"], writes=["s_d"])
                    P.op("dve", lambda e: e.tensor_tensor(out=s_c[:], in0=s_c[:], in1=s_d[:], op=ALU.subtract), reads=["s_c", "s_d"], writes=["s_c"])
                    P.op("dve", lambda e: e.tensor_tensor(out=ci[:], in0=s_c[:], in1=s_b[:], op=ALU.mult), reads=["s_c", "s_b"], writes=["ci"])
                    om_b = om[:].unsqueeze(2).broadcast_to([128, 32, 128]); a_b = a_[:].unsqueeze(2).broadcast_to([128, 32, 128])
                    s_bb = sidt[:].unsqueeze(1).broadcast_to([128, 32, 128])
                    P.op("dve", lambda e: e.tensor_tensor(out=T1[:], in0=om_b, in1=s_bb, op=ALU.mult), reads=["om", "sidt"], writes=["tth"])
                    sincos(T1[:], None, T2[:], TI[:], T3[:], T4[:], T5[:], T6[:], T5[:], "t")
                    P.barrier()
                    P.op("dve", lambda e: e.tensor_tensor(out=T1[:], in0=a_b, in1=s_bb, op=ALU.mult), reads=["a_", "sidt"], writes=["T1as"])
                    P.op("act", lambda e: e.activation(out=T2[:], in_=T1[:], func=AF.Exp), reads=["T1as"], writes=["Ep"])
                    P.op("act", lambda e: e.activation(out=T3[:], in_=T1[:], func=AF.Exp, scale=-1.0), reads=["T1as"], writes=["Em"])
                    P.op("dve", lambda e: e.tensor_tensor(out=Qr[:], in0=T2[:], in1=T5[:], op=ALU.mult), reads=["Ep"], writes=["Qr"])
                    P.op("dve", lambda e: e.tensor_tensor(out=Qi[:], in0=T2[:], in1=T6[:], op=ALU.mult), reads=["Ep"], writes=["Qi"])
                    P.op("dve", lambda e: e.tensor_tensor(out=Pr[:], in0=T3[:], in1=T5[:], op=ALU.mult), reads=["Em"], writes=["Pr"])
                    P.op("dve", lambda e: e.scalar_tensor_tensor(out=Pi[:], in0=T3[:], scalar=-1.0, in1=T6[:], op0=ALU.mult, op1=ALU.mult),
                         reads=["Em"], writes=["Pi"])
                    P.barrier()
                    P.op("dve", lambda e: e.tensor_tensor(out=s_a[:], in0=T2[:, :, 127], in1=T5[:, :, 127], op=ALU.mult), reads=[], writes=["q127"])
                    P.op("dve", lambda e: e.tensor_tensor(out=s_b[:], in0=T2[:, :, 127], in1=T6[:, :, 127], op=ALU.mult), reads=["q127"], writes=["q127"])
                    P.op("dve", lambda e: e.tensor_tensor(out=s_c[:], in0=s_a[:], in1=lbr[:], op=ALU.mult), reads=["q127"], writes=["q127"])
                    P.op("dve", lambda e: e.tensor_tensor(out=s_d[:], in0=s_b[:], in1=lbi[:], op=ALU.mult), reads=["q127"], writes=["q127"])
                    P.op("dve", lambda e: e.tensor_tensor(out=L_re[:], in0=s_c[:], in1=s_d[:], op=ALU.subtract), reads=["q127"], writes=["L_re"])
                    P.op("dve", lambda e: e.tensor_tensor(out=s_c[:], in0=s_a[:], in1=lbi[:], op=ALU.mult), reads=["q127", "L_re"], writes=["q127"])
                    P.op("dve", lambda e: e.tensor_tensor(out=s_d[:], in0=s_b[:], in1=lbr[:], op=ALU.mult), reads=["q127"], writes=["q127"])
                    P.op("dve", lambda e: e.tensor_tensor(out=L_im[:], in0=s_c[:], in1=s_d[:], op=ALU.add), reads=["q127"], writes=["L_im"])
                    P.barrier()
                    for nm_, t_ in [("lbr", lbr), ("lbi", lbi), ("cr", cr), ("ci", ci), ("om", om), ("a_", a_), ("sn", sn), ("cs", cs)]:
                        dbg_dump(P, nm_, t_[:], [128, 32], F32, [])
                    for nm_, t_ in [("Pr", Pr), ("Pi", Pi), ("Qr", Qr), ("Qi", Qi)]:
                        dbg_dump(P, nm_, t_[:], [128, 32, 128], BF16, [])
                    for nm_, t_ in [("T5", T5), ("T6", T6), ("T2", T2), ("T3", T3)]:
                        dbg_dump(P, nm_, t_[:], [128, 32, 128], F32, [])
                    P.barrier()
                Ccr = sb("Ccr", [128, 32, 128]); Cci = sb("Cci", [128, 32, 128])
                with ExitStack() as ph2:
                    sb2 = lambda n, s, d=F32: ph2.enter_context(nc.sbuf_tensor(n, list(s), d))
                    Cr_ = sb2("Cr_", [128, 32, 128]); Ci_ = sb2("Ci_", [128, 32, 128]); Tm = sb2("Tm", [128, 32, 128])
                    P.dma(lambda q: q.dma_start(out=Cr_[:], in_=Cre), writes=["Cr_"], key="ld")
                    P.dma(lambda q: q.dma_start(out=Ci_[:], in_=Cim), writes=["Ci_"], key="ld")
                    cr_b = cr[:].unsqueeze(2).broadcast_to([128, 32, 128]); ci_b = ci[:].unsqueeze(2).broadcast_to([128, 32, 128])
                    P.op("dve", lambda e: e.tensor_tensor(out=Ccr[:], in0=Cr_[:], in1=cr_b, op=ALU.mult), reads=["Cr_", "cr"], writes=["Ccr"])
                    P.op("dve", lambda e: e.tensor_tensor(out=Tm[:], in0=Ci_[:], in1=ci_b, op=ALU.mult), reads=["Ci_", "ci"], writes=["Tm"])
                    P.op("dve", lambda e: e.tensor_tensor(out=Ccr[:], in0=Ccr[:], in1=Tm[:], op=ALU.subtract), reads=["Ccr", "Tm"], writes=["Ccr"])
                    P.op("dve", lambda e: e.tensor_tensor(out=Cci[:], in0=Cr_[:], in1=ci_b, op=ALU.mult), reads=["Cr_", "ci"], writes=["Cci"])
                    P.op("dve", lambda e: e.tensor_tensor(out=Tm[:], in0=Ci_[:], in1=cr_b, op=ALU.mult), reads=["Ci_", "cr", "Ccr"], writes=["Tm"])
                    P.op("dve", lambda e: e.scalar_tensor_tensor(out=Cci[:], in0=Cci[:], scalar=-1.0, in1=Tm[:], op0=ALU.mult, op1=ALU.subtract),
                         reads=["Cci", "Tm"], writes=["Cci"])
                    P.barrier()
                wu = sb("wu", [128, 16, 1024], BF16)
                with ExitStack() as ph2:
                    stg = [ph2.enter_context(nc.sbuf_tensor("stg%d" % i, [128, 1024], F32)) for i in range(2)]
                    load_w_bf16(wu, w_in[:, 0:1024], 16, 1024, stg, "wu")
                    P.barrier()
                hTg = [sb("hTg%d" % i, [128, 16, 512], BF16) for i in range(2)]
                uT = [sb("uT%d" % i, [128, 8, 512], BF16) for i in range(2)]
                v_re = [sb("v_re%d" % i, [128, 4, 128]) for i in range(2)]; v_im = [sb("v_im%d" % i, [128, 4, 128]) for i in range(2)]
                tt = [sb("tt%d" % i, [128, 4, 128]) for i in range(8)]
                w_re = [sb("w_re%d" % i, [128, 4, 128]) for i in range(2)]; w_im = [sb("w_im%d" % i, [128, 4, 128]) for i in range(2)]
                rr = [sb("rr%d" % i, [128, 4]) for i in range(4)]
                x_re = [sb("x_re%d" % i, [128, 4, 128]) for i in range(2)]; x_im = [sb("x_im%d" % i, [128, 4, 128]) for i in range(2)]
                yv = sb("yv", [128, 128]); ygT = sb("ygT", [128, 8, 128], BF16); c1 = sb("c1", [128, 4]); c2 = sb("c2", [128, 4])
                P.op("pool", lambda e: e.memset(wi_re[:], 0.0), writes=["wi_re"])
                P.op("pool", lambda e: e.memset(wi_im[:], 0.0), writes=["wi_im"])
                it = 0
                for g in range(16):
                    hT_ = hTg[g % 2]; uT_ = uT[g % 2]
                    P.dma(lambda q, hT_=hT_, g=g: q.dma_start(out=hT_[:], in_=hT_d[:, :, g * 512:(g + 1) * 512].rearrange("k p t -> p k t")),
                          reads=["hT_d"], writes=[("hTg", g % 2)], key="hld")
                    for fc in range(8):
                        pu = psum[fc % 2]
                        for kt in range(16):
                            P.op("pe", lambda e, pu=pu, kt=kt, fc=fc, hT_=hT_: e.matmul(pu[:], lhsT=wu[:, kt, fc * 128:(fc + 1) * 128], rhs=hT_[:, kt, :], start=(kt == 0), stop=(kt == 15)),
                                 reads=[("wu", kt), ("hTg", g % 2)], writes=["ps%d" % (fc % 2)])
                        P.op("act", lambda e, pu=pu, fc=fc, uT_=uT_: e.activation(out=uT_[:, fc, :], in_=pu[:], func=AF.Copy), reads=["ps%d" % (fc % 2)], writes=[("uT", g % 2, fc)])
                    for cc in range(4):
                        c = g * 4 + cc
                        tok = slice(cc * 128, (cc + 1) * 128)
                        for fc in range(8):
                            xr = x_re[it % 2]; xi = x_im[it % 2]; xk = ("x", it % 2); it += 1
                            pbr = psum[2]; pbi = psum[3]
                            for k4 in range(4):
                                st_ = fc * 4 + k4
                                P.op("pe", lambda e, k4=k4, st_=st_, fc=fc, uT_=uT_, tok=tok: e.matmul(pbr[:, k4 * 128:(k4 + 1) * 128], lhsT=Bre[:, st_, :], rhs=uT_[:, fc, tok], start=True, stop=True),
                                     reads=["Bre", ("uT", g % 2, fc)], writes=["ps2"])
                                P.op("pe", lambda e, k4=k4, st_=st_, fc=fc, uT_=uT_, tok=tok: e.matmul(pbi[:, k4 * 128:(k4 + 1) * 128], lhsT=Bim[:, st_, :], rhs=uT_[:, fc, tok], start=True, stop=True),
                                     reads=["Bim", ("uT", g % 2, fc)], writes=["ps3"])
                            st0 = fc * 4
                            pr_ = Pr[:, st0:st0 + 4, :]; pi_ = Pi[:, st0:st0 + 4, :]; qr_ = Qr[:, st0:st0 + 4, :]; qi_ = Qi[:, st0:st0 + 4, :]
                            br4 = pbr[:].rearrange("p (k t) -> p k t", k=4); bi4 = pbi[:].rearrange("p (k t) -> p k t", k=4)
                            b2 = (it - 1) % 2
                            vr = v_re[b2]; vi = v_im[b2]; wr = w_re[b2]; wim = w_im[b2]
                            ta, tb, tc, td, te, tf, tg_, th_ = tt
                            P.op("dve", lambda e, br4=br4, pr_=pr_: e.tensor_tensor(out=ta[:], in0=br4, in1=pr_, op=ALU.mult), reads=["ps2", "Pr"], writes=["ta"])
                            P.op("dve", lambda e, bi4=bi4, pi_=pi_: e.tensor_tensor(out=tb[:], in0=bi4, in1=pi_, op=ALU.mult), reads=["ps3", "Pi"], writes=["tb"])
                            P.op("pool", lambda e, vr=vr: e.tensor_tensor(out=vr[:], in0=ta[:], in1=tb[:], op=ALU.subtract), reads=["ta", "tb"], writes=[("v_re", b2)])
                            P.op("dve", lambda e, br4=br4, pi_=pi_: e.tensor_tensor(out=tc[:], in0=br4, in1=pi_, op=ALU.mult), reads=["ps2", "Pi"], writes=["tc"])
                            P.op("dve", lambda e, bi4=bi4, pr_=pr_: e.tensor_tensor(out=td[:], in0=bi4, in1=pr_, op=ALU.mult), reads=["ps3", "Pr"], writes=["td"])
                            P.op("pool", lambda e, vi=vi: e.tensor_tensor(out=vi[:], in0=tc[:], in1=td[:], op=ALU.add), reads=["tc", "td"], writes=[("v_im", b2)])
                            if c < OWN0:
                                r0_, r1_, r2_, r3_ = rr
                                Lr_s = L_re[:, st0:st0 + 4]; Li_s = L_im[:, st0:st0 + 4]
                                P.op("dve", lambda e, vr=vr: e.tensor_reduce(out=r0_[:], in_=vr[:], axis=AX.X, op=ALU.add), reads=[("v_re", b2)], writes=["r0"])
                                P.op("dve", lambda e, vi=vi: e.tensor_reduce(out=r1_[:], in_=vi[:], axis=AX.X, op=ALU.add), reads=[("v_im", b2)], writes=["r1"])
                                P.op("dve", lambda e, st0=st0: e.tensor_tensor(out=r0_[:], in0=r0_[:], in1=wi_re[:, st0:st0 + 4], op=ALU.add), reads=["r0", "wi_re"], writes=["r0"])
                                P.op("dve", lambda e, st0=st0: e.tensor_tensor(out=r1_[:], in0=r1_[:], in1=wi_im[:, st0:st0 + 4], op=ALU.add), reads=["r1", "wi_im"], writes=["r1"])
                                P.op("dve", lambda e, Lr_s=Lr_s: e.tensor_tensor(out=r2_[:], in0=r0_[:], in1=Lr_s, op=ALU.mult), reads=["r0", "L_re"], writes=["r2"])
                                P.op("dve", lambda e, Li_s=Li_s: e.tensor_tensor(out=r3_[:], in0=r1_[:], in1=Li_s, op=ALU.mult), reads=["r1", "L_im"], writes=["r3"])
                                P.op("dve", lambda e, st0=st0: e.tensor_tensor(out=wi_re[:, st0:st0 + 4], in0=r2_[:], in1=r3_[:], op=ALU.subtract), reads=["r2", "r3"], writes=["wi_re"])
                                P.op("dve", lambda e, Li_s=Li_s: e.tensor_tensor(out=r2_[:], in0=r0_[:], in1=Li_s, op=ALU.mult), reads=["r0", "L_im"], writes=["r2"])
                                P.op("dve", lambda e, Lr_s=Lr_s: e.tensor_tensor(out=r3_[:], in0=r1_[:], in1=Lr_s, op=ALU.mult), reads=["r1", "L_re"], writes=["r3"])
                                P.op("dve", lambda e, st0=st0: e.tensor_tensor(out=wi_im[:, st0:st0 + 4], in0=r2_[:], in1=r3_[:], op=ALU.add), reads=["r2", "r3"], writes=["wi_im"])
                                continue
                            for k4 in range(4):
                                st_ = st0 + k4
                                P.op("dve", lambda e, k4=k4, st_=st_, vr=vr, wr=wr: e.tensor_tensor_scan(out=wr[:, k4, :], data0=ones[:, 0:128], data1=vr[:, k4, :], initial=wi_re[:, st_:st_ + 1], op0=ALU.mult, op1=ALU.add),
                                     reads=[("v_re", b2), "ones", "wi_re"], writes=[("w_re", b2)])
                                P.op("dve", lambda e, k4=k4, st_=st_, vi=vi, wim=wim: e.tensor_tensor_scan(out=wim[:, k4, :], data0=ones[:, 0:128], data1=vi[:, k4, :], initial=wi_im[:, st_:st_ + 1], op0=ALU.mult, op1=ALU.add),
                                     reads=[("v_im", b2), "ones", "wi_im"], writes=[("w_im", b2)])
                            P.op("pool", lambda e, qr_=qr_, wr=wr: e.tensor_tensor(out=te[:], in0=wr[:], in1=qr_, op=ALU.mult), reads=[("w_re", b2), "Qr"], writes=["te"])
                            P.op("pool", lambda e, qi_=qi_, wim=wim: e.tensor_tensor(out=tf[:], in0=wim[:], in1=qi_, op=ALU.mult), reads=[("w_im", b2), "Qi"], writes=["tf"])
                            P.op("pool", lambda e, xr=xr: e.tensor_tensor(out=xr[:], in0=te[:], in1=tf[:], op=ALU.subtract), reads=["te", "tf"], writes=[xk])
                            P.op("dve", lambda e, qi_=qi_, wr=wr: e.tensor_tensor(out=tg_[:], in0=wr[:], in1=qi_, op=ALU.mult), reads=[("w_re", b2), "Qi"], writes=["tg"])
                            P.op("dve", lambda e, qr_=qr_, wim=wim: e.tensor_tensor(out=th_[:], in0=wim[:], in1=qr_, op=ALU.mult), reads=[("w_im", b2), "Qr"], writes=["th"])
                            P.op("pool", lambda e, xi=xi: e.tensor_tensor(out=xi[:], in0=tg_[:], in1=th_[:], op=ALU.add), reads=["tg", "th"], writes=[xk])
                            xr_l = xr[:, :, 127]; xi_l = xi[:, :, 127]
                            lr_s = lbr[:, st0:st0 + 4]; li_s = lbi[:, st0:st0 + 4]
                            P.op("dve", lambda e, xr_l=xr_l, lr_s=lr_s: e.tensor_tensor(out=c1[:], in0=xr_l, in1=lr_s, op=ALU.mult), reads=[xk, "lbr"], writes=["c1"])
                            P.op("dve", lambda e, xi_l=xi_l, li_s=li_s: e.tensor_tensor(out=c2[:], in0=xi_l, in1=li_s, op=ALU.mult), reads=[xk, "lbi"], writes=["c2"])
                            P.op("dve", lambda e, st0=st0: e.tensor_tensor(out=wi_re[:, st0:st0 + 4], in0=c1[:], in1=c2[:], op=ALU.subtract), reads=["c1", "c2"], writes=["wi_re"])
                            P.op("dve", lambda e, xr_l=xr_l, li_s=li_s: e.tensor_tensor(out=c1[:], in0=xr_l, in1=li_s, op=ALU.mult), reads=[xk, "lbi"], writes=["c1"])
                            P.op("dve", lambda e, xi_l=xi_l, lr_s=lr_s: e.tensor_tensor(out=c2[:], in0=xi_l, in1=lr_s, op=ALU.mult), reads=[xk, "lbr"], writes=["c2"])
                            P.op("dve", lambda e, st0=st0: e.tensor_tensor(out=wi_im[:, st0:st0 + 4], in0=c1[:], in1=c2[:], op=ALU.add), reads=["c1", "c2"], writes=["wi_im"])
                            if c >= OWN0:
                                py = psum[4]
                                for k4 in range(4):
                                    st_ = st0 + k4
                                    P.op("pe", lambda e, k4=k4, st_=st_, xr=xr: e.matmul(py[:, 0:128], lhsT=Ccr[:, st_, :], rhs=xr[:, k4, :], start=(k4 == 0), stop=False),
                                         reads=["Ccr", xk], writes=["ps4"])
                                    P.op("pe", lambda e, k4=k4, st_=st_, xi=xi: e.matmul(py[:, 0:128], lhsT=Cci[:, st_, :], rhs=xi[:, k4, :], start=False, stop=(k4 == 3)),
                                         reads=["Cci", xk], writes=["ps4"])
                                P.op("dve", lambda e, fc=fc, uT_=uT_, tok=tok: e.scalar_tensor_tensor(out=yv[:], in0=uT_[:, fc, tok], scalar=dskt[:, fc:fc + 1], in1=py[:, 0:128], op0=ALU.mult, op1=ALU.add),
                                     reads=[("uT", g % 2, fc), "dskt", "ps4"], writes=["yv"])
                                P.op("act", lambda e, fc=fc: e.activation(out=ygT[:, fc, :], in_=yv[:], func=AF.Gelu_apprx_tanh), reads=["yv"], writes=["ygT"])
                        if c >= OWN0:
                            o0 = (c - OWN0) * 128
                            P.dma(lambda q, o0=o0: q.dma_start(out=ygT_d[:, :, o0:o0 + 128].rearrange("k p t -> p k t"), in_=ygT[:]),
                                  reads=["ygT"], writes=["ygT_d"], key="st")
            P.barrier()
            stage_end(3)

            with ExitStack() as ph:
                sb = lambda n, s, d=F32: ph.enter_context(nc.sbuf_tensor(n, list(s), d))
                wk = sb("wk", [128, 16, 1024], BF16)
                stg = [sb("stgk%d" % i, [128, 1024]) for i in range(4)]
                hTg = [sb("hTk%d" % i, [128, 16, 512], BF16) for i in range(2)]
                gbc = sb("gbc", [128, 128]); junk = sb("junkk", [128, 128]); ssk = sb("ssk", [128, 8]); rsk = sb("rsk", [128, 8])
                kn = sb("kn", [128, 1024], BF16); kTt = [sb("kTt%d" % i, [128, 8, 512], BF16) for i in range(2)]
                vsb = [sb("vsb%d" % i, [128, 1024], BF16) for i in range(2)]
                for pas in ("k", "v", "q"):
                    col0 = {"k": 2048, "v": 3072, "q": 1024}[pas]
                    P.barrier()
                    load_w_bf16(wk, w_in[:, col0:col0 + 1024], 16, 1024, stg, "wk")
                    if pas in ("k", "q"):
                        gsrc = kg if pas == "k" else qg
                        P.dma(lambda q, gsrc=gsrc: q.dma_start(out=gbc[:], in_=gsrc.broadcast_to([128, 128])), writes=["gbc"], key="ld")
                        if pas == "q":
                            P.op("dve", lambda e: e.tensor_scalar(out=gbc[:], in0=gbc[:], scalar1=128.0 ** -0.5, scalar2=None, op0=ALU.mult), reads=["gbc"], writes=["gbc"])
                    g_lo = 12 if pas == "q" else 0
                    for g in range(g_lo, 16):
                        hT_ = hTg[g % 2]
                        P.dma(lambda q, hT_=hT_, g=g: q.dma_start(out=hT_[:], in_=hT_d[:, :, g * 512:(g + 1) * 512].rearrange("k p t -> p k t")),
                              reads=["hT_d"], writes=[("hTk", g % 2)], key="hld")
                        for cc in range(4):
                            c = g * 4 + cc
                            for nb in range(2):
                                pp = psum[nb]
                                for kt in range(16):
                                    P.op("pe", lambda e, pp=pp, kt=kt, nb=nb, hT_=hT_, cc=cc: e.matmul(pp[:], lhsT=hT_[:, kt, cc * 128:(cc + 1) * 128], rhs=wk[:, kt, nb * 512:(nb + 1) * 512], start=(kt == 0), stop=(kt == 15)),
                                         reads=[("hTk", g % 2), ("wk", kt)], writes=["ps%d" % nb])
                            if pas == "v":
                                v_ = vsb[c % 2]
                                P.op("act", lambda e, v_=v_: e.activation(out=v_[:, 0:512], in_=psum[0][:], func=AF.Copy), reads=["ps0"], writes=[("vsb", c % 2)])
                                P.op("dve", lambda e, v_=v_: e.tensor_copy(out=v_[:, 512:1024], in_=psum[1][:]), reads=["ps1"], writes=[("vsb", c % 2)])
                                P.dma(lambda q, v_=v_, c=c: q.dma_start(out=v_d[c * 128:(c + 1) * 128, :], in_=v_[:]), reads=[("vsb", c % 2)], writes=["v_d"], key="st")
                                continue
                            for h in range(8):
                                pp = psum[h // 4]; hs = slice((h % 4) * 128, (h % 4 + 1) * 128)
                                P.op("act", lambda e, pp=pp, hs=hs, h=h: e.activation(out=junk[:], in_=pp[:, hs], func=AF.Square, accum_out=ssk[:, h:h + 1]),
                                     reads=["ps%d" % (h // 4)], writes=["junkk", "ssk"])
                            P.op("dve", lambda e: e.tensor_scalar(out=ssk[:], in0=ssk[:], scalar1=1.0 / 128, scalar2=EPS, op0=ALU.mult, op1=ALU.add), reads=["ssk"], writes=["ssk"])
                            P.op("act", lambda e: e.activation(out=ssk[:], in_=ssk[:], func=AF.Sqrt), reads=["ssk"], writes=["ssk"])
                            P.op("dve", lambda e: e.reciprocal(out=rsk[:], in_=ssk[:]), reads=["ssk"], writes=["rsk"])
                            for h in range(8):
                                pp = psum[h // 4]; hs = slice((h % 4) * 128, (h % 4 + 1) * 128)
                                P.op("dve", lambda e, pp=pp, hs=hs, h=h: e.scalar_tensor_tensor(out=kn[:, h * 128:(h + 1) * 128], in0=pp[:, hs], scalar=rsk[:, h:h + 1], in1=gbc[:], op0=ALU.mult, op1=ALU.mult),
                                     reads=["ps%d" % (h // 4), "rsk", "gbc"], writes=["kn"])
                            for h in range(8):
                                P.op("pe", lambda e, h=h: e.transpose(psb[0][:, h * 128:(h + 1) * 128], in_=kn[:, h * 128:(h + 1) * 128], identity=identb[:]),
                                     reads=["kn", "identb"], writes=["psb0"])
                            kT_ = kTt[g % 2]
                            P.op("act", lambda e, kT_=kT_, cc=cc: e.activation(out=kT_[:, :, cc * 128:(cc + 1) * 128], in_=psb[0][:].rearrange("p (h t) -> p h t", h=8), func=AF.Copy),
                                 reads=["psb0"], writes=[("kTt", g % 2)])
                        if pas == "k":
                            P.dma(lambda q, g=g: q.dma_start(out=kT_d[:, :, g * 512:(g + 1) * 512].rearrange("h p t -> p h t"), in_=kTt[g % 2][:]),
                                  reads=[("kTt", g % 2)], writes=["kT_d"], key="st")
                        elif pas == "q":
                            o0 = (g - 12) * 512
                            P.dma(lambda q, g=g, o0=o0: q.dma_start(out=qT_d[:, :, o0:o0 + 512].rearrange("h p t -> p h t"), in_=kTt[g % 2][:]),
                                  reads=[("kTt", g % 2)], writes=["qT_d"], key="st")
            P.barrier()
            stage_end(4)

            with ExitStack() as ph:
                sb = lambda n, s, d=F32: ph.enter_context(nc.sbuf_tensor(n, list(s), d))
                kTh = [sb("kTh%d" % i, [128, SEQ], BF16) for i in range(2)]
                vh = [sb("vh%d" % i, [128, NT, 128], BF16) for i in range(2)]
                qTh = [sb("qTh%d" % i, [128, 2048], BF16) for i in range(2)]
                lw0 = sb("lw0", [128, SEQ]); ee = [sb("ee%d" % i, [128, 512]) for i in range(2)]; sp = [sb("sp%d" % i, [128, 512]) for i in range(2)]
                pref = [sb("pref%d" % i, [128, 512]) for i in range(2)]; zs = [sb("zs%d" % i, [128, 512]) for i in range(2)]
                wb = [sb("wb%d" % i, [128, 512], BF16) for i in range(2)]; wTs = [sb("wTs%d" % i, [128, 4, 128], BF16) for i in range(2)]
                negT = sb("negT", [128, 1]); attT = [sb("attT%d" % i, [128, 2048], BF16) for i in range(2)]
                zero1 = sb("zero1", [128, 1]); nTt = sb("nTt", [128, 1])
                P.op("pool", lambda e: e.memset(zero1[:], 0.0), writes=["zero1"])
                pidx = 0
                for h in range(8):
                    kT_ = kTh[h % 2]; v_ = vh[h % 2]; q_ = qTh[h % 2]; at_ = attT[h % 2]
                    P.dma(lambda q, kT_=kT_, h=h: q.dma_start(out=kT_[:], in_=kT_d[h]), reads=["kT_d"], writes=[("kTh", h % 2)], key="ald")
                    P.dma(lambda q, v_=v_, h=h: q.dma_start(out=v_[:], in_=v_d[:, h * 128:(h + 1) * 128].rearrange("(c p) d -> p c d", p=128)),
                          reads=["v_d"], writes=[("vh", h % 2)], key="ald")
                    P.dma(lambda q, q_=q_, h=h: q.dma_start(out=q_[:], in_=qT_d[h]), reads=["qT_d"], writes=[("qTh", h % 2)], key="ald")
                    for j in range(16):
                        ntile = OWN0 + j + 1
                        pieces = []
                        t0 = 0
                        while t0 < ntile:
                            n = min(4, ntile - t0); pieces.append((t0, n)); t0 += n
                        for pi_, (t0, n) in enumerate(pieces):
                            W = n * 128; ks = slice(t0 * 128, t0 * 128 + W)
                            pz = psum[pidx % 2]; pzk = "ps%d" % (pidx % 2); b_ = pidx % 2; pidx += 1
                            ee_ = ee[b_]; sp_ = sp[b_]; pref_ = pref[b_]; zs_ = zs[b_]
                            P.op("pe", lambda e, pz=pz, W=W, ks=ks, q_=q_, kT_=kT_, j=j: e.matmul(pz[:, 0:W], lhsT=q_[:, j * 128:(j + 1) * 128], rhs=kT_[:, ks], start=True, stop=True),
                                 reads=[("qTh", h % 2), ("kTh", h % 2)], writes=[pzk])
                            P.op("act", lambda e, pz=pz, W=W, ee_=ee_: e.activation(out=ee_[:, 0:W], in_=pz[:, 0:W], func=AF.Exp), reads=[pzk], writes=[("ee", b_)])
                            P.op("act", lambda e, W=W, ee_=ee_, sp_=sp_: e.activation(out=sp_[:, 0:W], in_=ee_[:, 0:W], func=AF.Ln, bias=1.0), reads=[("ee", b_)], writes=[("sp", b_)])
                            last = (pi_ == len(pieces) - 1)
                            if last:
                                P.op("pool", lambda e, W=W, sp_=sp_: e.tensor_tensor(out=sp_[:, W - 128:W], in0=sp_[:, W - 128:W], in1=cmask[:], op=ALU.mult), reads=[("sp", b_), "cmask"], writes=[("sp", b_)])
                            init = zero1[:, 0:1] if pi_ == 0 else pref[1 - b_][:, 511:512]
                            P.op("dve", lambda e, W=W, init=init, sp_=sp_, pref_=pref_: e.tensor_tensor_scan(out=pref_[:, 0:W], data0=ones[:, 0:W], data1=sp_[:, 0:W], initial=init, op0=ALU.mult, op1=ALU.add),
                                 reads=[("sp", b_), "ones", "zero1", ("pref", 1 - b_)], writes=[("pref", b_)])
                            P.op("dve", lambda e, pz=pz, W=W, sp_=sp_, zs_=zs_: e.tensor_tensor(out=zs_[:, 0:W], in0=pz[:, 0:W], in1=sp_[:, 0:W], op=ALU.subtract), reads=[pzk, ("sp", b_)], writes=[("zs", b_)])
                            P.op("pool", lambda e, W=W, ks=ks, zs_=zs_, pref_=pref_: e.tensor_tensor(out=lw0[:, ks], in0=zs_[:, 0:W], in1=pref_[:, 0:W], op=ALU.add), reads=[("zs", b_), ("pref", b_)], writes=["lw0"])
                            if last:
                                P.op("pool", lambda e, W=W, pref_=pref_: e.tensor_scalar(out=nTt[:], in0=pref_[:, W - 1:W], scalar1=-1.0, scalar2=None, op0=ALU.mult), reads=[("pref", b_)], writes=["negTT"])
                        pa = psum[4]
                        for pi_, (t0, n) in enumerate(pieces):
                            W = n * 128; ks = slice(t0 * 128, t0 * 128 + W)
                            w_ = wb[pi_ % 2]; wT_ = wTs[pi_ % 2]
                            P.op("act", lambda e, W=W, ks=ks, w_=w_: e.activation(out=w_[:, 0:W], in_=lw0[:, ks], func=AF.Exp, bias=nTt[:, 0:1]), reads=["lw0", "negTT"], writes=[("wb", pi_ % 2)])
                            if pi_ == len(pieces) - 1:
                                P.op("pool", lambda e, W=W, w_=w_: e.tensor_tensor(out=w_[:, W - 128:W], in0=w_[:, W - 128:W], in1=cmask[:], op=ALU.mult), reads=[("wb", pi_ % 2), "cmask"], writes=[("wb", pi_ % 2)])
                            for i4 in range(n):
                                P.op("pe", lambda e, i4=i4, w_=w_: e.transpose(psb[1][:, i4 * 128:(i4 + 1) * 128], in_=w_[:, i4 * 128:(i4 + 1) * 128], identity=identb[:]),
                                     reads=[("wb", pi_ % 2), "identb"], writes=["psb1"])
                            P.op("dve", lambda e, n=n, wT_=wT_: e.tensor_copy(out=wT_[:, 0:n, :], in_=psb[1][:, 0:n * 128].rearrange("p (k t) -> p k t", k=n)), reads=["psb1"], writes=[("wTs", pi_ % 2)])
                            for i4 in range(n):
                                tl = t0 + i4
                                P.op("pe", lambda e, i4=i4, tl=tl, v_=v_, wT_=wT_, ntile=ntile: e.matmul(pa[:, 0:128], lhsT=v_[:, tl, :], rhs=wT_[:, i4, :], start=(tl == 0), stop=(tl == ntile - 1)),
                                     reads=[("vh", h % 2), ("wTs", pi_ % 2)], writes=["ps4"])
                        P.op("act", lambda e, at_=at_, j=j: e.activation(out=at_[:, j * 128:(j + 1) * 128], in_=pa[:, 0:128], func=AF.Copy), reads=["ps4"], writes=[("attT", h % 2)])
                    P.dma(lambda q, at_=at_, h=h: q.dma_start(out=attT_d[h], in_=at_[:]), reads=[("attT", h % 2)], writes=["attT_d"], key="st")
            P.barrier()
            stage_end(5)

            with ExitStack() as ph:
                sb = lambda n, s, d=F32: ph.enter_context(nc.sbuf_tensor(n, list(s), d))
                gt1 = sb("gt1", [128, D]); gs2 = sb("gs2", [128, D]); sh2 = sb("sh2", [128, D]); junk = sb("junkb", [128, D]); g2 = junk
                bc_row(gt1[:], mod_d[0:1, 2 * D:3 * D], D, "gt1")
                bc_row(sh2[:], mod_d[0:1, 3 * D:4 * D], D, "sh2")
                bc_row(gs2[:], mod_d[0:1, 4 * D:5 * D], D, "gs2")
                P.dma(lambda q: q.dma_start(out=g2[:], in_=norm2_g.broadcast_to([128, D])), writes=["junkB"], key="ld")
                P.op("dve", lambda e: e.scalar_tensor_tensor(out=gs2[:], in0=gs2[:], scalar=1.0, in1=g2[:], op0=ALU.add, op1=ALU.mult), reads=["gs2", "junkB"], writes=["gs2"])
                hTo = sb("hTo", [128, 16, 512], BF16); ygo = sb("ygo", [128, 8, 512], BF16); ato = sb("ato", [128, 8, 512], BF16)
                mT = sb("mT", [128, 16, 512], BF16)
                stg = [sb("stgb%d" % i, [128, 512]) for i in range(8)]
                wgs = sb("wgs", [128, 16, 128], BF16); wga = sb("wga", [128, 16, 128], BF16)
                wgv = sb("wgv", [128, 8, 128], BF16); wgg = sb("wgg", [128, 8, 128], BF16); wau = sb("wau", [128, 8, 128], BF16)
                wo = sb("wo", [128, 16, 512], BF16)
                sgs = sb("sgs", [128, 512]); sga = sb("sga", [128, 512]); syg = sb("syg", [128, 512]); tm1 = sb("tm1", [128, 512]); tm2 = sb("tm2", [128, 512])
                xall = sb("xall", [128, 4, D])
                tA = sb("tAb", [128, D]); hb2 = sb("hb2", [128, D], BF16)
                ss = sb("ssb", [128, 1]); rstd = sb("rstdb", [128, 1]); h2t = sb("h2t", [128, 16, 512], BF16)
                for tg in range(4):
                    o0 = tg * 512; wt0 = (OWN0 * 128) + o0
                    P.dma(lambda q, wt0=wt0: q.dma_start(out=hTo[:], in_=hT_d[:, :, wt0:wt0 + 512].rearrange("k p t -> p k t")), reads=["hT_d"], writes=["hTo"], key="ld")
                    P.dma(lambda q, o0=o0: q.dma_start(out=ygo[:], in_=ygT_d[:, :, o0:o0 + 512].rearrange("k p t -> p k t")), reads=["ygT_d"], writes=["ygo"], key="ld")
                    P.dma(lambda q, o0=o0: q.dma_start(out=ato[:], in_=attT_d[:, :, o0:o0 + 512].rearrange("k p t -> p k t")), reads=["attT_d"], writes=["ato"], key="ld")
                    for dc in range(16):
                        cs_ = slice(dc * 128, (dc + 1) * 128)
                        load_w_bf16(wgs, w_in[:, 4096 + dc * 128:4096 + (dc + 1) * 128], 16, 128, stg, "wgs")
                        load_w_bf16(wga, w_in[:, 6144 + dc * 128:6144 + (dc + 1) * 128], 16, 128, stg, "wga")
                        load_w_bf16(wgv, w_glu[:, dc * 128:(dc + 1) * 128], 8, 128, stg, "wgv")
                        load_w_bf16(wgg, w_glu[:, 2048 + dc * 128:2048 + (dc + 1) * 128], 8, 128, stg, "wgg")
                        load_w_bf16(wau, w_up[:, dc * 128:(dc + 1) * 128], 8, 128, stg, "wau")
                        for kt in range(16):
                            P.op("pe", lambda e, kt=kt: e.matmul(psum[0][:], lhsT=wgs[:, kt, :], rhs=hTo[:, kt, :], start=(kt == 0), stop=(kt == 15)), reads=[("wgs", kt), "hTo"], writes=["ps0"])
                        for kt in range(16):
                            P.op("pe", lambda e, kt=kt: e.matmul(psum[1][:], lhsT=wga[:, kt, :], rhs=hTo[:, kt, :], start=(kt == 0), stop=(kt == 15)), reads=[("wga", kt), "hTo"], writes=["ps1"])
                        for kt in range(8):
                            P.op("pe", lambda e, kt=kt: e.matmul(psum[2][:], lhsT=wgv[:, kt, :], rhs=ygo[:, kt, :], start=(kt == 0), stop=(kt == 7)), reads=[("wgv", kt), "ygo"], writes=["ps2"])
                        for kt in range(8):
                            P.op("pe", lambda e, kt=kt: e.matmul(psum[3][:], lhsT=wgg[:, kt, :], rhs=ygo[:, kt, :], start=(kt == 0), stop=(kt == 7)), reads=[("wgg", kt), "ygo"], writes=["ps3"])
                        for kt in range(8):
                            P.op("pe", lambda e, kt=kt: e.matmul(psum[4][:], lhsT=wau[:, kt, :], rhs=ato[:, kt, :], start=(kt == 0), stop=(kt == 7)), reads=[("wau", kt), "ato"], writes=["ps4"])
                        P.op("act", lambda e: e.activation(out=sgs[:], in_=psum[0][:], func=AF.Sigmoid), reads=["ps0"], writes=["sgs"])
                        P.op("act", lambda e: e.activation(out=sga[:], in_=psum[1][:], func=AF.Sigmoid), reads=["ps1"], writes=["sga"])
                        P.op("act", lambda e: e.activation(out=syg[:], in_=psum[3][:], func=AF.Sigmoid), reads=["ps3"], writes=["syg"])
                        P.op("dve", lambda e: e.tensor_tensor(out=tm1[:], in0=psum[2][:], in1=syg[:], op=ALU.mult), reads=["ps2", "syg"], writes=["tm1"])
                        P.op("pool", lambda e: e.tensor_tensor(out=tm1[:], in0=tm1[:], in1=sgs[:], op=ALU.mult), reads=["tm1", "sgs"], writes=["tm1"])
                        P.op("dve", lambda e: e.tensor_tensor(out=tm2[:], in0=psum[4][:], in1=sga[:], op=ALU.mult), reads=["ps4", "sga"], writes=["tm2"])
                        P.op("pool", lambda e, dc=dc: e.tensor_tensor(out=mT[:, dc, :], in0=tm1[:], in1=tm2[:], op=ALU.add), reads=["tm1", "tm2"], writes=[("mT", dc)])
                    for cc in range(4):
                        r0 = (OWN0 + tg * 4 + cc) * 128
                        P.dma(lambda q, cc=cc, r0=r0: q.dma_start(out=xall[:, cc, :], in_=xw[r0:r0 + 128, :]), writes=[("xall", cc)], key="xld")
                    for nb in range(4):
                        load_w_bf16(wo, w_out[:, nb * 512:(nb + 1) * 512], 16, 512, stg, "wo")
                        for cc in range(4):
                            pp = psum[(nb * 4 + cc) % 2]; ppk = "ps%d" % ((nb * 4 + cc) % 2)
                            for dc in range(16):
                                P.op("pe", lambda e, pp=pp, dc=dc, cc=cc: e.matmul(pp[:], lhsT=mT[:, dc, cc * 128:(cc + 1) * 128], rhs=wo[:, dc, :], start=(dc == 0), stop=(dc == 15)),
                                     reads=[("mT", dc), ("wo", dc)], writes=[ppk])
                            P.op("dve", lambda e, pp=pp, nb=nb: e.tensor_tensor(out=tm1[:], in0=pp[:], in1=gt1[:, nb * 512:(nb + 1) * 512], op=ALU.mult),
                                 reads=[ppk, "gt1"], writes=["tm1"])
                            P.op("pool", lambda e, nb=nb, cc=cc: e.tensor_tensor(out=xall[:, cc, nb * 512:(nb + 1) * 512], in0=xall[:, cc, nb * 512:(nb + 1) * 512], in1=tm1[:], op=ALU.add),
                                 reads=["tm1", ("xall", cc)], writes=[("xall", cc)])
                    for cc in range(4):
                        tl = tg * 4 + cc
                        x1_ = xall[:, cc, :]
                        P.dma(lambda q, x1_=x1_, tl=tl: q.dma_start(out=out[tl * 128:(tl + 1) * 128, :], in_=x1_), reads=[("xall", cc)], writes=["out"], key="st")
                        rms_stats(x1_, ("xall", cc), junk[:], ss[:], rstd[:], "B")
                        P.op("dve", lambda e, x1_=x1_: e.scalar_tensor_tensor(out=tA[:], in0=x1_, scalar=rstd[:, 0:1], in1=gs2[:], op0=ALU.mult, op1=ALU.mult),
                             reads=[("xall", cc), "rstdB", "gs2"], writes=["tAb"])
                        P.op("pool", lambda e: e.tensor_tensor(out=hb2[:], in0=tA[:], in1=sh2[:], op=ALU.add), reads=["tAb", "sh2"], writes=["hb2"])
                        for half in range(2):
                            pb = psb[half]
                            for k8 in range(8):
                                kt = half * 8 + k8
                                P.op("pe", lambda e, pb=pb, k8=k8, kt=kt: e.transpose(pb[:, k8 * 128:(k8 + 1) * 128], in_=hb2[:, kt * 128:(kt + 1) * 128], identity=identb[:]),
                                     reads=["hb2", "identb"], writes=["psb%d" % half])
                            dst = h2t[:, half * 8:(half + 1) * 8, cc * 128:(cc + 1) * 128]
                            src = pb[:].rearrange("p (k t) -> p k t", k=8)
                            if half == 0:
                                P.op("dve", lambda e, dst=dst, src=src: e.tensor_copy(out=dst, in_=src), reads=["psb0"], writes=["h2t"])
                            else:
                                P.op("act", lambda e, dst=dst, src=src: e.activation(out=dst, in_=src, func=AF.Copy), reads=["psb1"], writes=["h2t"])
                    P.dma(lambda q, o0=o0: q.dma_start(out=h2T_d[:, :, o0:o0 + 512].rearrange("k p t -> p k t"), in_=h2t[:]), reads=["h2t"], writes=["h2T_d"], key="st")
            P.barrier()
            stage_end(6)
            with ExitStack() as ph:
                sb = lambda n, s, d=F32: ph.enter_context(nc.sbuf_tensor(n, list(s), d))
                gt2 = sb("gt2", [128, D]); k1b = sb("k1b", [128, 128], BF16); k2b = sb("k2b", [128, 128], BF16)
                bc_row(gt2[:], mod_d[0:1, 5 * D:6 * D], D, "gt2")
                h2g = sb("h2g", [128, 16, 512], BF16); acc = sb("acc", [128, 4, D])
                s2 = sb("s2p", [128, 4, 8, 128]); thr = sb("thr", [128, 4, 8, 128])
                e1 = sb("e1p", [128, 4, 8, 128]); E2 = sb("E2p", [128, 4, 8, 128], BF16)
                with ExitStack() as ph2:
                    sb2 = lambda n, s, d=F32: ph2.enter_context(nc.sbuf_tensor(n, list(s), d))
                    kst = sb2("kst", [128, 256])
                    P.dma(lambda q: q.dma_start(out=kst[:, 0:128], in_=k1T), writes=["kst"], key="ld")
                    P.dma(lambda q: q.dma_start(out=kst[:, 128:256], in_=k2T), writes=["kst"], key="ld")
                    P.op("dve", lambda e: e.tensor_copy(out=k1b[:], in_=kst[:, 0:128]), reads=["kst"], writes=["k1b"])
                    P.op("dve", lambda e: e.tensor_copy(out=k2b[:], in_=kst[:, 128:256]), reads=["kst"], writes=["k2b"])
                    P.barrier()
                for tg in range(4):
                    o0 = tg * 512
                    P.barrier()
                    P.dma(lambda q, o0=o0: q.dma_start(out=h2g[:], in_=h2T_d[:, :, o0:o0 + 512].rearrange("k p t -> p k t")), reads=["h2T_d"], writes=["h2g"], key="ld")
                    with ExitStack() as ph2:
                        sb2 = lambda n, s, d=F32, tg=tg: ph2.enter_context(nc.sbuf_tensor("%s_%d" % (n, tg), list(s), d))
                        qT = sb2("qTp", [128, 16, 512], BF16); wqc = sb2("wqc", [128, 16, 128], BF16)
                        stq = [sb2("stq%d" % i, [128, 128]) for i in range(8)]
                        s1 = sb2("s1p", [128, 8, 128]); v1 = sb2("v1p", [128, 8, 16]); v2 = sb2("v2p", [128, 8, 16])
                        scr = sb2("scr", [128, 256]); cand = sb2("cand", [128, 8, 256]); best = sb2("best", [128, 8, 16])
                        eb = sb2("eb", [128, 8, 16]); Zs = sb2("Zs", [128, 8]); lnZ = sb2("lnZ", [128, 8]); off = sb2("offp", [128, 8])
                        tmp = sb2("tmpp", [128, 8, 128])
                        for fcq in range(16):
                            load_w_bf16(wqc, wq[:, fcq * 128:(fcq + 1) * 128], 16, 128, stq, "wqc")
                            pq = psum[fcq % 2]
                            for kt in range(16):
                                P.op("pe", lambda e, pq=pq, kt=kt: e.matmul(pq[:], lhsT=wqc[:, kt, :], rhs=h2g[:, kt, :], start=(kt == 0), stop=(kt == 15)),
                                     reads=[("wqc", kt), "h2g"], writes=["ps%d" % (fcq % 2)])
                            P.op("act", lambda e, pq=pq, fcq=fcq: e.activation(out=qT[:, fcq, :], in_=pq[:], func=AF.Copy), reads=["ps%d" % (fcq % 2)], writes=[("qTp", fcq)])
                        for tl in range(4):
                            ts_ = slice(tl * 128, (tl + 1) * 128)
                            for h in range(8):
                                P.op("pe", lambda e, h=h, ts_=ts_: e.matmul(psum[2 + h // 4][:, (h % 4) * 128:(h % 4 + 1) * 128], lhsT=qT[:, 2 * h, ts_], rhs=k1b[:], start=True, stop=True),
                                     reads=[("qTp", 2 * h), "k1b"], writes=["ps%d" % (2 + h // 4)])
                                P.op("pe", lambda e, h=h, ts_=ts_: e.matmul(psum[4 + h // 4][:, (h % 4) * 128:(h % 4 + 1) * 128], lhsT=qT[:, 2 * h + 1, ts_], rhs=k2b[:], start=True, stop=True),
                                     reads=[("qTp", 2 * h + 1), "k2b"], writes=["ps%d" % (4 + h // 4)])
                            for hh in range(2):
                                P.op("act", lambda e, hh=hh: e.activation(out=s1[:, hh * 4:(hh + 1) * 4, :], in_=psum[2 + hh][:].rearrange("p (h i) -> p h i", h=4), func=AF.Copy),
                                     reads=["ps%d" % (2 + hh)], writes=["s1p"])
                                P.op("dve", lambda e, hh=hh, tl=tl: e.tensor_copy(out=s2[:, tl, hh * 4:(hh + 1) * 4, :], in_=psum[4 + hh][:].rearrange("p (h i) -> p h i", h=4)),
                                     reads=["ps%d" % (4 + hh)], writes=[("s2p", tl)])
                            for (src_, vv, rk) in ((s1, v1, "s1p"), (None, v2, ("s2p", tl))):
                                for h in range(8):
                                    sv = s1[:, h, :] if src_ is not None else s2[:, tl, h, :]
                                    P.op("dve", lambda e, sv=sv, vv=vv, h=h: e.max(out=vv[:, h, 0:8], in_=sv), reads=[rk], writes=["vtop"])
                                    P.op("dve", lambda e, sv=sv, vv=vv, h=h: e.match_replace(out=scr[:, 0:128], in_to_replace=vv[:, h, 0:8], in_values=sv, imm_value=-1e30),
                                         reads=[rk, "vtop"], writes=["scr"])
                                    P.op("dve", lambda e, vv=vv, h=h: e.max(out=vv[:, h, 8:16], in_=scr[:, 0:128]), reads=["scr"], writes=["vtop"])
                            P.op("dve", lambda e: e.tensor_tensor(out=cand[:].rearrange("p h (a b) -> p h a b", a=16), in0=v1[:].unsqueeze(3).broadcast_to([128, 8, 16, 16]),
                                                                  in1=v2[:].unsqueeze(2).broadcast_to([128, 8, 16, 16]), op=ALU.add), reads=["vtop"], writes=["cand"])
                            for h in range(8):
                                P.op("dve", lambda e, h=h: e.max(out=best[:, h, 0:8], in_=cand[:, h, :]), reads=["cand"], writes=["best"])
                                P.op("dve", lambda e, h=h: e.match_replace(out=scr[:], in_to_replace=best[:, h, 0:8], in_values=cand[:, h, :], imm_value=-1e30),
                                     reads=["cand", "best"], writes=["scr"])
                                P.op("dve", lambda e, h=h: e.max(out=best[:, h, 8:16], in_=scr[:]), reads=["scr"], writes=["best"])
                            P.op("dve", lambda e: e.tensor_tensor(out=eb[:], in0=best[:], in1=best[:, :, 0:1].broadcast_to([128, 8, 16]), op=ALU.subtract), reads=["best"], writes=["eb"])
                            P.op("act", lambda e: e.activation(out=eb[:], in_=eb[:], func=AF.Exp), reads=["eb"], writes=["eb"])
                            P.op("dve", lambda e: e.tensor_reduce(out=Zs[:], in_=eb[:], axis=AX.X, op=ALU.add), reads=["eb"], writes=["Zs"])
                            P.op("act", lambda e: e.activation(out=lnZ[:], in_=Zs[:], func=AF.Ln), reads=["Zs"], writes=["lnZ"])
                            P.op("dve", lambda e, tl=tl: e.tensor_tensor(out=thr[:, tl], in0=best[:, :, 15:16].broadcast_to([128, 8, 128]), in1=s1[:], op=ALU.subtract),
                                 reads=["best", "s1p"], writes=[("thr", tl)])
                            P.op("dve", lambda e, tl=tl: e.tensor_scalar(out=thr[:, tl], in0=thr[:, tl], scalar1=-1e-5, scalar2=None, op0=ALU.add),
                                 reads=[("thr", tl)], writes=[("thr", tl)])
                            P.op("dve", lambda e: e.tensor_tensor(out=off[:], in0=v1[:, :, 0], in1=lnZ[:], op=ALU.add), reads=["vtop", "lnZ"], writes=["offp"])
                            P.op("dve", lambda e: e.tensor_tensor(out=tmp[:], in0=s1[:], in1=off[:].unsqueeze(2).broadcast_to([128, 8, 128]), op=ALU.subtract),
                                 reads=["s1p", "offp"], writes=["tmpp"])
                            P.op("act", lambda e, tl=tl: e.activation(out=e1[:, tl], in_=tmp[:], func=AF.Exp), reads=["tmpp"], writes=[("e1p", tl)])
                            P.op("dve", lambda e, tl=tl: e.tensor_tensor(out=tmp[:], in0=s2[:, tl], in1=v2[:, :, 0:1].broadcast_to([128, 8, 128]), op=ALU.subtract),
                                 reads=[("s2p", tl), "vtop", "tmpp"], writes=["tmpp"])
                            P.op("act", lambda e, tl=tl: e.activation(out=E2[:, tl], in_=tmp[:], func=AF.Exp), reads=["tmpp"], writes=[("E2p", tl)])
                        P.barrier()
                    with ExitStack() as ph2:
                        sb2 = lambda n, s, d=F32, tg=tg: ph2.enter_context(nc.sbuf_tensor("%s_%d" % (n, tg), list(s), d))
                        stU = [sb2("stU%d" % i, [128, D]) for i in range(2)]
                        Ub = sb2("Ub", [128, D], BF16)
                        UcT = [sb2("UcT%d" % i, [128, 16, 128], BF16) for i in range(2)]
                        Vc = [sb2("Vc%d" % i, [128, D], BF16) for i in range(8)]
                        gA = [sb2("gA%d" % i, [128, 512]) for i in range(2)]
                        GAT = [sb2("GAT%d" % i, [128, 512], BF16) for i in range(4)]
                        Gm = [sb2("Gmp%d" % i, [128, 8, 128], BF16) for i in range(3)]
                        Gs = [sb2("Gsp%d" % i, [128, 8, 128], BF16) for i in range(3)]
                        xfin = stU
                        WTp = psb[1][:].bitcast(F32)
                        state = {"nld": 0, "ng": 0}

                        def prepA(c):
                            su = stU[state["nld"] % 2]; suk = ("stU", state["nld"] % 2); state["nld"] += 1
                            uT_ = UcT[c % 2]; utk = ("UcT", c % 2)
                            P.dma(lambda q: q.dma_start(out=su[:], in_=u_tab[c * 128:(c + 1) * 128, :]), writes=[suk], key="tab")
                            P.op("pool", lambda e: e.tensor_copy(out=Ub[:], in_=su[:]), reads=[suk], writes=["Ub"])
                            for half in range(2):
                                for k8 in range(8):
                                    kt = half * 8 + k8
                                    P.op("pe", lambda e, k8=k8, kt=kt: e.transpose(psb[0][:, k8 * 128:(k8 + 1) * 128], in_=Ub[:, kt * 128:(kt + 1) * 128], identity=identb[:]),
                                         reads=["Ub", "identb"], writes=["psb0"])
                                P.op("act", lambda e, half=half: e.activation(out=uT_[:, half * 8:(half + 1) * 8, :], in_=psb[0][:].rearrange("p (k t) -> p k t", k=8), func=AF.Copy),
                                     reads=["psb0"], writes=[utk])
                            sv_ = stU[state["nld"] % 2]; svk = ("stU", state["nld"] % 2); state["nld"] += 1
                            vc_ = Vc[c % 8]; vck = ("Vc", c % 8)
                            P.dma(lambda q: q.dma_start(out=sv_[:], in_=v_tab[c * 128:(c + 1) * 128, :]), writes=[svk], key="tab")
                            P.op("act", lambda e: e.activation(out=vc_[:], in_=sv_[:], func=AF.Copy), reads=[svk], writes=[vck])
                            pa = psum[c % 2]; pak = "ps%d" % (c % 2); ga_ = gA[c % 2]; gak = ("gA", c % 2)
                            for kt in range(16):
                                P.op("pe", lambda e, kt=kt: e.matmul(pa[:], lhsT=uT_[:, kt, :], rhs=h2g[:, kt, :], start=(kt == 0), stop=(kt == 15)),
                                     reads=[utk, "h2g"], writes=[pak])
                            P.op("act", lambda e: e.activation(out=ga_[:], in_=pa[:], func=AF.Gelu_apprx_tanh), reads=[pak], writes=[gak])

                        def gate(c):
                            ga_ = gA[c % 2]; gak = ("gA", c % 2); gat_ = GAT[c % 4]; gatk = ("GAT", c % 4)
                            for tl in range(4):
                                b_ = state["ng"] % 3; state["ng"] += 1
                                gm_ = Gm[b_]; gs_ = Gs[b_]
                                for h in range(8):
                                    P.op("dve", lambda e, tl=tl, h=h, gm_=gm_: e.tensor_scalar(out=gm_[:, h, :], in0=s2[:, tl, h, :], scalar1=thr[:, tl, h, c:c + 1], scalar2=e1[:, tl, h, c:c + 1], op0=ALU.is_ge, op1=ALU.mult),
                                         reads=[("s2p", tl), ("thr", tl), ("e1p", tl)], writes=[("Gmp", b_, h)])
                                eng = "dve" if tl % 2 == 0 else "pool"
                                P.op(eng, lambda e, tl=tl, gm_=gm_, gs_=gs_: e.tensor_tensor(out=gs_[:], in0=gm_[:], in1=E2[:, tl], op=ALU.mult),
                                     reads=[("Gmp", b_, h) for h in range(8)] + [("E2p", tl)], writes=[("Gsp", b_)])
                                for h in range(8):
                                    P.op("pe", lambda e, tl=tl, h=h, gs_=gs_: e.matmul(WTp[:, tl * 128:(tl + 1) * 128], lhsT=gs_[:, h, :], rhs=identb[:], start=(h == 0), stop=(h == 7)),
                                         reads=[("Gsp", b_), "identb"], writes=["psb1"])
                            P.op("dve", lambda e: e.tensor_tensor(out=gat_[:], in0=WTp, in1=ga_[:], op=ALU.mult), reads=["psb1", gak], writes=[gatk])

                        def vmm(cg):
                            for tl in range(4):
                                for ci in range(4):
                                    c = cg * 4 + ci
                                    for nb in range(4):
                                        P.op("pe", lambda e, tl=tl, ci=ci, nb=nb, c=c: e.matmul(psum[2 + nb][:], lhsT=GAT[c % 4][:, tl * 128:(tl + 1) * 128], rhs=Vc[c % 8][:, nb * 512:(nb + 1) * 512], start=(ci == 0), stop=(ci == 3)),
                                             reads=[("GAT", c % 4), ("Vc", c % 8)], writes=["ps%d" % (2 + nb)])
                                for nb in range(4):
                                    a_sl = acc[:, tl, nb * 512:(nb + 1) * 512]
                                    if cg == 0:
                                        P.op("dve", lambda e, a_sl=a_sl, nb=nb: e.tensor_copy(out=a_sl, in_=psum[2 + nb][:]), reads=["ps%d" % (2 + nb)], writes=[("acc", tl, nb)])
                                    else:
                                        P.op("dve", lambda e, a_sl=a_sl, nb=nb: e.tensor_tensor(out=a_sl, in0=psum[2 + nb][:], in1=a_sl, op=ALU.add), reads=["ps%d" % (2 + nb), ("acc", tl, nb)], writes=[("acc", tl, nb)])

                        prepA(0)
                        for c in range(128):
                            if c + 1 < 128:
                                prepA(c + 1)
                            gate(c)
                            if c % 4 == 3:
                                vmm(c // 4)
                        for tl in range(4):
                            r0 = o0 + tl * 128
                            xf = xfin[tl % 2]; xfk = ("stU", tl % 2)
                            P.dma(lambda q, xf=xf, r0=r0: q.dma_start(out=xf[:], in_=out[r0:r0 + 128, :]), reads=["out"], writes=[xfk], key="tab")
                            P.op("pool", lambda e, tl=tl: e.tensor_tensor(out=acc[:, tl, :], in0=acc[:, tl, :], in1=gt2[:], op=ALU.mult),
                                 reads=[("acc", tl, 0), ("acc", tl, 1), ("acc", tl, 2), ("acc", tl, 3), "gt2"], writes=[("acc", tl, 0), ("acc", tl, 1), ("acc", tl, 2), ("acc", tl, 3)])
                            P.op("dve", lambda e, tl=tl, xf=xf: e.tensor_tensor(out=xf[:], in0=xf[:], in1=acc[:, tl, :], op=ALU.add),
                                 reads=[xfk, ("acc", tl, 0), ("acc", tl, 1), ("acc", tl, 2), ("acc", tl, 3)], writes=[xfk])
                            P.dma(lambda q, xf=xf, r0=r0: q.dma_start(out=out[r0:r0 + 128, :], in_=xf[:]), reads=[xfk], writes=["out"], key="st")
                        P.barrier()
            P.barrier()
            stage_end(7)
        except _Stop:
            pass
        P.barrier()
        P.emit()
    return nc


_NC_CACHE = {}


def _host_inputs(i, x, c, w_ada, b_ada, norm1_g, w_in, lam_re, lam_im, log_dt, ssm_b_re, ssm_b_im, ssm_c_re, ssm_c_im,
                 ssm_d, w_glu, q_norm_g, k_norm_g, w_att_up, w_out, norm2_g, peer_wq, peer_k1, peer_k2, peer_u, peer_v, shared):
    b, q = i // 4, i % 4
    t_end = (q + 1) * 2048
    t_start = t_end - SEQ
    xw = np.zeros((SEQ, D), np.float32)
    lo = max(t_start, 0)
    xw[lo - t_start:] = x[b, lo:t_end]
    tok = t_start + np.arange(SEQ)
    valid = (tok >= 0).astype(np.float32).reshape(NT, 128).T.copy()
    cbm = np.ascontiguousarray(c[b].reshape(16, 128).T)
    d = dict(shared)
    d.update(xw=xw, valid=valid, cb=cbm)
    return d


def _prep(x, c, w_ada, b_ada, norm1_g, w_in, lam_re, lam_im, log_dt, ssm_b_re, ssm_b_im, ssm_c_re, ssm_c_im,
           ssm_d, w_glu, q_norm_g, k_norm_g, w_att_up, w_out, norm2_g, peer_wq, peer_k1, peer_k2, peer_u, peer_v):
    f = lambda a: np.ascontiguousarray(np.asarray(a, dtype=np.float32))
    x = f(x); c = f(c)
    lamT_re = f(np.asarray(lam_re)[0].reshape(32, 2, 64).transpose(1, 2, 0).reshape(128, 32))
    lamT_im = f(np.asarray(lam_im)[0].reshape(32, 2, 64).transpose(1, 2, 0).reshape(128, 32))
    ldtT = f(np.broadcast_to(np.asarray(log_dt)[0].reshape(32, 2).T[:, None, :], (2, 64, 32)).reshape(128, 32))
    Bre = np.asarray(ssm_b_re)[0]; Bim = np.asarray(ssm_b_im)[0]
    BreT = np.zeros((128, 32, 128), np.float32); BimT = np.zeros((128, 32, 128), np.float32)
    for g in range(64):
        gl = g % 8
        st, g2 = g // 2, g % 2
        BreT[gl * 16:(gl + 1) * 16, st, g2 * 64:(g2 + 1) * 64] = Bre[g].T
        BimT[gl * 16:(gl + 1) * 16, st, g2 * 64:(g2 + 1) * 64] = Bim[g].T
    Cr = np.asarray(ssm_c_re)[0]; Ci = np.asarray(ssm_c_im)[0]
    Cre = np.zeros((128, 32, 128), np.float32); Cim = np.zeros((128, 32, 128), np.float32)
    for g in range(64):
        st, g2, gl = g // 2, g % 2, g % 8
        Cre[g2 * 64:(g2 + 1) * 64, st, gl * 16:(gl + 1) * 16] = Cr[g].T
        Cim[g2 * 64:(g2 + 1) * 64, st, gl * 16:(gl + 1) * 16] = Ci[g].T
    dsk = f(np.asarray(ssm_d)[0].reshape(8, 128).T)
    sidx = f(np.broadcast_to(np.arange(128, dtype=np.float32)[None, :], (128, 128)))
    shared = dict(w_ada=f(np.asarray(w_ada)[0]), b_ada=f(np.asarray(b_ada)[0][None, :]), norm1_g=f(np.asarray(norm1_g)[0][None, :]),
                  norm2_g=f(np.asarray(norm2_g)[0][None, :]), qg=f(np.asarray(q_norm_g)[0][None, :]), kg=f(np.asarray(k_norm_g)[0][None, :]),
                  w_in=f(np.asarray(w_in)[0]), w_glu=f(np.asarray(w_glu)[0]), w_up=f(np.asarray(w_att_up)[0]), w_out=f(np.asarray(w_out)[0]),
                  wq=f(np.asarray(peer_wq)[0]), k1T=f(np.asarray(peer_k1)[0].T), k2T=f(np.asarray(peer_k2)[0].T),
                  u_tab=f(np.asarray(peer_u)[0]), v_tab=f(np.asarray(peer_v)[0]),
                  lamT_re=lamT_re, lamT_im=lamT_im, ldtT=ldtT, BreT=BreT, BimT=BimT, Cre=Cre, Cim=Cim, dsk=dsk, sidx=sidx)
    args = (x, c, w_ada, b_ada, norm1_g, w_in, lam_re, lam_im, log_dt, ssm_b_re, ssm_b_im, ssm_c_re, ssm_c_im,
            ssm_d, w_glu, q_norm_g, k_norm_g, w_att_up, w_out, norm2_g, peer_wq, peer_k1, peer_k2, peer_u, peer_v)
    in_maps = [_host_inputs(i, *args, shared) for i in range(8)]
    return in_maps


def kernel(**inputs):
    in_maps = _prep(**inputs)
    if "nc" not in _NC_CACHE:
        _NC_CACHE["nc"] = build_nc()
    nc = _NC_CACHE["nc"]
    res = run_bass_kernel_spmd(nc, in_maps, core_ids=list(range(8)))
    outp = np.zeros((2, SEQ, D), np.float32)
    for i in range(8):
        b, q = i // 4, i % 4
        outp[b, q * 2048:(q + 1) * 2048] = res.results[i]["out"]
    return outp
```
